# Optimizing a Trainium2 kernel written in Bass

```python
import jax, jax.numpy as jnp
from jax import lax
import numpy as np

D_MODEL = 1024
BATCH = 8
SEQ = 4096
DEPTH = 2

HG_HEADS = 4
HG_DK = 128
HG_W = HG_HEADS * HG_DK
RET_HEADS = 4
RET_DK = 128
RET_W = RET_HEADS * RET_DK
RW_HEADS = 8
RW_N = 64
RW_W = RW_HEADS * RW_N
RW_DECAY_LORA = 64
RW_A_LORA = 64
RW_GATE_LORA = 128
RW_GN_EPS = 64e-5
N_BRANCH = 3
HG_COLS = 4 * HG_W
RET_COLS = 4 * RET_W
RW_COLS = 3 * RW_W + RW_DECAY_LORA + RW_A_LORA + RW_GATE_LORA
GATE_COLS = N_BRANCH * D_MODEL
IN_COLS = HG_COLS + RET_COLS + RW_COLS + GATE_COLS
CHUNK = 64
N_GROUPS = 4
EXPERTS_PER_GROUP = 8
N_EXPERTS = N_GROUPS * EXPERTS_PER_GROUP
TOP_K_IN_GROUP = 2
D_EXPERT = 512

ROPE_THETA = 10000.0
NORM_EPS = 1e-6

kernel_name = 'hybrid_hgrn2_retnet_rwkv7_hmoe'


def split_cols(z, sizes):
    offs = np.cumsum([0] + list(sizes))
    return [z[..., int(offs[i]):int(offs[i + 1])] for i in range(len(sizes))]


def split_heads(z, n_heads):
    b, t, w = z.shape
    return z.reshape(b, t, n_heads, w // n_heads)


def to_chunks(z):
    b, t, h, d = z.shape
    return z.reshape(b, t // CHUNK, CHUNK, h, d).transpose(0, 3, 1, 2, 4)


def from_chunks(z):
    b, h, nc, l, d = z.shape
    return z.transpose(0, 2, 3, 1, 4).reshape(b, nc * l, h, d)


def rmsnorm(x, gain):
    xf = x.astype(jnp.float32)
    y = xf * lax.rsqrt(jnp.mean(xf * xf, axis=-1, keepdims=True) + NORM_EPS)
    return (y * gain.astype(jnp.float32)).astype(x.dtype)


def head_rmsnorm(o):
    return o * lax.rsqrt(jnp.mean(o * o, axis=-1, keepdims=True) + NORM_EPS)


def rope(z, positions):
    d = z.shape[-1]
    inv_freq = ROPE_THETA ** (-jnp.arange(0, d, 2, dtype=jnp.float32) / d)
    ang = positions.astype(jnp.float32)[:, :, None, None] * inv_freq
    cos, sin = jnp.cos(ang), jnp.sin(ang)
    z1, z2 = z[..., : d // 2], z[..., d // 2:]
    return jnp.concatenate([z1 * cos - z2 * sin, z1 * sin + z2 * cos], axis=-1)


def hgrn2_mixer(zq, zf, zi, zg, lower_bound, norm_w):
    f32 = jnp.float32
    dtype = zq.dtype
    f = lower_bound + (1.0 - lower_bound) * jax.nn.sigmoid(zf.astype(f32))
    log_f = jnp.log(jnp.maximum(f, 1e-30))
    q = jax.nn.silu(zq.astype(f32)) * (HG_DK ** -0.5)
    k = 1.0 - f
    v = zi.astype(f32)
    qc, kc, gc, vc = (to_chunks(split_heads(a, HG_HEADS)) for a in (q, k, log_f, v))
    b, h, nc, l, dk = qc.shape
    causal = jnp.tril(jnp.ones((CHUNK, CHUNK), dtype=bool))[:, :, None]

    def chunk_step(S, inp):
        q_, k_, g_, v_ = inp
        cum = jnp.cumsum(g_, axis=2)
        diff = cum[:, :, :, None, :] - cum[:, :, None, :, :]
        decay = jnp.exp(jnp.where(causal, diff, -jnp.inf))
        scores = jnp.einsum('bhtd,bhsd,bhtsd->bhts', q_, k_, decay)
        o = jnp.einsum('bhts,bhsv->bhtv', scores, v_) + jnp.einsum('bhtd,bhdv->bhtv', q_ * jnp.exp(cum), S)
        last = cum[:, :, -1:, :]
        S = jnp.exp(last[:, :, 0, :])[..., None] * S + jnp.einsum('bhsd,bhsv->bhdv', k_ * jnp.exp(last - cum), v_)
        return S, o

    S0 = jnp.zeros((b, h, dk, vc.shape[-1]), f32)
    _, o = lax.scan(chunk_step, S0, tuple(jnp.moveaxis(a, 2, 0) for a in (qc, kc, gc, vc)))
    o = from_chunks(jnp.moveaxis(o, 0, 2))
    o = head_rmsnorm(o) * norm_w.astype(f32) * jax.nn.silu(split_heads(zg.astype(f32), HG_HEADS))
    bb, t = o.shape[:2]
    return o.reshape(bb, t, HG_W).astype(dtype)


def retention_mixer(zq, zk, zv, zg, positions):
    f32 = jnp.float32
    dtype = zq.dtype
    q = rope(split_heads(zq.astype(f32), RET_HEADS), positions) * (RET_DK ** -0.5)
    k = rope(split_heads(zk.astype(f32), RET_HEADS), positions)
    v = split_heads(zv.astype(f32), RET_HEADS)
    qc, kc, vc = (to_chunks(a) for a in (q, k, v))
    log_gamma = jnp.log(1.0 - 2.0 ** (-5.0 - jnp.arange(RET_HEADS, dtype=f32)))
    j = jnp.arange(CHUNK, dtype=f32)
    rel = j[:, None] - j[None, :]
    causal = rel >= 0
    dmask = jnp.where(causal, jnp.exp(jnp.where(causal, rel, 0.0)[None] * log_gamma[:, None, None]), 0.0)
    scores = jnp.einsum('bhctd,bhcsd->bhcts', qc, kc) * dmask[None, :, None]
    inner = jnp.einsum('bhcts,bhcsv->bhctv', scores, vc)
    zeta = jnp.exp((CHUNK - 1.0 - j)[None, :] * log_gamma[:, None])
    kv = jnp.einsum('bhcsd,hs,bhcsv->cbhdv', kc, zeta, vc)
    chunk_decay = jnp.exp(CHUNK * log_gamma)[None, :, None, None]

    def step(R, kv_c):
        return chunk_decay * R + kv_c, R

    _, r_before = lax.scan(step, jnp.zeros(kv.shape[1:], f32), kv)
    xi = jnp.exp((j + 1.0)[None, :] * log_gamma[:, None])
    cross = jnp.einsum('bhctd,ht,cbhdv->bhctv', qc, xi, r_before)
    o = from_chunks(inner + cross)
    o = head_rmsnorm(o) * jax.nn.silu(split_heads(zg.astype(f32), RET_HEADS))
    bb, t = o.shape[:2]
    return o.reshape(bb, t, RET_W).astype(dtype)


def rwkv7_mixer(z, mu, w0, w2, a0, a2, g2, k_k, k_a, r_k, ln_w, ln_b):
    f32 = jnp.float32
    dtype = z.dtype
    z = z.astype(f32)
    z_prev = jnp.pad(z[:, :-1], ((0, 0), (1, 0), (0, 0)))
    z = z + mu * (z_prev - z)
    r, k, v, w_lo, a_lo, g_lo = split_cols(z, (RW_W, RW_W, RW_W, RW_DECAY_LORA, RW_A_LORA, RW_GATE_LORA))
    w = -jax.nn.softplus(-(w0 + jnp.tanh(w_lo) @ w2)) - 0.5
    decay = jnp.exp(-jnp.exp(w))
    a = jax.nn.sigmoid(a0 + a_lo @ a2)
    g = jax.nn.sigmoid(g_lo) @ g2
    kk = split_heads(k * k_k, RW_HEADS)
    kk = kk * lax.rsqrt(jnp.maximum(jnp.sum(kk * kk, axis=-1, keepdims=True), 1e-24))
    k = k * (1.0 + (a - 1.0) * k_a)
    rh, wh, kh, vh, ah = (split_heads(t, RW_HEADS) for t in (r, decay, k, v, a))

    def step(S, inp):
        r_, w_, k_, v_, kk_, a_ = inp
        sa = jnp.einsum('bhvk,bhk->bhv', S, -kk_)
        S = S * w_[:, :, None, :] + sa[..., None] * (kk_ * a_)[:, :, None, :] + v_[..., None] * k_[:, :, None, :]
        return S, jnp.einsum('bhvk,bhk->bhv', S, r_)

    bb, t = z.shape[:2]
    S0 = jnp.zeros((bb, RW_HEADS, RW_N, RW_N), f32)
    _, o = lax.scan(step, S0, tuple(jnp.moveaxis(a_, 1, 0) for a_ in (rh, wh, kh, vh, kk, ah)))
    o = jnp.moveaxis(o, 0, 1)
    mean = jnp.mean(o, axis=-1, keepdims=True)
    var = jnp.mean(jnp.square(o - mean), axis=-1, keepdims=True)
    o = ((o - mean) * lax.rsqrt(var + RW_GN_EPS)).reshape(bb, t, RW_W) * ln_w + ln_b
    bonus = (jnp.sum(rh * kh * r_k, axis=-1, keepdims=True) * vh).reshape(bb, t, RW_W)
    return ((o + bonus) * g).astype(dtype)


def token_mixer(h, positions, lower_bound, w_in, hg_norm_w, rw_mu, rw_w0, rw_w2, rw_a0, rw_a2, rw_g2,
                rw_k_k, rw_k_a, rw_r_k, rw_ln_w, rw_ln_b, br_hg, br_ret, br_rw, w_out):
    z = h @ w_in
    z_hg, z_ret, z_rw, z_gate = split_cols(z, (HG_COLS, RET_COLS, RW_COLS, GATE_COLS))
    y_hg = hgrn2_mixer(*split_cols(z_hg, (HG_W,) * 4), lower_bound, hg_norm_w) @ br_hg
    y_ret = retention_mixer(*split_cols(z_ret, (RET_W,) * 4), positions) @ br_ret
    y_rw = rwkv7_mixer(z_rw, rw_mu, rw_w0, rw_w2, rw_a0, rw_a2, rw_g2, rw_k_k, rw_k_a, rw_r_k,
                       rw_ln_w, rw_ln_b) @ br_rw
    gates = jax.nn.sigmoid(z_gate.astype(jnp.float32)).astype(h.dtype)
    g_hg, g_ret, g_rw = split_cols(gates, (D_MODEL,) * N_BRANCH)
    return (g_hg * y_hg + g_ret * y_ret + g_rw * y_rw) @ w_out


def hier_moe(h, router_g, router_e, w1, w3, w2):
    b, t, d = h.shape
    hf = h.reshape(b * t, d)
    g_logits = (hf @ router_g).astype(jnp.float32)
    g_idx = jnp.argmax(g_logits, axis=-1)
    g_w = jnp.take_along_axis(jax.nn.softmax(g_logits, axis=-1), g_idx[:, None], axis=-1)
    e_logits = (hf @ router_e).astype(jnp.float32).reshape(b * t, N_GROUPS, EXPERTS_PER_GROUP)
    e_logits = jnp.take_along_axis(e_logits, g_idx[:, None, None], axis=1)[:, 0]
    top_p, top_i = lax.top_k(jax.nn.softmax(e_logits, axis=-1), TOP_K_IN_GROUP)
    top_p = top_p / jnp.sum(top_p, axis=-1, keepdims=True)
    expert_id = g_idx[:, None] * EXPERTS_PER_GROUP + top_i
    combine = jnp.sum(jax.nn.one_hot(expert_id, N_EXPERTS, dtype=jnp.float32) * (g_w * top_p)[..., None], axis=1)
    combine = combine.astype(h.dtype).T

    def expert_step(acc, p):
        w1_e, w3_e, w2_e, c_e = p
        y = (jax.nn.silu(hf @ w1_e) * (hf @ w3_e)) @ w2_e
        return acc + c_e[:, None] * y, None

    y, _ = lax.scan(expert_step, jnp.zeros_like(hf), (w1, w3, w2, combine))
    return y.reshape(b, t, d)


def setup_inputs(seed: int = 0) -> dict:
    key = jax.random.key(seed)
    keys = iter(jax.random.split(key, 48))

    def nrm(shape, scale):
        return jax.random.normal(next(keys), shape, jnp.float32) * scale

    def unif(shape, lo, hi):
        return jax.random.uniform(next(keys), shape, jnp.float32, lo, hi)

    L = DEPTH
    x = nrm((BATCH, SEQ, D_MODEL), 1.0)
    c = nrm((BATCH, D_MODEL), 1.0)
    offsets = jax.random.randint(next(keys), (BATCH, 1), 0, SEQ, dtype=jnp.int32)
    positions = offsets + jnp.arange(SEQ, dtype=jnp.int32)[None, :]
    return {
        'x': x,
        'c': c,
        'positions': positions,
        'ada_w': nrm((L, D_MODEL, 6 * D_MODEL), 0.5 * D_MODEL ** -0.5),
        'ada_b': nrm((L, 6 * D_MODEL), 0.02),
        'norm1_g': 1.0 + nrm((L, D_MODEL), 0.05),
        'norm2_g': 1.0 + nrm((L, D_MODEL), 0.05),
        'w_in': nrm((L, D_MODEL, IN_COLS), D_MODEL ** -0.5),
        'hg_lb_table': nrm((L, HG_W), 0.5),
        'hg_norm_w': 1.0 + nrm((L, HG_DK), 0.05),
        'rw_mu': unif((L, RW_COLS), 0.0, 1.0),
        'rw_w0': unif((L, RW_W), -6.5, -1.5),
        'rw_w2': nrm((L, RW_DECAY_LORA, RW_W), 0.1 * RW_DECAY_LORA ** -0.5),
        'rw_a0': nrm((L, RW_W), 0.5),
        'rw_a2': nrm((L, RW_A_LORA, RW_W), 0.5 * RW_A_LORA ** -0.5),
        'rw_g2': nrm((L, RW_GATE_LORA, RW_W), RW_GATE_LORA ** -0.5),
        'rw_k_k': 0.85 + nrm((L, RW_W), 0.05),
        'rw_k_a': 1.0 + nrm((L, RW_W), 0.05),
        'rw_r_k': nrm((L, RW_HEADS, RW_N), 0.1),
        'rw_ln_w': 1.0 + nrm((L, RW_W), 0.05),
        'rw_ln_b': nrm((L, RW_W), 0.02),
        'br_hg': nrm((L, HG_W, D_MODEL), HG_W ** -0.5),
        'br_ret': nrm((L, RET_W, D_MODEL), RET_W ** -0.5),
        'br_rw': nrm((L, RW_W, D_MODEL), RW_W ** -0.5),
        'w_out': nrm((L, D_MODEL, D_MODEL), D_MODEL ** -0.5),
        'router_g': nrm((L, D_MODEL, N_GROUPS), D_MODEL ** -0.5),
        'router_e': nrm((L, D_MODEL, N_EXPERTS), D_MODEL ** -0.5),
        'moe_w1': nrm((L, N_EXPERTS, D_MODEL, D_EXPERT), D_MODEL ** -0.5),
        'moe_w3': nrm((L, N_EXPERTS, D_MODEL, D_EXPERT), D_MODEL ** -0.5),
        'moe_w2': nrm((L, N_EXPERTS, D_EXPERT, D_MODEL), D_EXPERT ** -0.5),
        'final_g': 1.0 + nrm((D_MODEL,), 0.05),
    }


def reference(x, c, positions, ada_w, ada_b, norm1_g, norm2_g, w_in, hg_lb_table, hg_norm_w, rw_mu, rw_w0,
              rw_w2, rw_a0, rw_a2, rw_g2, rw_k_k, rw_k_a, rw_r_k, rw_ln_w, rw_ln_b, br_hg, br_ret, br_rw, w_out,
              router_g, router_e, moe_w1, moe_w3, moe_w2, final_g):
    lb_p = jax.nn.softmax(hg_lb_table.astype(jnp.float32), axis=0)
    lower_bounds = jnp.cumsum(lb_p, axis=0) - lb_p[0]
    cond = jax.nn.silu(c)
    for l in range(DEPTH):
        mod = cond @ ada_w[l] + ada_b[l]
        shift1, scale1, gate1, shift2, scale2, gate2 = jnp.split(mod[:, None, :], 6, axis=-1)
        h = rmsnorm(x, norm1_g[l]) * (1.0 + scale1) + shift1
        y = token_mixer(h, positions, lower_bounds[l], w_in[l], hg_norm_w[l], rw_mu[l], rw_w0[l], rw_w2[l],
                        rw_a0[l], rw_a2[l], rw_g2[l], rw_k_k[l], rw_k_a[l], rw_r_k[l], rw_ln_w[l], rw_ln_b[l],
                        br_hg[l], br_ret[l], br_rw[l], w_out[l])
        x = x + gate1 * y
        h = rmsnorm(x, norm2_g[l]) * (1.0 + scale2) + shift2
        x = x + gate2 * hier_moe(h, router_g[l], router_e[l], moe_w1[l], moe_w3[l], moe_w2[l])
    return rmsnorm(x, final_g)
```

```python
import contextlib
import math
import numpy as np
import concourse.bass as bass
import concourse.mybir as mybir
from concourse.bass_utils import run_bass_kernel_spmd

F32 = mybir.dt.float32
BF16 = mybir.dt.bfloat16
I32 = mybir.dt.int32
AF = mybir.ActivationFunctionType
ALU = mybir.AluOpType
AX = mybir.AxisListType

T = 4096
D = 1024
NL = 2
NE = 32
DE = 512
IN_COLS = 8960
EPS = 1e-6
D1_SEQ = False
PI = math.pi


class Sched:
    def __init__(self, nc, stack):
        self.nc = nc
        self.stack = stack
        self.eng = {"pe": nc.tensor, "act": nc.scalar, "dve": nc.vector, "pool": nc.gpsimd, "sp": nc.sync}
        self.esem = {}
        self.ecount = {}
        for e in self.eng:
            self.esem[e] = stack.enter_context(nc.semaphore("s_" + e))
            self.ecount[e] = 0
        self.sems = dict((id(s), s) for s in self.esem.values())
        self.seen = {e: {} for e in self.eng}
        self.lastw = {}
        self.reads = {}
        self.dsem = {}
        self.dcount = {}
        self.n_inst = 0
        self.n_wait = 0
        self.psum_keys = set()

    @property
    def cur_counts(self):
        return {id(self.esem[e]): (lambda e=e: self.ecount[e]) for e in ("act", "dve", "pe")}

    def _dma_sem(self, key):
        if key not in self.dsem:
            s = self.stack.enter_context(self.nc.semaphore("d%d" % len(self.dsem)))
            self.dsem[key] = s
            self.dcount[key] = 0
            self.sems[id(s)] = s
        return self.dsem[key]

    def _deps(self, e, reads, writes):
        evs = []
        for k in reads:
            w = self.lastw.get(k)
            if w is not None:
                evs.append(w)
        for k in writes:
            w = self.lastw.get(k)
            if w is not None and not (e != "dma" and w[2] == e):
                evs.append(w)
            for r in self.reads.get(k, ()):
                if e != "dma" and r[2] == e:
                    continue
                evs.append(r)
        return evs

    def _wait(self, e, evs):
        seen = self.seen[e]
        best = {}
        for (sid, val, _src) in evs:
            if seen.get(sid, 0) >= val:
                continue
            if best.get(sid, 0) < val:
                best[sid] = val
        for sid, val in best.items():
            if getattr(self, "coarse", False) and sid in self.cur_counts:
                val = max(val, self.cur_counts[sid]())
            self.eng[e].wait_ge(self.sems[sid], val)
            seen[sid] = val
            self.n_wait += 1

    def _record(self, ev, reads, writes):
        for k in reads:
            lst = self.reads.setdefault(k, [])
            lst[:] = [r for r in lst if r[0] != ev[0]]
            lst.append(ev)
        for k in writes:
            self.lastw[k] = ev
            self.reads[k] = []

    def op(self, e, fn, reads=(), writes=()):
        px = [k for k in reads if k in self.psum_keys and k not in writes]
        if px:
            writes = list(writes) + px
        evs = self._deps(e, reads, writes)
        if e == "pe":
            evs = [x for x in evs if x[2] != "pe"]
        self._wait(e, evs)
        inst = fn(self.eng[e])
        self.ecount[e] += 1
        inst.then_inc(self.esem[e], 1)
        ev = (id(self.esem[e]), self.ecount[e], e)
        self._record(ev, reads, writes)
        self.n_inst += 1
        return inst

    def dma(self, q, out, in_, reads=(), writes=(), **kw):
        evs = self._deps("dma", reads, writes)
        self._wait(q, evs)
        key = writes[0]
        s = self._dma_sem(key)
        inst = self.eng[q].dma_start(out=out, in_=in_, **kw)
        self.dcount[key] += 16
        inst.then_inc(s, 16)
        ev = (id(s), self.dcount[key], "dma")
        self._record(ev, reads, writes)
        self.n_inst += 1
        return inst

    def barrier(self):
        evs = [(id(self.esem[e]), self.ecount[e], e) for e in self.eng if self.ecount[e] > 0]
        evs += [(id(self.dsem[k]), self.dcount[k], "dma") for k in self.dsem if self.dcount[k] > 0]
        for e in self.eng:
            self._wait(e, [x for x in evs if x[2] != e or e == "dma"])
        self.nbar = getattr(self, "nbar", 0) + 1
        for e in self.eng:
            s_ = self.stack.enter_context(self.nc.semaphore("s_%s_%d" % (e, self.nbar)))
            self.esem[e] = s_
            self.sems[id(s_)] = s_
            self.ecount[e] = 0
        self.lastw = {}
        self.reads = {}


class Ctx:
    pass


_UID = [0]


def _un(name):
    _UID[0] += 1
    return "%s_u%d" % (name, _UID[0])


def _consts(S, nc, st, g):
    sb = lambda name, shape, dt=F32: st.enter_context(nc.sbuf_tensor(_un(name), list(shape), dt))
    g.ident = sb("ident", [128, 128])
    g.identb = sb("identb", [128, 128], BF16)
    g.ones = sb("ones", [128, 128])
    S.op("pool", lambda e: e.memset(g.ones[:], 1.0), writes=["ones"])
    S.op("pool", lambda e: e.memset(g.ident[:], 1.0), writes=["ident"])
    S.op("pool", lambda e: e.affine_select(out=g.ident[:], in_=g.ident[:], pattern=[[1, 128]],
                                           compare_op=ALU.is_equal, fill=0.0, base=0, channel_multiplier=-1),
         reads=["ident"], writes=["ident"])
    S.op("dve", lambda e: e.tensor_copy(out=g.identb[:], in_=g.ident[:]), reads=["ident"], writes=["identb"])
    g.eps = sb("epsc", [128, 1])
    S.op("pool", lambda e: e.memset(g.eps[:], EPS), writes=["epsc"])


def _rms_rstd(S, g, xt, xkey, junk, jkey, ss, rstd, tag):
    S.op("act", lambda e: e.activation(out=junk, in_=xt, func=AF.Square, accum_out=ss),
         reads=[xkey], writes=[jkey, tag + "ss"])
    S.op("dve", lambda e: e.tensor_scalar(out=ss, in0=ss, scalar1=1.0 / D, scalar2=EPS, op0=ALU.mult, op1=ALU.add),
         reads=[tag + "ss"], writes=[tag + "ss"])
    S.op("act", lambda e: e.activation(out=ss, in_=ss, func=AF.Sqrt), reads=[tag + "ss"], writes=[tag + "ss"])
    S.op("dve", lambda e: e.reciprocal(out=rstd, in_=ss), reads=[tag + "ss"], writes=[tag + "rstd"])


def _phase0(S, nc, g, io):
    with contextlib.ExitStack() as st:
        sb = lambda name, shape, dt=F32: st.enter_context(nc.sbuf_tensor(_un(name), list(shape), dt))
        cc = sb("p0_c", [128, 8])
        aw = [sb("p0_aw%d" % i, [128, 8, 512]) for i in range(2)]
        ab = sb("p0_ab", [1, 6 * D])
        row = sb("p0_row", [1, 6 * D])
        ps = [st.enter_context(nc.psum_tensor(_un("p0_ps%d" % i), [128, 512], F32)) for i in range(2)]
        S.psum_keys.update(["p0_ps0", "p0_ps1"])
        S.dma("sp", cc[:], io.c.rearrange("o (k p) -> p (o k)", p=128), writes=["p0_c"], allow_slow_non_contiguous=True)
        S.op("act", lambda e: e.activation(out=cc[:], in_=cc[:], func=AF.Silu), reads=["p0_c"], writes=["p0_c"])
        for l in range(NL):
            S.dma("sp", ab[:], io.ada_b[l:l + 1, :], writes=["p0_ab"])
            for nb in range(12):
                b = nb % 2
                S.dma("sp", aw[b][:], io.ada_w[l, :, nb * 512:(nb + 1) * 512].rearrange("(k p) n -> p k n", p=128),
                      writes=["p0_aw%d" % b])
                for k in range(8):
                    S.op("pe", lambda e, k=k, b=b: e.matmul(ps[b][0:1, :], lhsT=cc[:, k:k + 1], rhs=aw[b][:, k, :],
                                                           start=(k == 0), stop=(k == 7)),
                         reads=["p0_c", "p0_aw%d" % b], writes=["p0_ps%d" % b])
                S.op("dve", lambda e, b=b, nb=nb: e.tensor_tensor(out=row[0:1, nb * 512:(nb + 1) * 512], in0=ps[b][0:1, :],
                                                                 in1=ab[0:1, nb * 512:(nb + 1) * 512], op=ALU.add),
                     reads=["p0_ps%d" % b, "p0_ab"], writes=["p0_row"])
            S.dma("sp", io.modrow[l:l + 1, :], row[:], reads=["p0_row"], writes=["modrow%d" % l])
    S.barrier()


def _load_mod(S, nc, g, io, l, which, gain_ap, A, Akey, B, Bkey, G, Gkey, tmp, tkey):
    o = 0 if which == 1 else 3
    mr = io.modrow
    S.dma("sp", B, mr[l:l + 1, (o + 0) * D:(o + 1) * D].to_broadcast([128, D]), reads=["modrow%d" % l], writes=[Bkey])
    S.dma("sp", A, mr[l:l + 1, (o + 1) * D:(o + 2) * D].to_broadcast([128, D]), reads=["modrow%d" % l], writes=[Akey])
    if G is not None:
        S.dma("sp", G, mr[l:l + 1, (o + 2) * D:(o + 3) * D].to_broadcast([128, D]), reads=["modrow%d" % l], writes=[Gkey])
    S.dma("sp", tmp, gain_ap.to_broadcast([128, D]), writes=[tkey])
    S.op("dve", lambda e: e.scalar_tensor_tensor(out=A, in0=A, scalar=1.0, in1=tmp, op0=ALU.add, op1=ALU.mult),
         reads=[Akey, tkey], writes=[Akey])


def _moe_phase(S, nc, g, io, l, x_src, xs_key, x_dst, xd_key, final, pre=None):
    NPASS = 2
    TP = T // NPASS
    NT = TP // 128
    with contextlib.ExitStack() as st:
        sb = lambda name, shape, dt=F32: st.enter_context(nc.sbuf_tensor(_un(name), list(shape), dt))
        pst = lambda name, shape, dt=F32: (S.psum_keys.add(name), st.enter_context(nc.psum_tensor(_un(name), list(shape), dt)))[1]
        A2 = sb("m_A2", [128, D]); B2 = sb("m_B2", [128, D]); G2 = sb("m_G2", [128, D])
        FG = sb("m_FG", [128, D])
        h2T = sb("m_h2T", [128, 8, TP], BF16)
        yacc = sb("m_yacc", [128, NT, D])
        combT = sb("m_combT", [32, TP])
        Wr = sb("m_Wr", [128, 8, 36])
        xt = [sb("m_xt%d" % i, [128, D]) for i in range(2)]
        hfs = [sb("m_hf%d" % i, [128, D]) for i in range(2)]
        hTfs = [sb("m_hTf%d" % i, [128, 8, 128]) for i in range(2)]
        smalls = [sb("m_small%d" % i, [128, 160]) for i in range(2)]
        sss = [sb("m%d_ss" % i, [128, 1]) for i in range(2)]; rstds = [sb("m%d_rstd" % i, [128, 1]) for i in range(2)]
        hf = hfs[0]; ss = sss[0]; rstd = rstds[0]
        w1 = [sb("m_w1_%d" % i, [128, 8, DE], BF16) for i in range(2)]
        w3 = [sb("m_w3_%d" % i, [128, 8, DE], BF16) for i in range(2)]
        w2 = [sb("m_w2_%d" % i, [128, 4, D], BF16) for i in range(2)]
        sil = [sb("m_sil%d" % i, [128, 512], BF16) for i in range(2)]
        tmp = [sb("m_tmp%d" % i, [128, 512], BF16) for i in range(2)]
        actT = [sb("m_act%d" % i, [128, 4, 512], BF16) for i in range(2)]
        p_h1 = [pst("m_ph1_%d" % i, [128, 512]) for i in range(2)]
        p_h3 = [pst("m_ph3_%d" % i, [128, 512]) for i in range(2)]
        p_cb = pst("m_pcb", [128, 512])
        p_y = [pst("m_py%d" % i, [128, 512]) for i in range(2)]
        p_tb = pst("m_ptb", [128, 8, 128], BF16)
        p_cbs = [(p_cb[:], "m_pcb"), (p_tb[:].rearrange("p a b -> p (a b)").bitcast(F32), "m_ptb")]

        _load_mod(S, nc, g, io, l, 2, io.norm2_g[l:l + 1, :], A2[:], "m_A2", B2[:], "m_B2", G2[:], "m_G2",
                  hf[:], "m_hf0")
        if final:
            S.dma("sp", FG[:], io.final_g.to_broadcast([128, D]), writes=["m_FG"])
        S.dma("sp", Wr[:, :, 0:4], io.router_g[l].rearrange("(k p) n -> p k n", p=128), writes=["m_Wr"])
        S.dma("sp", Wr[:, :, 4:36], io.router_e[l].rearrange("(k p) n -> p k n", p=128), writes=["m_Wr"])

        def load_expert(e):
            b = e % 2
            S.dma("pool", w1[b][:], io.moe_w1[l, e].rearrange("(k p) n -> p k n", p=128), writes=["m_w1_%d" % b])
            S.dma("pool", w3[b][:], io.moe_w3[l, e].rearrange("(k p) n -> p k n", p=128), writes=["m_w3_%d" % b])
            S.dma("pool", w2[b][:], io.moe_w2[l, e].rearrange("(k p) n -> p k n", p=128), writes=["m_w2_%d" % b])

        pre = list(pre) if pre is not None else []
        for ps_ in range(NPASS):
            t0 = ps_ * TP
            def d1_tile(i, par):
                xb = xt[par]; xk = "m_xt%d" % par
                hf_ = hfs[par]; hfk = "m_hf%d" % par
                hTf_ = hTfs[par]; hTk = "m_hTf%d" % par
                small = smalls[par]; K = "m_small%d" % par
                ss_ = sss[par]; rstd_ = rstds[par]; tg = "m%d_" % par
                py = p_y[par]; pyk = "m_py%d" % par
                plg, plk = (p_cb, "m_pcb") if par == 0 else (p_h1[0], "m_ph1_0")
                S.dma("sp", xb[:], x_src[t0 + i * 128:t0 + (i + 1) * 128, :], reads=[xs_key], writes=[xk])
                _rms_rstd(S, g, xb[:], xk, hf_[:], hfk, ss_[:], rstd_[:], tg)
                yield
                S.op("dve", lambda e: e.scalar_tensor_tensor(out=hf_[:], in0=xb[:], scalar=rstd_[:], in1=A2[:],
                                                             op0=ALU.mult, op1=ALU.mult),
                     reads=[xk, tg + "rstd", "m_A2"], writes=[hfk])
                S.op("dve", lambda e: e.tensor_tensor(out=hf_[:], in0=hf_[:], in1=B2[:], op=ALU.add),
                     reads=[hfk, "m_B2"], writes=[hfk])
                yield
                for hh in range(2):
                    for k in range(4):
                        kk = hh * 4 + k
                        S.op("pe", lambda e, k=k, kk=kk: e.transpose(py[:, k * 128:(k + 1) * 128],
                                                                     hf_[:, kk * 128:(kk + 1) * 128], g.ident[:]),
                             reads=[hfk, "ident"], writes=[pyk])
                    S.op("dve", lambda e, hh=hh: e.tensor_copy(out=hTf_[:, hh * 4:(hh + 1) * 4, :], in_=py[:]),
                         reads=[pyk], writes=[hTk])
                    yield
                S.op("act", lambda e: e.copy(out=h2T[:, :, i * 128:(i + 1) * 128], in_=hTf_[:]),
                     reads=[hTk], writes=["m_h2T"])
                for k in range(8):
                    S.op("pe", lambda e, k=k: e.matmul(plg[:, 0:36], lhsT=hTf_[:, k, :], rhs=Wr[:, k, :],
                                                       start=(k == 0), stop=(k == 7)),
                         reads=[hTk, "m_Wr"], writes=[plk])
                yield
                Lg = small[:, 0:36]
                gmax = small[:, 36:37]; oh = small[:, 40:44]; eg = small[:, 44:48]; sumg = small[:, 48:49]
                gw = small[:, 49:50]; esel = small[:, 52:60]; top8 = small[:, 60:68]; negm1 = small[:, 68:69]
                p2 = small[:, 69:70]; den = small[:, 70:71]; w1g = small[:, 71:72]; w2g = small[:, 72:73]
                c1 = small[:, 76:84]; c2 = small[:, 84:92]; comb = small[:, 96:128]; ngmax = small[:, 37:38]
                vop = lambda fn: S.op("dve", fn, reads=[K], writes=[K])
                S.op("dve", lambda e: e.tensor_copy(out=Lg, in_=plg[:, 0:36]), reads=[plk], writes=[K])
                vop(lambda e: e.tensor_reduce(out=gmax, in_=small[:, 0:4], axis=AX.X, op=ALU.max))
                vop(lambda e: e.tensor_scalar(out=oh, in0=small[:, 0:4], scalar1=gmax, scalar2=None, op0=ALU.is_equal))
                vop(lambda e: e.tensor_scalar(out=ngmax, in0=gmax, scalar1=-1.0, scalar2=None, op0=ALU.mult))
                yield
                S.op("act", lambda e: e.activation(out=eg, in_=small[:, 0:4], func=AF.Exp, bias=ngmax, accum_out=sumg),
                     reads=[K], writes=[K])
                vop(lambda e: e.reciprocal(out=gw, in_=sumg))
                vop(lambda e: e.tensor_scalar(out=esel, in0=small[:, 4:12], scalar1=small[:, 40:41], scalar2=None,
                                              op0=ALU.mult))
                yield
                for gi in range(1, 4):
                    vop(lambda e, gi=gi: e.scalar_tensor_tensor(out=esel, in0=small[:, 4 + 8 * gi:12 + 8 * gi],
                                                               scalar=small[:, 40 + gi:41 + gi], in1=esel,
                                                               op0=ALU.mult, op1=ALU.add))
                    yield
                vop(lambda e: e.max(out=top8, in_=esel))
                vop(lambda e: e.tensor_scalar(out=negm1, in0=small[:, 60:61], scalar1=-1.0, scalar2=None, op0=ALU.mult))
                yield
                S.op("act", lambda e: e.activation(out=p2, in_=small[:, 61:62], func=AF.Exp, bias=negm1),
                     reads=[K], writes=[K])
                vop(lambda e: e.tensor_scalar(out=den, in0=p2, scalar1=1.0, scalar2=None, op0=ALU.add))
                yield
                vop(lambda e: e.reciprocal(out=den, in_=den))
                yield
                vop(lambda e: e.tensor_tensor(out=w1g, in0=den, in1=gw, op=ALU.mult))
                yield
                vop(lambda e: e.tensor_tensor(out=w2g, in0=w1g, in1=p2, op=ALU.mult))
                vop(lambda e: e.tensor_scalar(out=c1, in0=esel, scalar1=small[:, 60:61], scalar2=w1g,
                                              op0=ALU.is_equal, op1=ALU.mult))
                yield
                vop(lambda e: e.tensor_scalar(out=c2, in0=esel, scalar1=small[:, 61:62], scalar2=w2g,
                                              op0=ALU.is_equal, op1=ALU.mult))
                yield
                vop(lambda e: e.tensor_tensor(out=c1, in0=c1, in1=c2, op=ALU.add))
                yield
                for gi in range(4):
                    vop(lambda e, gi=gi: e.tensor_scalar(out=small[:, 96 + 8 * gi:104 + 8 * gi], in0=c1,
                                                        scalar1=small[:, 40 + gi:41 + gi], scalar2=None, op0=ALU.mult))
                yield
                S.op("pe", lambda e: e.transpose(plg[0:32, 128:256], comb, g.ident[:]),
                     reads=[K, "ident"], writes=[plk])
                S.op("dve", lambda e: e.tensor_copy(out=combT[:, i * 128:(i + 1) * 128], in_=plg[0:32, 128:256]),
                     reads=[plk], writes=["m_combT"])

            for i in range(0, NT, 2):
                gens = [d1_tile(i, 0), d1_tile(i + 1, 1)]
                if D1_SEQ:
                    for gn in gens:
                        for _ in gn:
                            pass
                    gens = []
                while gens:
                    for gn in list(gens):
                        try:
                            next(gn)
                        except StopIteration:
                            gens.remove(gn)
            units = [(ex, blk) for ex in range(NE) for blk in range(TP // 512)]

            def front(ex, blk, it, fcs=range(4)):
                b = ex % 2; c0 = blk * 512; ab = it % 2
                pcb, pck = p_cbs[it % 2]
                wk = ["m_w1_%d" % b, "m_w3_%d" % b]
                if 0 in fcs:
                    S.op("pe", lambda e: e.matmul(pcb, lhsT=g.ident[0:32, ex:ex + 1].to_broadcast([32, 128]),
                                                  rhs=combT[:, c0:c0 + 512], start=True, stop=True),
                         reads=["ident", "m_combT"], writes=[pck])
                for fc in fcs:
                    pb = fc % 2
                    for k in range(8):
                        S.op("pe", lambda e, k=k, fc=fc, pb=pb: e.matmul(
                            p_h1[pb][:], lhsT=w1[b][:, k, fc * 128:(fc + 1) * 128], rhs=h2T[:, k, c0:c0 + 512],
                            start=(k == 0), stop=(k == 7)), reads=[wk[0], "m_h2T"], writes=["m_ph1_%d" % pb])
                    for k in range(8):
                        S.op("pe", lambda e, k=k, fc=fc, pb=pb: e.matmul(
                            p_h3[pb][:], lhsT=w3[b][:, k, fc * 128:(fc + 1) * 128], rhs=h2T[:, k, c0:c0 + 512],
                            start=(k == 0), stop=(k == 7)), reads=[wk[1], "m_h2T"], writes=["m_ph3_%d" % pb])
                    S.op("act", lambda e, pb=pb: e.activation(out=sil[pb][:], in_=p_h1[pb][:], func=AF.Silu),
                         reads=["m_ph1_%d" % pb], writes=["m_sil%d" % pb])
                    S.op("dve", lambda e, pb=pb: e.tensor_tensor(out=tmp[pb][:], in0=sil[pb][:], in1=p_h3[pb][:], op=ALU.mult),
                         reads=["m_sil%d" % pb, "m_ph3_%d" % pb], writes=["m_tmp%d" % pb])
                    S.op("dve", lambda e, pb=pb, fc=fc: e.tensor_tensor(out=actT[ab][:, fc, :], in0=tmp[pb][:], in1=pcb, op=ALU.mult),
                         reads=["m_tmp%d" % pb, pck], writes=["m_act%d" % ab])

            def back(ex, blk, it, tls=range(4)):
                b = ex % 2; ab = it % 2
                for tl in tls:
                    ti = blk * 4 + tl
                    for hh in range(2):
                        for fc in range(4):
                            S.op("pe", lambda e, fc=fc, tl=tl, hh=hh: e.matmul(
                                p_y[hh][:], lhsT=actT[ab][:, fc, tl * 128:(tl + 1) * 128],
                                rhs=w2[b][:, fc, hh * 512:(hh + 1) * 512], start=(fc == 0), stop=(fc == 3)),
                                 reads=["m_act%d" % ab, "m_w2_%d" % b], writes=["m_py%d" % hh])
                        ya = yacc[:, ti, hh * 512:(hh + 1) * 512]
                        yk = "m_yacc%d" % ti
                        if ex == 0:
                            S.op("dve", lambda e, ya=ya, hh=hh: e.tensor_copy(out=ya, in_=p_y[hh][:]),
                                 reads=["m_py%d" % hh], writes=[yk])
                        else:
                            S.op("dve", lambda e, ya=ya, hh=hh: e.tensor_tensor(out=ya, in0=ya, in1=p_y[hh][:], op=ALU.add),
                                 reads=["m_py%d" % hh, yk], writes=[yk])

            load_expert(0)
            load_expert(1)
            for it, (ex, blk) in enumerate(units):
                for st_ in range(4):
                    front(ex, blk, it, fcs=[st_])
                    if it > 0:
                        back(units[it - 1][0], units[it - 1][1], it - 1, tls=[st_])
                if it > 0:
                    pex, pblk = units[it - 1]
                    if pblk == TP // 512 - 1 and pex + 2 < NE:
                        load_expert(pex + 2)
                        if pre:
                            pre.pop(0)()
            back(units[-1][0], units[-1][1], len(units) - 1)
            while pre:
                pre.pop(0)()
            S.coarse = False
            for i in range(NT):
                xb = xt[i % 2]; xk = "m_xt%d" % (i % 2)
                S.dma("sp", xb[:], x_src[t0 + i * 128:t0 + (i + 1) * 128, :], reads=[xs_key], writes=[xk])
                S.op("dve", lambda e, i=i: e.tensor_tensor(out=hf[:], in0=yacc[:, i, :], in1=G2[:], op=ALU.mult),
                     reads=["m_yacc%d" % i, "m_G2"], writes=["m_hf0"])
                S.op("dve", lambda e, xb=xb: e.tensor_tensor(out=xb[:], in0=xb[:], in1=hf[:], op=ALU.add),
                     reads=["m_hf0", xk], writes=[xk])
                if final:
                    _rms_rstd(S, g, xb[:], xk, hf[:], "m_hf0", ss[:], rstd[:], "m0_")
                    S.op("dve", lambda e, xb=xb: e.scalar_tensor_tensor(out=xb[:], in0=xb[:], scalar=rstd[:], in1=FG[:],
                                                                        op0=ALU.mult, op1=ALU.mult),
                         reads=[xk, "m0_rstd", "m_FG"], writes=[xk])
                S.dma("sp", x_dst[t0 + i * 128:t0 + (i + 1) * 128, :], xb[:], reads=[xk], writes=[xd_key])
    S.barrier()


def build(mixer=True, nlayers=NL, moe=True, nblocks=8, dbg_blk=None):
    nc = bass.Bass("TRN2", target_bir_lowering=False)
    io = Ctx()
    din = lambda name, shape, dt=F32: nc.dram_tensor(name, list(shape), dt, kind="ExternalInput").ap()
    io.x = din("x", [T, D]); io.c = din("c", [1, D]); io.pos = din("positions", [1, T], I32)
    io.ada_w = din("ada_w", [NL, D, 6 * D]); io.ada_b = din("ada_b", [NL, 6 * D])
    io.norm1_g = din("norm1_g", [NL, D]); io.norm2_g = din("norm2_g", [NL, D])
    io.w_in = din("w_in", [NL, D, IN_COLS])
    io.hg_lb_table = din("hg_lb_table", [NL, 512]); io.hg_norm_w = din("hg_norm_w", [NL, 128])
    io.rw_mu = din("rw_mu", [NL, 1792]); io.rw_w0 = din("rw_w0", [NL, 512]); io.rw_w2 = din("rw_w2", [NL, 64, 512])
    io.rw_a0 = din("rw_a0", [NL, 512]); io.rw_a2 = din("rw_a2", [NL, 64, 512]); io.rw_g2 = din("rw_g2", [NL, 128, 512])
    io.rw_k_k = din("rw_k_k", [NL, 512]); io.rw_k_a = din("rw_k_a", [NL, 512]); io.rw_r_k = din("rw_r_k", [NL, 512])
    io.rw_ln_w = din("rw_ln_w", [NL, 512]); io.rw_ln_b = din("rw_ln_b", [NL, 512])
    io.br_hg = din("br_hg", [NL, 512, D]); io.br_ret = din("br_ret", [NL, 512, D]); io.br_rw = din("br_rw", [NL, 512, D])
    io.w_out = din("w_out", [NL, D, D])
    io.router_g = din("router_g", [NL, D, 4]); io.router_e = din("router_e", [NL, D, 32])
    io.moe_w1 = din("moe_w1", [NL, NE, D, DE]); io.moe_w3 = din("moe_w3", [NL, NE, D, DE])
    io.moe_w2 = din("moe_w2", [NL, NE, DE, D])
    io.final_g = din("final_g", [1, D])
    io.out = nc.dram_tensor("out", [T, D], F32, kind="ExternalOutput").ap()
    io.modrow = nc.dram_tensor("modrow", [NL, 6 * D], F32, kind="Internal").ap()
    io.xa = nc.dram_tensor("xa", [T, D], F32, kind="Internal").ap()
    io.xb = nc.dram_tensor("xb", [T, D], F32, kind="Internal").ap()
    io.wbf = nc.dram_tensor("wbf", [NL, 23, 128, 4096], BF16, kind="Internal").ap()
    g = Ctx()
    with contextlib.ExitStack() as st:
        S = Sched(nc, st)
        _consts(S, nc, st, g)
        dbg = None
        if dbg_blk is not None:
            dout = nc.dram_tensor("dbg_y", [3, 128, 4, 512], F32, kind="ExternalOutput").ap()
            dbg = (dout, dbg_blk)
        if mixer:
            for f_ in _precast_mixer_weights(S, io, 0):
                f_()
        _phase0(S, nc, g, io)
        cur, ck = io.x, "x_in"
        for l in range(nlayers):
            last = (l == nlayers - 1)
            if mixer:
                mdst, mk = (io.out, "out") if (last and not moe) else (io.xa, "xa")
                _mixer_phase(S, nc, g, io, l, cur, ck, mdst, mk, nblocks=nblocks, dbg=dbg if l == 0 else None)
                cur, ck = mdst, mk
            if moe:
                dst, dk = (io.out, "out") if last else (io.xb, "xb")
                pre = _precast_mixer_weights(S, io, l + 1) if (mixer and not last) else None
                _moe_phase(S, nc, g, io, l, cur, ck, dst, dk, final=last, pre=pre)
                cur, ck = dst, dk
        S.barrier()
        print("instructions", S.n_inst, "waits", S.n_wait, "dma sems", len(S.dsem), "barriers", getattr(S, "nbar", 0))
    return nc


def _mixer_consts(S, nc, st, g):
    sb = lambda name, shape, dt=F32: st.enter_context(nc.sbuf_tensor(_un(name), list(shape), dt))
    g.blk = sb("c_blk", [128, 128])
    S.op("pool", lambda e: e.memset(g.blk[:], 0.0), writes=["c_blk"])
    S.op("pool", lambda e: e.memset(g.blk[0:64, 0:64], 1.0), reads=["c_blk"], writes=["c_blk"])
    S.op("pool", lambda e: e.memset(g.blk[64:128, 64:128], 1.0), reads=["c_blk"], writes=["c_blk"])
    g.mge = sb("c_mge", [128, 64])
    g.m4 = sb("c_m4", [128, 256])
    S.op("pool", lambda e: e.memset(g.mge[:], 1.0), writes=["c_mge"])
    S.op("pool", lambda e: e.memset(g.m4[:], 1.0), writes=["c_m4"])
    for hh in range(2):
        rows = slice(hh * 64, hh * 64 + 64)
        S.op("pool", lambda e, rows=rows: e.affine_select(out=g.mge[rows, :], in_=g.mge[rows, :], pattern=[[1, 64]],
                                                          compare_op=ALU.is_ge, fill=0.0, base=0, channel_multiplier=-1),
             reads=["c_mge"], writes=["c_mge"])
        for q in range(4):
            op = ALU.is_gt if q % 2 == 0 else ALU.is_ge
            S.op("pool", lambda e, rows=rows, q=q, op=op: e.affine_select(
                out=g.m4[rows, q * 64:(q + 1) * 64], in_=g.m4[rows, q * 64:(q + 1) * 64], pattern=[[1, 64]],
                compare_op=op, fill=0.0, base=0, channel_multiplier=-1), reads=["c_m4"], writes=["c_m4"])
    g.mgei = sb("c_mgei", [128, 64], I32)
    S.op("dve", lambda e: e.tensor_copy(out=g.mgei[:], in_=g.mge[:]), reads=["c_mge"], writes=["c_mgei"])
    g.rm = sb("c_rm", [128, 512])
    S.op("pool", lambda e: e.memset(g.rm[:], 1.0), writes=["c_rm"])
    S.op("pool", lambda e: e.memset(g.rm[:].rearrange("p (c j) -> p c j", j=64)[:, :, 0:1], 0.0),
         reads=["c_rm"], writes=["c_rm"])
    g.J = sb("c_J", [128, 128]); g.Ji = sb("c_Ji", [128, 128], I32)
    g.DF = sb("c_DF", [128, 128])
    S.op("pool", lambda e: e.iota(g.Ji[:], pattern=[[1, 128]], base=0, channel_multiplier=0), writes=["c_Ji"])
    S.op("dve", lambda e: e.tensor_copy(out=g.J[:], in_=g.Ji[:]), reads=["c_Ji"], writes=["c_J"])
    S.op("pool", lambda e: e.iota(g.Ji[:], pattern=[[1, 128]], base=0, channel_multiplier=-1), reads=["c_Ji"], writes=["c_Ji"])
    S.op("dve", lambda e: e.tensor_copy(out=g.DF[:], in_=g.Ji[:]), reads=["c_Ji"], writes=["c_DF"])
    g.XI = sb("c_XI", [128, 4, 128]); g.ZE = sb("c_ZE", [128, 4, 128]); g.DM = sb("c_DM", [128, 4, 128])
    g.lng = [math.log(1.0 - 2.0 ** (-5.0 - h)) for h in range(4)]
    g.cb = sb("c_cb", [128, 16])
    for h in range(4):
        lg = g.lng[h]
        S.op("pool", lambda e, h=h, lg=lg: e.memset(g.cb[:, h:h + 1], lg), reads=["c_cb"], writes=["c_cb"])
        S.op("pool", lambda e, h=h, lg=lg: e.memset(g.cb[:, 4 + h:5 + h], 127.0 * lg), reads=["c_cb"], writes=["c_cb"])
    for h in range(4):
        lg = g.lng[h]
        S.op("act", lambda e, h=h, lg=lg: e.activation(out=g.XI[:, h, :], in_=g.J[:], func=AF.Exp, scale=lg, bias=g.cb[:, h:h + 1]),
             reads=["c_J", "c_cb"], writes=["c_XI"])
        S.op("act", lambda e, h=h, lg=lg: e.activation(out=g.ZE[:, h, :], in_=g.J[:], func=AF.Exp, scale=-lg, bias=g.cb[:, 4 + h:5 + h]),
             reads=["c_J", "c_cb"], writes=["c_ZE"])
        S.op("act", lambda e, h=h, lg=lg: e.activation(out=g.DM[:, h, :], in_=g.DF[:], func=AF.Exp, scale=lg),
             reads=["c_DF"], writes=["c_DM"])
        S.op("pool", lambda e, h=h: e.affine_select(out=g.DM[:, h, :], in_=g.DM[:, h, :], pattern=[[1, 128]],
                                                    compare_op=ALU.is_ge, fill=0.0, base=0, channel_multiplier=-1),
             reads=["c_DM"], writes=["c_DM"])
    g.invf = sb("c_invf", [128, 1]); g.pm = sb("c_pm", [128, 1], I32); g.sgn = sb("c_sgn", [128, 1])
    for hh in range(2):
        rows = slice(hh * 64, hh * 64 + 64)
        S.op("pool", lambda e, rows=rows: e.iota(g.pm[rows, :], pattern=[[0, 1]], base=0, channel_multiplier=1),
             reads=["c_pm"], writes=["c_pm"])
    S.op("dve", lambda e: e.tensor_copy(out=g.invf[:], in_=g.pm[:]), reads=["c_pm"], writes=["c_invf"])
    S.op("act", lambda e: e.activation(out=g.invf[:], in_=g.invf[:], func=AF.Exp, scale=-math.log(10000.0) / 64.0),
         reads=["c_invf"], writes=["c_invf"])
    S.op("pool", lambda e: e.memset(g.sgn[0:64, :], -1.0), reads=["c_sgn"], writes=["c_sgn"])
    S.op("pool", lambda e: e.memset(g.sgn[64:128, :], 1.0), reads=["c_sgn"], writes=["c_sgn"])


def _mixer_wlist(io, l):
    wsrc = lambda c0, n=512: (io.w_in[l, :, c0:c0 + n], 8, n)
    wlist = [wsrc(0), wsrc(512), wsrc(1024), wsrc(1536), wsrc(2048), wsrc(2560), wsrc(3072), wsrc(3584),
             wsrc(4096 + 1536, 256), wsrc(4096), wsrc(4608), wsrc(5120)]
    for b_, brn_ in enumerate((io.br_hg, io.br_ret, io.br_rw)):
        wlist += [(brn_[l], 4, 1024), wsrc(5888 + b_ * 1024), wsrc(5888 + b_ * 1024 + 512)]
    wlist += [(io.w_out[l, :, 0:512], 8, 512), (io.w_out[l, :, 512:1024], 8, 512)]
    return wlist


def _precast_mixer_weights(S, io, l):
    def mk(j, src, kp, ncols):
        def f():
            dst = io.wbf[l, j, :, 0:kp * ncols].rearrange("p (k n) -> p k n", k=kp)
            S.dma("pool", dst, src.rearrange("(k p) n -> p k n", p=128), writes=["wbf%d" % l])
        return f
    return [mk(j, src, kp, ncols) for j, (src, kp, ncols) in enumerate(_mixer_wlist(io, l))]


def _mixer_phase(S, nc, g, io, l, x_src, xs_key, x_dst, xd_key, nblocks=8, dbg=None):
    BT = 512
    E05 = math.exp(-0.5)
    with contextlib.ExitStack() as st:
        sb = lambda name, shape, dt=F32: st.enter_context(nc.sbuf_tensor(_un(name), list(shape), dt))
        pst_ = lambda name, shape, dt=F32: (S.psum_keys.add(name), st.enter_context(nc.psum_tensor(_un(name), list(shape), dt)))[1]
        V = lambda fn, r, w: S.op("dve", fn, reads=r, writes=w)
        A = lambda fn, r, w: S.op("act", fn, reads=r, writes=w)
        P = lambda fn, r, w: S.op("pe", fn, reads=r, writes=w)
        G = lambda fn, r, w: S.op("pool", fn, reads=r, writes=w)
        _mixer_consts(S, nc, st, g)
        A1 = sb("x_A1", [128, D]); B1 = sb("x_B1", [128, D])
        xt = [sb("x_xt%d" % i, [128, D]) for i in range(2)]
        hf = sb("x_hf", [128, D]); hb = sb("x_hb", [128, D], BF16)
        ss = sb("x_ss", [128, 1]); rstd = sb("x_rstd", [128, 1])
        hT = sb("x_hT", [128, 8, BT], BF16)
        wb = [sb("x_wb%d" % i, [128, 4096], BF16) for i in range(3)]
        yT = [sb("x_yT%d" % i, [128, 4, BT], BF16) for i in range(3)]
        ymix = sb("x_ymix", [128, 8, BT])
        ymixb = sb("x_ymixb", [128, 8, BT], BF16)
        cols = sb("x_cols", [128, 64])
        lbt = sb("x_lbt", [128, 8])
        f32t = [sb("x_f%d" % i, [128, BT]) for i in range(6)]
        f513 = sb("x_f513", [128, BT + 1])
        posi = f32t[5][:].bitcast(I32)
        carry = sb("x_carry", [128, 16])
        bq = [sb("x_bq%d" % i, [128, 4, BT], BF16) for i in range(6)]
        vtm = sb("x_vtm", [128, 4, 512], BF16)
        Sst = sb("x_S", [128, 4, 128]); Sbf = sb("x_Sbf", [128, 4, 128], BF16)
        Rst = sb("x_R", [128, 4, 128]); Rbf = sb("x_Rbf", [128, 4, 128], BF16)
        Wst = sb("x_Wst", [128, 4, 64])
        dS = sb("x_dS", [128, 4, 8]); dSr = sb("x_dSr", [128, 4, 8])
        sc4 = sb("x_sc4", [128, 4, 128], BF16)
        schg = sb("x_schg", [128, 4, 64], BF16)
        ktm = sb("x_ktm", [128, 4, 128], BF16)
        cosT = sb("x_cos", [128, BT]); sinT = sb("x_sin", [128, BT])
        lora = [sb("x_lora0", [128, BT]), sb("x_lora1", [128, BT], BF16)]
        w2a2 = sb("x_w2a2", [128, 512]); g2t = sb("x_g2", [128, 512], BF16)
        rw = [sb("x_rw%d" % i, [128, BT]) for i in range(3)]
        ARs = [sb("x_AR%d" % p, [128, 8, 128], BF16) for p in range(4)]
        bts = [sb("x_bt%d" % p, [128, BT], BF16) for p in range(4)]
        kts = [sb("x_kt%d" % p, [128, BT], BF16) for p in range(4)]
        vbs = [sb("x_vb%d" % p, [128, BT], BF16) for p in range(4)]
        bons = [sb("x_bon%d" % p, [128, BT], BF16) for p in range(4)]
        ABKs = [sb("x_ABK%d" % p, [128, 256], BF16) for p in range(4)]
        MTs = [sb("x_MT%d" % p, [128, 128], BF16) for p in range(4)]
        MNs = [[sb("x_MN%d_%d" % (p, i), [128, 128], BF16) for i in range(2)] for p in range(4)]
        NPs = [[MTs[p], sb("x_NP%d_1" % p, [128, 128], BF16)] for p in range(4)]
        Xs = [sb("x_X%d" % p, [128, 128], BF16) for p in range(4)]
        Wbf = sb("x_Wbf", [128, 4, 64], BF16)
        TMss = [sb("x_TMs%d" % p, [128, 192], BF16) for p in range(4)]
        Wsbs = [sb("x_Wsb%d" % p, [128, 64], BF16) for p in range(4)]
        Usbs = [sb("x_Usb%d" % p, [128, 64], BF16) for p in range(4)]
        tSs = [sb("x_tS%d" % p, [128, 64]) for p in range(4)]
        pz = [pst_("x_pz%d" % i, [128, 512]) for i in range(2)]
        ptb = pst_("x_ptb", [128, 8, 128], BF16)
        pw = [pst_("x_pw%d" % i, [128, 512]) for i in range(4)]
        psc, po, pss = pw[0], pw[1], pw[2]

        _load_mod(S, nc, g, io, l, 1, io.norm1_g[l:l + 1, :], A1[:], "x_A1", B1[:], "x_B1", None, None, hf[:], "x_hf")
        CK = "x_cols"
        colv = lambda v, n: v.rearrange("o (j p) -> p (o j)", p=128)
        nslow = dict(allow_slow_non_contiguous=True)
        S.dma("sp", cols[:, 0:14], colv(io.rw_mu[l:l + 1, :], 14), writes=[CK], **nslow)
        for i, nm in enumerate(["rw_w0", "rw_a0", "rw_k_k", "rw_k_a", "rw_r_k", "rw_ln_w", "rw_ln_b"]):
            S.dma("sp", cols[:, 16 + 4 * i:20 + 4 * i], colv(getattr(io, nm)[l:l + 1, :], 4), reads=[CK], writes=[CK], **nslow)
        c_w0, c_a0, c_kk, c_ka, c_rk, c_lw, c_lb = [lambda p, i=i: cols[:, 16 + 4 * i + p:17 + 4 * i + p] for i in range(7)]
        c_omka = lambda p: cols[:, 44 + p:45 + p]
        V(lambda e: e.tensor_scalar(out=cols[:, 44:48], in0=cols[:, 28:32], scalar1=-1.0, scalar2=1.0, op0=ALU.mult, op1=ALU.add), [CK], [CK])
        S.dma("sp", cols[:, 48:49], io.hg_norm_w[l:l + 1, :].rearrange("o p -> p o"), reads=[CK], writes=[CK], **nslow)
        S.dma("sp", lbt[:, 0:4], colv(io.hg_lb_table[0:1, :], 4), writes=["x_lbt"], **nslow)
        S.dma("sp", lbt[:, 4:8], colv(io.hg_lb_table[1:2, :], 4), reads=["x_lbt"], writes=["x_lbt"], **nslow)
        c_lbv = lambda h: cols[:, 52 + h:53 + h]
        c_oml = lambda h: cols[:, 56 + h:57 + h]
        if l == 0:
            V(lambda e: e.memset(cols[:, 52:56], 0.0), [CK], [CK])
        else:
            V(lambda e: e.tensor_tensor(out=cols[:, 52:56], in0=lbt[:, 4:8], in1=lbt[:, 0:4], op=ALU.subtract), ["x_lbt", CK], [CK])
            A(lambda e: e.activation(out=cols[:, 52:56], in_=cols[:, 52:56], func=AF.Sigmoid), [CK], [CK])
        V(lambda e: e.tensor_scalar(out=cols[:, 56:60], in0=cols[:, 52:56], scalar1=-1.0, scalar2=1.0, op0=ALU.mult, op1=ALU.add), [CK], [CK])
        S.dma("sp", w2a2[0:64, :], io.rw_w2[l], writes=["x_w2a2"])
        S.dma("sp", w2a2[64:128, :], io.rw_a2[l], reads=["x_w2a2"], writes=["x_w2a2"])
        S.dma("pool", g2t[:], io.rw_g2[l], writes=["x_g2"])
        for p in range(4):
            G(lambda e, p=p: e.memset(Wbf[:, p, :], 0.0), [], ["x_Wbf%d" % p])
            G(lambda e, p=p: e.memset(Wst[:, p, :], 0.0), [], ["x_Wst%d" % p])
            G(lambda e, p=p: e.memset(MTs[p][:], 0.0), [], ["x_MT%d" % p])
        G(lambda e: e.memset(carry[:], 0.0), [], ["x_carry"])
        for h_ in range(4):
            G(lambda e, h_=h_: e.memset(Rst[:, h_, :], 0.0), [], ["x_R%d" % h_])
            G(lambda e, h_=h_: e.memset(Rbf[:, h_, :], 0.0), [], ["x_Rbf%d" % h_])
        for h_ in range(4):
            G(lambda e, h_=h_: e.memset(schg[:, h_, :], 0.0), [], ["x_schg%d" % h_])
            G(lambda e, h_=h_: e.memset(Sst[:, h_, :], 0.0), [], ["x_S%d" % h_])
            G(lambda e, h_=h_: e.memset(Sbf[:, h_, :], 0.0), [], ["x_Sbf%d" % h_])

        wlist = _mixer_wlist(io, l)
        NW = len(wlist)
        wst = {"issued": 0, "got": 0, "rel": -1, "total": NW * nblocks}

        def w_issue():
            j = wst["issued"]; wst["issued"] += 1
            src, kparts, ncols = wlist[j % NW]
            i = j % 3
            S.dma("sp", wb[i][:, 0:kparts * ncols], io.wbf[l, j % NW, :, 0:kparts * ncols],
                  reads=["wbf%d" % l], writes=["x_wb%d" % i])

        def w_pump():
            while wst["issued"] < wst["total"] and wst["issued"] <= wst["got"] + 1 and wst["issued"] - 3 <= wst["rel"]:
                w_issue()

        def w_get():
            j = wst["got"]; wst["got"] += 1
            while wst["issued"] <= j:
                assert wst["issued"] - 3 <= wst["rel"], "weight buffer still live"
                w_issue()
            src, kparts, ncols = wlist[j % NW]
            i = j % 3
            view = wb[i][:, 0:kparts * ncols].rearrange("p (k n) -> p k n", k=kparts)
            return view, "x_wb%d" % i

        def w_rel(n=1):
            wst["rel"] += n
            w_pump()

        pzc = [0]

        def proj_fm(W, wk, c0, nc_=128, row0=0, same=False):
            if same:
                i = (pzc[0] - 1) % 2
            else:
                i = pzc[0] % 2
                pzc[0] += 1
            for k in range(8):
                P(lambda e, k=k, i=i: e.matmul(pz[i][row0:row0 + nc_, :], lhsT=W[:, k, c0:c0 + nc_], rhs=hT[:, k, :],
                                               start=(k == 0), stop=(k == 7)), [wk, "x_hT"], ["x_pz%d" % i])
            return pz[i], "x_pz%d" % i

        def wgrp(c0, ncols=512):
            return w_get()

        for blk in range(nblocks):
            t0 = blk * BT
            for i in range(4):
                xb = xt[i % 2]; xk = "x_xt%d" % (i % 2)
                S.dma("sp", xb[:], x_src[t0 + i * 128:t0 + (i + 1) * 128, :], reads=[xs_key], writes=[xk])
                _rms_rstd(S, g, xb[:], xk, hf[:], "x_hf", ss[:], rstd[:], "x_")
                V(lambda e, xb=xb: e.scalar_tensor_tensor(out=hf[:], in0=xb[:], scalar=rstd[:], in1=A1[:], op0=ALU.mult, op1=ALU.mult),
                  [xk, "x_rstd", "x_A1"], ["x_hf"])
                V(lambda e: e.tensor_tensor(out=hb[:], in0=hf[:], in1=B1[:], op=ALU.add), ["x_hf", "x_B1"], ["x_hb"])
                for k in range(8):
                    P(lambda e, k=k: e.transpose(ptb[:, k, :], hb[:, k * 128:(k + 1) * 128], g.identb[:]), ["x_hb", "identb"], ["x_ptb"])
                A(lambda e, i=i: e.copy(out=hT[:, :, i * 128:(i + 1) * 128], in_=ptb[:]), ["x_ptb"], ["x_hT"])

            qsil, qh, qs_, kh, kt_, sgt = bq
            qk = ["x_bq%d" % i for i in range(6)]
            W, wk = wgrp(0)
            for h in range(4):
                pzt, pk = proj_fm(W, wk, h * 128)
                A(lambda e, h=h, pzt=pzt: e.activation(out=qsil[:, h, :], in_=pzt[:], func=AF.Silu), [pk], [qk[0]])
            w_rel()
            W, wk = wgrp(512)
            lf, cum, tmpa, tmpb, kf = f32t[0], f32t[1], f32t[2], f32t[3], f32t[4]
            c3 = lambda tl: tl[:].rearrange("p (c j) -> p c j", j=64)
            for h in range(4):
                pzt, pk = proj_fm(W, wk, h * 128)
                A(lambda e, pzt=pzt: e.activation(out=tmpa[:], in_=pzt[:], func=AF.Sigmoid), [pk], ["x_f2"])
                V(lambda e, h=h: e.tensor_scalar(out=tmpa[:], in0=tmpa[:], scalar1=c_oml(h), scalar2=c_lbv(h), op0=ALU.mult, op1=ALU.add),
                  ["x_f2", CK], ["x_f2"])
                A(lambda e: e.activation(out=lf[:], in_=tmpa[:], func=AF.Ln), ["x_f2"], ["x_f0"])
                V(lambda e: e.tensor_scalar(out=kf[:], in0=tmpa[:], scalar1=-1.0, scalar2=1.0, op0=ALU.mult, op1=ALU.add), ["x_f2"], ["x_f4"])
                V(lambda e: e.tensor_tensor_scan(out=cum[:], data0=g.rm[:], data1=lf[:], initial=0.0, op0=ALU.mult, op1=ALU.add),
                  ["c_rm", "x_f0"], ["x_f1"])
                V(lambda e: e.tensor_tensor(out=c3(tmpa), in0=c3(cum), in1=c3(cum)[:, :, 31:32].to_broadcast([128, 8, 64]), op=ALU.subtract),
                  ["x_f1"], ["x_f2"])
                A(lambda e: e.activation(out=tmpb[:], in_=tmpa[:], func=AF.Exp), ["x_f2"], ["x_f3"])
                V(lambda e, h=h: e.scalar_tensor_tensor(out=qh[:, h, :], in0=qsil[:, h, :], scalar=128.0 ** -0.5, in1=tmpb[:], op0=ALU.mult, op1=ALU.mult),
                  [qk[0], "x_f3"], [qk[1]])
                A(lambda e: e.activation(out=tmpb[:], in_=tmpa[:], func=AF.Exp, scale=-1.0), ["x_f2"], ["x_f3"])
                V(lambda e, h=h: e.tensor_tensor(out=kh[:, h, :], in0=kf[:], in1=tmpb[:], op=ALU.mult), ["x_f4", "x_f3"], [qk[3]])
                A(lambda e: e.activation(out=tmpb[:], in_=cum[:], func=AF.Exp), ["x_f1"], ["x_f3"])
                V(lambda e, h=h: e.scalar_tensor_tensor(out=qs_[:, h, :], in0=qsil[:, h, :], scalar=128.0 ** -0.5, in1=tmpb[:], op0=ALU.mult, op1=ALU.mult),
                  [qk[0], "x_f3"], [qk[2]])
                V(lambda e, h=h: e.tensor_copy(out=dS[:, h, :], in_=c3(tmpb)[:, :, 63]), ["x_f3"], ["x_dS"])
                V(lambda e: e.tensor_tensor(out=c3(tmpa), in0=c3(cum)[:, :, 63:64].to_broadcast([128, 8, 64]), in1=c3(cum), op=ALU.subtract),
                  ["x_f1"], ["x_f2"])
                A(lambda e: e.activation(out=tmpb[:], in_=tmpa[:], func=AF.Exp), ["x_f2"], ["x_f3"])
                V(lambda e, h=h: e.tensor_tensor(out=kt_[:, h, :], in0=kf[:], in1=tmpb[:], op=ALU.mult), ["x_f4", "x_f3"], [qk[4]])
            w_rel()
            W, wk = wgrp(1024)
            for i in range(4):
                pi_ = pzc[0] % 2; pzc[0] += 1
                for k in range(8):
                    P(lambda e, k=k, i=i, pi_=pi_: e.matmul(pz[pi_][:], lhsT=hT[:, k, i * 128:(i + 1) * 128], rhs=W[:, k, :], start=(k == 0), stop=(k == 7)),
                      [wk, "x_hT"], ["x_pz%d" % pi_])
                A(lambda e, i=i, pi_=pi_: e.copy(out=vtm[:, i, :], in_=pz[pi_][:]), ["x_pz%d" % pi_], ["x_vtm"])
            w_rel()
            W, wk = wgrp(1536)
            for h in range(4):
                pzt, pk = proj_fm(W, wk, h * 128)
                A(lambda e, h=h, pzt=pzt: e.activation(out=sgt[:, h, :], in_=pzt[:], func=AF.Silu), [pk], [qk[5]])
            w_rel()
            def rope_gen():
                S.dma("sp", posi, io.pos[0:1, t0:t0 + BT].to_broadcast([128, BT]), writes=["x_f5"])
                ang, rr, nf, mm = f32t[0], f32t[1], f32t[2], f32t[3]
                V(lambda e: e.tensor_copy(out=ang[:], in_=posi), ["x_f5"], ["x_f0"])
                yield
                V(lambda e: e.tensor_scalar(out=ang[:], in0=ang[:], scalar1=g.invf[:], scalar2=None, op0=ALU.mult), ["x_f0", "c_invf"], ["x_f0"])
                yield
                for which in range(2):
                    dst, dk_ = (sinT, "x_sin") if which == 0 else (cosT, "x_cos")
                    off = 0.0 if which == 0 else PI / 2
                    V(lambda e, off=off: e.tensor_scalar(out=rr[:], in0=ang[:], scalar1=off, scalar2=None, op0=ALU.add), ["x_f0"], ["x_f1"])
                    yield
                    V(lambda e: e.tensor_scalar(out=posi, in0=rr[:], scalar1=1.0 / (2 * PI), scalar2=None, op0=ALU.mult), ["x_f1"], ["x_f5"])
                    yield
                    V(lambda e: e.tensor_copy(out=nf[:], in_=posi), ["x_f5"], ["x_f2"])
                    yield
                    V(lambda e: e.scalar_tensor_tensor(out=rr[:], in0=nf[:], scalar=-2 * PI, in1=rr[:], op0=ALU.mult, op1=ALU.add), ["x_f2", "x_f1"], ["x_f1"])
                    yield
                    V(lambda e: e.tensor_scalar(out=mm[:], in0=rr[:], scalar1=PI, scalar2=None, op0=ALU.is_gt), ["x_f1"], ["x_f3"])
                    yield
                    V(lambda e: e.scalar_tensor_tensor(out=rr[:], in0=mm[:], scalar=-2 * PI, in1=rr[:], op0=ALU.mult, op1=ALU.add), ["x_f3", "x_f1"], ["x_f1"])
                    yield
                    V(lambda e: e.tensor_scalar(out=mm[:], in0=rr[:], scalar1=-PI, scalar2=None, op0=ALU.is_lt), ["x_f1"], ["x_f3"])
                    yield
                    V(lambda e: e.scalar_tensor_tensor(out=rr[:], in0=mm[:], scalar=2 * PI, in1=rr[:], op0=ALU.mult, op1=ALU.add), ["x_f3", "x_f1"], ["x_f1"])
                    yield
                    V(lambda e: e.tensor_scalar(out=rr[:], in0=rr[:], scalar1=3.1415925, scalar2=-3.1415925, op0=ALU.min, op1=ALU.max), ["x_f1"], ["x_f1"])
                    yield
                    if which == 0:
                        A(lambda e, dst=dst: e.activation(out=dst[:], in_=rr[:], func=AF.Sin, scale=g.sgn[:]), ["x_f1", "c_sgn"], [dk_])
                        yield
                    else:
                        A(lambda e, dst=dst: e.activation(out=dst[:], in_=rr[:], func=AF.Sin), ["x_f1"], [dk_])
                        yield
                yield

            rg = rope_gen()
            for c in range(8):
                par = c % 2; rows = slice(par * 64, par * 64 + 64); cs = slice(c * 64, c * 64 + 64); tl = c // 2
                for h in range(4):
                    P(lambda e, h=h: e.matmul(pz[0][rows, h * 64:(h + 1) * 64], lhsT=kh[:, h, cs], rhs=qh[:, h, cs], start=True, stop=True),
                      [qk[3], qk[1]], ["x_pz0"])
                for h in range(4):
                    V(lambda e, h=h: e.copy_predicated(out=schg[rows, h, :], mask=g.mgei[rows, :], data=pz[0][rows, h * 64:(h + 1) * 64]),
                      ["x_pz0", "c_mgei"], ["x_schg%d" % h])
                for h in range(4):
                    P(lambda e, h=h: e.matmul(pw[h][:, cs], lhsT=vtm[rows, tl, h * 128:(h + 1) * 128], rhs=schg[rows, h, :], start=True, stop=False),
                      ["x_vtm", "x_schg%d" % h], ["x_pw%d" % h])
                    P(lambda e, h=h: e.matmul(pw[h][:, cs], lhsT=Sbf[:, h, :], rhs=qs_[:, h, cs], start=False, stop=True),
                      ["x_Sbf%d" % h, qk[2]], ["x_pw%d" % h])
                for h in range(4):
                    P(lambda e, h=h: e.transpose(ptb[rows, h, :], kt_[:, h, cs], g.identb[:]), [qk[4], "identb"], ["x_ptb"])
                for h in range(4):
                    A(lambda e, h=h: e.copy(out=ktm[rows, h, :], in_=ptb[rows, h, :]), ["x_ptb"], ["x_ktm%d" % h])
                for h in range(4):
                    P(lambda e, h=h: e.matmul(pz[1][:, h * 128:(h + 1) * 128], lhsT=ktm[rows, h, :], rhs=vtm[rows, tl, h * 128:(h + 1) * 128], start=True, stop=True),
                      ["x_ktm%d" % h, "x_vtm"], ["x_pz1"])
                for h in range(4):
                    V(lambda e, h=h: e.scalar_tensor_tensor(out=Sst[:, h, :], in0=Sst[:, h, :], scalar=dS[:, h, c:c + 1], in1=pz[1][:, h * 128:(h + 1) * 128], op0=ALU.mult, op1=ALU.add),
                      ["x_S%d" % h, "x_dS", "x_pz1"], ["x_S%d" % h])
                for h in range(4):
                    A(lambda e, h=h: e.copy(out=Sbf[:, h, :], in_=Sst[:, h, :]), ["x_S%d" % h], ["x_Sbf%d" % h])
                for _ in range(4):
                    next(rg, None)
            for _ in rg:
                pass
            for h in range(4):
                A(lambda e, h=h: e.activation(out=f32t[h][:], in_=pw[h][:], func=AF.Square), ["x_pw%d" % h], ["x_f%d" % h])
            for h in range(4):
                P(lambda e, h=h: e.matmul(pz[h % 2][:], lhsT=g.ones[:], rhs=f32t[h][:], start=True, stop=True), ["ones", "x_f%d" % h], ["x_pz%d" % (h % 2)])
                V(lambda e, h=h: e.tensor_scalar(out=f32t[h][:], in0=pz[h % 2][:], scalar1=1.0 / 128, scalar2=EPS, op0=ALU.mult, op1=ALU.add), ["x_pz%d" % (h % 2)], ["x_f%d" % h])
            for h in range(4):
                A(lambda e, h=h: e.activation(out=f32t[h][:], in_=f32t[h][:], func=AF.Sqrt), ["x_f%d" % h], ["x_f%d" % h])
            for h in range(4):
                V(lambda e, h=h: e.reciprocal(out=f32t[h][:], in_=f32t[h][:]), ["x_f%d" % h], ["x_f%d" % h])
            for h in range(4):
                V(lambda e, h=h: e.tensor_tensor(out=f32t[h][:], in0=pw[h][:], in1=f32t[h][:], op=ALU.mult), ["x_pw%d" % h, "x_f%d" % h], ["x_f%d" % h])
            for h in range(4):
                V(lambda e, h=h: e.scalar_tensor_tensor(out=yT[0][:, h, :], in0=f32t[h][:], scalar=cols[:, 48:49], in1=sgt[:, h, :], op0=ALU.mult, op1=ALU.mult),
                  ["x_f%d" % h, CK, qk[5]], ["x_yT0"])

            qr, qx, kr, kz, sgr = bq[0], bq[1], bq[2], bq[3], bq[4]
            c4 = lambda ap: ap.rearrange("p (c j) -> p c j", j=128)
            for isk in range(2):
                if isk == 1:
                    w_rel()
                W, wk = wgrp(2048 + isk * 512)
                for h in range(4):
                    pzt, pk = proj_fm(W, wk, h * 128)
                    V(lambda e, pzt=pzt: e.tensor_tensor(out=f32t[4][:], in0=pzt[:], in1=cosT[:], op=ALU.mult), [pk, "x_cos"], ["x_f4"])
                    proj_fm(W, wk, h * 128 + 64, 64, 0)
                    pzr, pkr = proj_fm(W, wk, h * 128, 64, 64, same=True)
                    V(lambda e, pzr=pzr: e.tensor_tensor(out=f32t[5][:], in0=pzr[:], in1=sinT[:], op=ALU.mult), [pkr, "x_sin"], ["x_f5"])
                    V(lambda e: e.tensor_tensor(out=f32t[4][:], in0=f32t[4][:], in1=f32t[5][:], op=ALU.add), ["x_f4", "x_f5"], ["x_f4"])
                    if isk == 0:
                        A(lambda e, h=h: e.mul(out=qr[:, h, :], in_=f32t[4][:], mul=128.0 ** -0.5), ["x_f4"], [qk[0]])
                        V(lambda e, h=h: e.scalar_tensor_tensor(out=c4(qx[:, h, :]), in0=c4(f32t[4][:]), scalar=128.0 ** -0.5,
                                                                in1=g.XI[:, h:h + 1, :].to_broadcast([128, 4, 128]), op0=ALU.mult, op1=ALU.mult),
                          ["x_f4", "c_XI"], [qk[1]])
                    else:
                        A(lambda e, h=h: e.copy(out=kr[:, h, :], in_=f32t[4][:]), ["x_f4"], [qk[2]])
                        V(lambda e, h=h: e.tensor_tensor(out=c4(kz[:, h, :]), in0=c4(f32t[4][:]), in1=g.ZE[:, h:h + 1, :].to_broadcast([128, 4, 128]), op=ALU.mult),
                          ["x_f4", "c_ZE"], [qk[3]])
            w_rel()
            W, wk = wgrp(3072)
            for i in range(4):
                pi_ = pzc[0] % 2; pzc[0] += 1
                for k in range(8):
                    P(lambda e, k=k, i=i, pi_=pi_: e.matmul(pz[pi_][:], lhsT=hT[:, k, i * 128:(i + 1) * 128], rhs=W[:, k, :], start=(k == 0), stop=(k == 7)),
                      [wk, "x_hT"], ["x_pz%d" % pi_])
                A(lambda e, i=i, pi_=pi_: e.copy(out=vtm[:, i, :], in_=pz[pi_][:]), ["x_pz%d" % pi_], ["x_vtm"])
            w_rel()
            W, wk = wgrp(3584)
            for h in range(4):
                pzt, pk = proj_fm(W, wk, h * 128)
                A(lambda e, h=h, pzt=pzt: e.activation(out=sgr[:, h, :], in_=pzt[:], func=AF.Silu), [pk], [qk[4]])
            w_rel()
            gam = [math.exp(128.0 * g.lng[h]) for h in range(4)]
            scr = sc4
            for c in range(4):
                cs = slice(c * 128, c * 128 + 128)
                for h in range(4):
                    P(lambda e, h=h: e.matmul(pz[0][:, h * 128:(h + 1) * 128], lhsT=kr[:, h, cs], rhs=qr[:, h, cs], start=True, stop=True), [qk[2], qk[0]], ["x_pz0"])
                for h in range(4):
                    V(lambda e, h=h: e.tensor_tensor(out=scr[:, h, :], in0=pz[0][:, h * 128:(h + 1) * 128], in1=g.DM[:, h, :], op=ALU.mult), ["x_pz0", "c_DM"], ["x_sc4_%d" % h])
                for h in range(4):
                    P(lambda e, h=h: e.matmul(pw[h][:, cs], lhsT=vtm[:, c, h * 128:(h + 1) * 128], rhs=scr[:, h, :], start=True, stop=False),
                      ["x_vtm", "x_sc4_%d" % h], ["x_pw%d" % h])
                    P(lambda e, h=h: e.matmul(pw[h][:, cs], lhsT=Rbf[:, h, :], rhs=qx[:, h, cs], start=False, stop=True), ["x_Rbf%d" % h, qk[1]], ["x_pw%d" % h])
                for h in range(4):
                    P(lambda e, h=h: e.transpose(ptb[:, h, :], kz[:, h, cs], g.identb[:]), [qk[3], "identb"], ["x_ptb"])
                for h in range(4):
                    A(lambda e, h=h: e.copy(out=ktm[:, h, :], in_=ptb[:, h, :]), ["x_ptb"], ["x_ktm%d" % h])
                for h in range(4):
                    P(lambda e, h=h: e.matmul(pz[1][:, h * 128:(h + 1) * 128], lhsT=ktm[:, h, :], rhs=vtm[:, c, h * 128:(h + 1) * 128], start=True, stop=True),
                      ["x_ktm%d" % h, "x_vtm"], ["x_pz1"])
                for h in range(4):
                    V(lambda e, h=h: e.scalar_tensor_tensor(out=Rst[:, h, :], in0=Rst[:, h, :], scalar=gam[h], in1=pz[1][:, h * 128:(h + 1) * 128], op0=ALU.mult, op1=ALU.add),
                      ["x_R%d" % h, "x_pz1"], ["x_R%d" % h])
                for h in range(4):
                    A(lambda e, h=h: e.copy(out=Rbf[:, h, :], in_=Rst[:, h, :]), ["x_R%d" % h], ["x_Rbf%d" % h])
            for h in range(4):
                A(lambda e, h=h: e.activation(out=f32t[h][:], in_=pw[h][:], func=AF.Square), ["x_pw%d" % h], ["x_f%d" % h])
            for h in range(4):
                P(lambda e, h=h: e.matmul(pz[h % 2][:], lhsT=g.ones[:], rhs=f32t[h][:], start=True, stop=True), ["ones", "x_f%d" % h], ["x_pz%d" % (h % 2)])
                V(lambda e, h=h: e.tensor_scalar(out=f32t[h][:], in0=pz[h % 2][:], scalar1=1.0 / 128, scalar2=EPS, op0=ALU.mult, op1=ALU.add), ["x_pz%d" % (h % 2)], ["x_f%d" % h])
            for h in range(4):
                A(lambda e, h=h: e.activation(out=f32t[h][:], in_=f32t[h][:], func=AF.Sqrt), ["x_f%d" % h], ["x_f%d" % h])
            for h in range(4):
                V(lambda e, h=h: e.reciprocal(out=f32t[h][:], in_=f32t[h][:]), ["x_f%d" % h], ["x_f%d" % h])
            for h in range(4):
                V(lambda e, h=h: e.tensor_tensor(out=f32t[h][:], in0=pw[h][:], in1=f32t[h][:], op=ALU.mult), ["x_pw%d" % h, "x_f%d" % h], ["x_f%d" % h])
            for h in range(4):
                V(lambda e, h=h: e.tensor_tensor(out=yT[1][:, h, :], in0=f32t[h][:], in1=sgr[:, h, :], op=ALU.mult), ["x_f%d" % h, qk[4]], ["x_yT1"])

            def shifted(pzt, pk, j, dst, dkey):
                A(lambda e: e.copy(out=f513[:, 1:BT + 1], in_=pzt[:]), [pk], ["x_f513"])
                A(lambda e: e.copy(out=f513[:, 0:1], in_=carry[:, j:j + 1]), ["x_carry", "x_f513"], ["x_f513"])
                V(lambda e: e.tensor_tensor(out=dst[:], in0=f513[:, 0:BT], in1=f513[:, 1:BT + 1], op=ALU.subtract), ["x_f513"], [dkey])
                V(lambda e: e.scalar_tensor_tensor(out=dst[:], in0=dst[:], scalar=cols[:, j:j + 1], in1=f513[:, 1:BT + 1], op0=ALU.mult, op1=ALU.add),
                  [dkey, CK, "x_f513"], [dkey])
                A(lambda e: e.copy(out=carry[:, j:j + 1], in_=f513[:, BT:BT + 1]), ["x_f513"], ["x_carry"])

            W, wk = wgrp(4096 + 1536, 256)
            pzt, pk = proj_fm(W, wk, 0)
            shifted(pzt, pk, 12, lora[0], "x_lora0")
            A(lambda e: e.activation(out=lora[0][0:64, :], in_=lora[0][0:64, :], func=AF.Tanh), ["x_lora0"], ["x_lora0"])
            pzt, pk = proj_fm(W, wk, 128)
            shifted(pzt, pk, 13, lora[1], "x_lora1")
            A(lambda e: e.activation(out=lora[1][:], in_=lora[1][:], func=AF.Sigmoid), ["x_lora1"], ["x_lora1"])
            w_rel()
            Wr_, wkr = wgrp(4096)
            Wk_, wkk = wgrp(4096 + 512)
            Wv_, wkv = wgrp(4096 + 1024)
            rs, ks, vs = rw
            rk = ["x_rw0", "x_rw1", "x_rw2"]
            ar3 = lambda tl: tl[:].rearrange("p (c j) -> p c j", j=64)
            t0_, t1_, t2_, t3_ = f32t[0], f32t[1], f32t[2], f32t[3]
            pq = pw[3]; PQ = ["x_pw3"]
            for p in range(4):
                pc = slice(p * 128, p * 128 + 128)
                AR = ARs[p]; bt = bts[p]; kt = kts[p]; vb = vbs[p]; bon = bons[p]
                KAR = "x_AR%d" % p; KBT = "x_bt%d" % p; KKT = "x_kt%d" % p; KVB = "x_vb%d" % p; KBON = "x_bon%d" % p
                pzt, pk = proj_fm(Wr_, wkr, p * 128); shifted(pzt, pk, p, rs, rk[0])
                pzt, pk = proj_fm(Wk_, wkk, p * 128); shifted(pzt, pk, 4 + p, ks, rk[1])
                pzt, pk = proj_fm(Wv_, wkv, p * 128); shifted(pzt, pk, 8 + p, vs, rk[2])
                A(lambda e, vb=vb: e.copy(out=vb[:], in_=vs[:]), [rk[2]], [KVB])
                P(lambda e, pc=pc: e.matmul(pq[:], lhsT=w2a2[0:64, pc], rhs=lora[0][0:64, :], start=True, stop=True), ["x_w2a2", "x_lora0"], PQ)
                A(lambda e, p=p: e.activation(out=t0_[:], in_=pq[:], func=AF.Sigmoid, bias=c_w0(p)), PQ + [CK], ["x_f0"])
                P(lambda e, pc=pc: e.matmul(pq[:], lhsT=w2a2[64:128, pc], rhs=lora[0][64:128, :], start=True, stop=True), ["x_w2a2", "x_lora0"], PQ)
                A(lambda e, p=p: e.activation(out=t1_[:], in_=pq[:], func=AF.Sigmoid, bias=c_a0(p)), PQ + [CK], ["x_f1"])
                V(lambda e, p=p: e.tensor_scalar(out=t2_[:], in0=ks[:], scalar1=c_kk(p), scalar2=None, op0=ALU.mult), [rk[1], CK], ["x_f2"])
                A(lambda e: e.activation(out=t3_[:], in_=t2_[:], func=AF.Square), ["x_f2"], ["x_f3"])
                P(lambda e: e.matmul(pq[:], lhsT=g.blk[:], rhs=t3_[:], start=True, stop=True), ["c_blk", "x_f3"], PQ)
                V(lambda e: e.tensor_scalar(out=t3_[:], in0=pq[:], scalar1=1e-24, scalar2=None, op0=ALU.max), PQ, ["x_f3"])
                A(lambda e: e.activation(out=t3_[:], in_=t3_[:], func=AF.Sqrt), ["x_f3"], ["x_f3"])
                V(lambda e: e.reciprocal(out=t3_[:], in_=t3_[:]), ["x_f3"], ["x_f3"])
                V(lambda e: e.tensor_tensor(out=t2_[:], in0=t2_[:], in1=t3_[:], op=ALU.mult), ["x_f2", "x_f3"], ["x_f2"])
                V(lambda e, p=p: e.tensor_scalar(out=t3_[:], in0=t1_[:], scalar1=c_ka(p), scalar2=c_omka(p), op0=ALU.mult, op1=ALU.add), ["x_f1", CK], ["x_f3"])
                V(lambda e: e.tensor_tensor(out=ks[:], in0=ks[:], in1=t3_[:], op=ALU.mult), [rk[1], "x_f3"], [rk[1]])
                V(lambda e, p=p: e.scalar_tensor_tensor(out=t3_[:], in0=rs[:], scalar=c_rk(p), in1=ks[:], op0=ALU.mult, op1=ALU.mult), [rk[0], CK, rk[1]], ["x_f3"])
                P(lambda e: e.matmul(pq[:], lhsT=g.blk[:], rhs=t3_[:], start=True, stop=True), ["c_blk", "x_f3"], PQ)
                V(lambda e, bon=bon: e.tensor_tensor(out=bon[:], in0=pq[:], in1=vs[:], op=ALU.mult), PQ + [rk[2]], [KBON])
                cs_, u_ = f32t[4], f32t[5]
                V(lambda e: e.tensor_tensor_scan(out=cs_[:], data0=g.rm[:], data1=t0_[:], initial=0.0, op0=ALU.mult, op1=ALU.add), ["c_rm", "x_f0"], ["x_f4"])
                A(lambda e: e.activation(out=u_[:], in_=cs_[:], func=AF.Exp, scale=-E05), ["x_f4"], ["x_f5"])
                V(lambda e, AR=AR: e.tensor_tensor(out=AR[:, :, 64:128], in0=ar3(rs), in1=ar3(u_), op=ALU.mult), [rk[0], "x_f5"], [KAR])
                V(lambda e, p=p: e.tensor_copy(out=dSr[:, p, :], in_=c3(u_)[:, :, 63]), ["x_f5"], ["x_dSr"])
                A(lambda e: e.activation(out=u_[:], in_=cs_[:], func=AF.Exp, scale=E05), ["x_f4"], ["x_f5"])
                V(lambda e, kt=kt: e.tensor_tensor(out=kt[:], in0=ks[:], in1=u_[:], op=ALU.mult), [rk[1], "x_f5"], [KKT])
                V(lambda e: e.tensor_tensor(out=t3_[:], in0=t2_[:], in1=t1_[:], op=ALU.mult), ["x_f2", "x_f1"], ["x_f3"])
                V(lambda e, bt=bt: e.tensor_tensor(out=bt[:], in0=t3_[:], in1=u_[:], op=ALU.mult), ["x_f3", "x_f5"], [KBT])
                V(lambda e: e.tensor_tensor(out=cs_[:], in0=cs_[:], in1=t0_[:], op=ALU.subtract), ["x_f4", "x_f0"], ["x_f4"])
                A(lambda e: e.activation(out=u_[:], in_=cs_[:], func=AF.Exp, scale=-E05), ["x_f4"], ["x_f5"])
                V(lambda e, AR=AR: e.scalar_tensor_tensor(out=AR[:, :, 0:64], in0=ar3(t2_), scalar=-1.0, in1=ar3(u_), op0=ALU.mult, op1=ALU.mult), ["x_f2", "x_f5"], [KAR])
            w_rel(3)
            HH = [slice(0, 64), slice(64, 128)]
            PR = range(4)
            ka = lambda p: "x_pw%d" % p
            kb = lambda p: "x_pw%d" % p
            for c in range(8):
                cs = slice(c * 64, c * 64 + 64)
                for p in PR:
                    for rows in HH:
                        for q, (lt, lk) in enumerate(((bts[p], "x_bt%d" % p), (kts[p], "x_kt%d" % p))):
                            P(lambda e, rows=rows, q=q, lt=lt, p=p: e.matmul(pw[p][rows, q * 128:(q + 1) * 128], lhsT=lt[rows, cs], rhs=ARs[p][rows, c, :], start=True, stop=True),
                              [lk, "x_AR%d" % p], [ka(p)])
                for p in PR:
                    V(lambda e, p=p: e.tensor_tensor(out=ABKs[p][:], in0=pw[p][:, 0:256], in1=g.m4[:], op=ALU.mult), [ka(p), "c_m4"], ["x_ABK%d" % p])
                for p in PR:
                    for rows in HH:
                        for q, (src, sk) in enumerate(((bts[p], "x_bt%d" % p), (kts[p], "x_kt%d" % p), (vbs[p], "x_vb%d" % p))):
                            P(lambda e, rows=rows, q=q, src=src, p=p: e.matmul(pw[p][rows, 256 + q * 64:256 + (q + 1) * 64], lhsT=src[rows, cs], rhs=g.identb[rows, rows], start=True, stop=True),
                              [sk, "identb"], [kb(p)])
                for p in PR:
                    A(lambda e, p=p: e.copy(out=TMss[p][:], in_=pw[p][:, 256:448]), [kb(p)], ["x_TMs%d" % p])
                for p in PR:
                    A(lambda e, p=p: e.copy(out=MTs[p][0:64, 0:64], in_=ABKs[p][0:64, 0:64]), ["x_ABK%d" % p], ["x_MT%d" % p])
                    A(lambda e, p=p: e.copy(out=MTs[p][64:128, 64:128], in_=ABKs[p][64:128, 0:64]), ["x_ABK%d" % p], ["x_MT%d" % p])
                for p in PR:
                    P(lambda e, p=p: e.transpose(ptb[:, p, :], MTs[p][:], g.identb[:]), ["x_MT%d" % p, "identb"], ["x_ptb"])
                for p in PR:
                    A(lambda e, p=p: e.copy(out=MNs[p][0][:], in_=ptb[:, p, :]), ["x_ptb"], ["x_MN%d_0" % p])
                    V(lambda e, p=p: e.tensor_tensor(out=Xs[p][:], in0=MTs[p][:], in1=g.identb[:], op=ALU.add), ["x_MT%d" % p, "identb"], ["x_X%d" % p])
                curP = [(MTs[p], "x_MT%d" % p) for p in PR]
                curT = [(MNs[p][0], "x_MN%d_0" % p) for p in PR]
                for lev in range(1, 6):
                    nT = [(MNs[p][lev % 2], "x_MN%d_%d" % (p, lev % 2)) for p in PR]
                    nP = [(NPs[p][lev % 2], ("x_NP%d_1" % p) if lev % 2 == 1 else ("x_MT%d" % p)) for p in PR]
                    for p in PR:
                        P(lambda e, p=p, a_=curP[p][0], b_=curT[p][0]: e.matmul(pw[p][:, 0:128], lhsT=a_[:], rhs=b_[:], start=True, stop=True), [curP[p][1], curT[p][1]], [ka(p)])
                        if lev < 5:
                            P(lambda e, p=p, a_=curP[p][0], b_=curT[p][0]: e.matmul(pw[p][:, 128:256], lhsT=b_[:], rhs=a_[:], start=True, stop=True), [curP[p][1], curT[p][1]], [ka(p)])
                    for p in PR:
                        A(lambda e, p=p, t_=nT[p][0]: e.copy(out=t_[:], in_=pw[p][:, 0:128]), [ka(p)], [nT[p][1]])
                        if lev < 5:
                            V(lambda e, p=p, t_=nP[p][0]: e.tensor_copy(out=t_[:], in_=pw[p][:, 128:256]), [ka(p)], [nP[p][1]])
                    for p in PR:
                        P(lambda e, p=p, t_=nT[p][0]: e.matmul(pw[p][:, 256:384], lhsT=t_[:], rhs=Xs[p][:], start=True, stop=True), [nT[p][1], "x_X%d" % p], [kb(p)])
                    for p in PR:
                        V(lambda e, p=p: e.tensor_tensor(out=Xs[p][:], in0=Xs[p][:], in1=pw[p][:, 256:384], op=ALU.add), ["x_X%d" % p, kb(p)], ["x_X%d" % p])
                    if lev < 5:
                        curP = nP
                    curT = nT
                for p in PR:
                    for rows in HH:
                        P(lambda e, rows=rows, p=p: e.matmul(pw[p][rows, 384:448], lhsT=ARs[p][rows, c, 0:64], rhs=Wbf[rows, p, :], start=True, stop=False), ["x_AR%d" % p, "x_Wbf%d" % p], [kb(p)])
                        P(lambda e, rows=rows, p=p: e.matmul(pw[p][rows, 384:448], lhsT=ABKs[p][rows, 128:192], rhs=TMss[p][rows, 128:192], start=False, stop=True), ["x_ABK%d" % p, "x_TMs%d" % p], [kb(p)])
                for p in PR:
                    V(lambda e, p=p: e.tensor_copy(out=Wsbs[p][:], in_=pw[p][:, 384:448]), [kb(p)], ["x_Wsb%d" % p])
                for p in PR:
                    P(lambda e, p=p: e.matmul(pw[p][:, 448:512], lhsT=Xs[p][:], rhs=Wsbs[p][:], start=True, stop=True), ["x_X%d" % p, "x_Wsb%d" % p], [kb(p)])
                for p in PR:
                    A(lambda e, p=p: e.copy(out=Usbs[p][:], in_=pw[p][:, 448:512]), [kb(p)], ["x_Usb%d" % p])
                for p in PR:
                    for rows in HH:
                        P(lambda e, rows=rows, p=p: e.matmul(pw[p][rows, 256:320], lhsT=Wbf[rows, p, :], rhs=ARs[p][rows, c, 64:128], start=True, stop=False), ["x_Wbf%d" % p, "x_AR%d" % p], [kb(p)])
                        P(lambda e, rows=rows, p=p: e.matmul(pw[p][rows, 256:320], lhsT=Usbs[p][rows, :], rhs=ABKs[p][rows, 64:128], start=False, stop=False), ["x_Usb%d" % p, "x_ABK%d" % p], [kb(p)])
                        P(lambda e, rows=rows, p=p: e.matmul(pw[p][rows, 256:320], lhsT=TMss[p][rows, 128:192], rhs=ABKs[p][rows, 192:256], start=False, stop=True), ["x_TMs%d" % p, "x_ABK%d" % p], [kb(p)])
                for p in PR:
                    A(lambda e, p=p: e.copy(out=ymix[:, p, cs], in_=pw[p][:, 256:320]), [kb(p)], ["x_ymix%d" % p])
                for p in PR:
                    for rows in HH:
                        P(lambda e, rows=rows, p=p: e.matmul(pw[p][rows, 320:384], lhsT=TMss[p][rows, 0:64], rhs=Usbs[p][rows, :], start=True, stop=False), ["x_TMs%d" % p, "x_Usb%d" % p], [kb(p)])
                        P(lambda e, rows=rows, p=p: e.matmul(pw[p][rows, 320:384], lhsT=TMss[p][rows, 64:128], rhs=TMss[p][rows, 128:192], start=False, stop=True), ["x_TMs%d" % p], [kb(p)])
                for p in PR:
                    V(lambda e, p=p: e.tensor_tensor(out=tSs[p][:], in0=Wst[:, p, :], in1=pw[p][:, 320:384], op=ALU.add), ["x_Wst%d" % p, kb(p)], ["x_tS%d" % p])
                    V(lambda e, p=p: e.tensor_scalar(out=Wst[:, p, :], in0=tSs[p][:], scalar1=dSr[:, p, c:c + 1], scalar2=None, op0=ALU.mult), ["x_tS%d" % p, "x_dSr"], ["x_Wst%d" % p])
                    A(lambda e, p=p: e.copy(out=Wbf[:, p, :], in_=Wst[:, p, :]), ["x_Wst%d" % p], ["x_Wbf%d" % p])
            for p in range(4):
                pc = slice(p * 128, p * 128 + 128)
                ta = f32t[2 * (p % 2)]; tak = "x_f%d" % (2 * (p % 2)); tb_ = f32t[2 * (p % 2) + 1]; tbk = "x_f%d" % (2 * (p % 2) + 1)
                pe_ = pw[p]; PEK = ["x_pw%d" % p]
                OK_ = "x_ymix%d" % p
                P(lambda e, p=p, pe_=pe_: e.matmul(pe_[:], lhsT=g.blk[:], rhs=ymix[:, p, :], start=True, stop=True), ["c_blk", OK_], PEK)
                V(lambda e, p=p, pe_=pe_, ta=ta: e.scalar_tensor_tensor(out=ta[:], in0=pe_[:], scalar=-1.0 / 64, in1=ymix[:, p, :], op0=ALU.mult, op1=ALU.add), PEK + [OK_], [tak])
                A(lambda e, ta=ta, tb_=tb_: e.activation(out=tb_[:], in_=ta[:], func=AF.Square), [tak], [tbk])
                P(lambda e, pe_=pe_, tb_=tb_: e.matmul(pe_[:], lhsT=g.blk[:], rhs=tb_[:], start=True, stop=True), ["c_blk", tbk], PEK)
                V(lambda e, pe_=pe_, tb_=tb_: e.tensor_scalar(out=tb_[:], in0=pe_[:], scalar1=1.0 / 64, scalar2=64e-5, op0=ALU.mult, op1=ALU.add), PEK, [tbk])
                A(lambda e, tb_=tb_: e.activation(out=tb_[:], in_=tb_[:], func=AF.Sqrt), [tbk], [tbk])
                V(lambda e, tb_=tb_: e.reciprocal(out=tb_[:], in_=tb_[:]), [tbk], [tbk])
                V(lambda e, ta=ta, tb_=tb_: e.tensor_tensor(out=ta[:], in0=ta[:], in1=tb_[:], op=ALU.mult), [tak, tbk], [tak])
                V(lambda e, p=p, ta=ta: e.tensor_scalar(out=ta[:], in0=ta[:], scalar1=c_lw(p), scalar2=c_lb(p), op0=ALU.mult, op1=ALU.add), [tak, CK], [tak])
                V(lambda e, p=p, ta=ta: e.tensor_tensor(out=ta[:], in0=ta[:], in1=bons[p][:], op=ALU.add), [tak, "x_bon%d" % p], [tak])
                P(lambda e, pc=pc, pe_=pe_: e.matmul(pe_[:], lhsT=g2t[:, pc], rhs=lora[1][:], start=True, stop=True), ["x_g2", "x_lora1"], PEK)
                V(lambda e, p=p, ta=ta, pe_=pe_: e.tensor_tensor(out=yT[2][:, p, :], in0=ta[:], in1=pe_[:], op=ALU.mult), [tak] + PEK, ["x_yT2"])

            if dbg is not None and blk == dbg[1]:
                for b in range(3):
                    S.dma("pool", dbg[0][b], yT[b][:], reads=["x_yT%d" % b], writes=["dbg"])

            for b, brn in enumerate((io.br_hg, io.br_ret, io.br_rw)):
                BR, bk = w_get()
                for gh in range(2):
                    W, wk = w_get()
                    for dl in range(4):
                        dc = gh * 4 + dl
                        pzt, pk = proj_fm(W, wk, dl * 128)
                        db = dl % 2
                        sgm = f32t[2 * db]; sgk = "x_f%d" % (2 * db); prd = f32t[2 * db + 1]; prk = "x_f%d" % (2 * db + 1)
                        pbr = pw[db]; pbk = "x_pw%d" % db
                        A(lambda e, pzt=pzt, sgm=sgm: e.activation(out=sgm[:], in_=pzt[:], func=AF.Sigmoid), [pk], [sgk])
                        for k in range(4):
                            P(lambda e, k=k, dc=dc, b=b, BR=BR, pbr=pbr: e.matmul(pbr[:], lhsT=BR[:, k, dc * 128:(dc + 1) * 128], rhs=yT[b][:, k, :], start=(k == 0), stop=(k == 3)),
                              [bk, "x_yT%d" % b], [pbk])
                        ymk = "x_ymix%d" % dc
                        if b == 0:
                            V(lambda e, dc=dc, sgm=sgm, pbr=pbr: e.tensor_tensor(out=ymix[:, dc, :], in0=sgm[:], in1=pbr[:], op=ALU.mult), [sgk, pbk], [ymk])
                        else:
                            V(lambda e, sgm=sgm, pbr=pbr, prd=prd: e.tensor_tensor(out=prd[:], in0=sgm[:], in1=pbr[:], op=ALU.mult), [sgk, pbk], [prk])
                            if b == 1:
                                V(lambda e, dc=dc, prd=prd: e.tensor_tensor(out=ymix[:, dc, :], in0=ymix[:, dc, :], in1=prd[:], op=ALU.add), [ymk, prk], [ymk])
                            else:
                                V(lambda e, dc=dc, prd=prd: e.tensor_tensor(out=ymixb[:, dc, :], in0=ymix[:, dc, :], in1=prd[:], op=ALU.add), [ymk, prk], ["x_ymixb"])
                    if gh == 1:
                        w_rel(3)
            Wo = [w_get() for hh in range(2)]
            for hh in range(2):
                S.dma("sp", f32t[hh][:], io.modrow[l:l + 1, 2 * D + hh * 512:2 * D + (hh + 1) * 512].to_broadcast([128, 512]),
                      reads=["modrow%d" % l], writes=["x_f%d" % hh])
            for i in range(4):
                xb = xt[i % 2]; xk = "x_xt%d" % (i % 2)
                S.dma("sp", xb[:], x_src[t0 + i * 128:t0 + (i + 1) * 128, :], reads=[xs_key], writes=[xk])
                for hh in range(2):
                    pi_ = pzc[0] % 2; pzc[0] += 1
                    for k in range(8):
                        P(lambda e, k=k, i=i, hh=hh, pi_=pi_: e.matmul(pz[pi_][:], lhsT=ymixb[:, k, i * 128:(i + 1) * 128], rhs=Wo[hh][0][:, k, :], start=(k == 0), stop=(k == 7)),
                          ["x_ymixb", Wo[hh][1]], ["x_pz%d" % pi_])
                    hs = slice(hh * 512, hh * 512 + 512)
                    V(lambda e, pi_=pi_, hs=hs, hh=hh: e.tensor_tensor(out=hf[:, hs], in0=pz[pi_][:], in1=f32t[hh][:], op=ALU.mult), ["x_pz%d" % pi_, "x_f%d" % hh], ["x_hf"])
                    V(lambda e, xb=xb, hs=hs: e.tensor_tensor(out=xb[:, hs], in0=xb[:, hs], in1=hf[:, hs], op=ALU.add), ["x_hf", xk], [xk])
                S.dma("sp", x_dst[t0 + i * 128:t0 + (i + 1) * 128, :], xb[:], reads=[xk], writes=[xd_key])
            w_rel(2)
    S.barrier()


_NAMES = ["ada_w", "ada_b", "norm1_g", "norm2_g", "w_in", "hg_lb_table", "hg_norm_w", "rw_mu", "rw_w0", "rw_w2",
          "rw_a0", "rw_a2", "rw_g2", "rw_k_k", "rw_k_a", "rw_ln_w", "rw_ln_b", "br_hg", "br_ret", "br_rw", "w_out",
          "router_g", "router_e", "moe_w1", "moe_w3", "moe_w2"]


def make_in_maps(inputs):
    f = lambda a: np.ascontiguousarray(np.asarray(a, dtype=np.float32))
    shared = {n: f(inputs[n]) for n in _NAMES}
    shared["rw_r_k"] = f(inputs["rw_r_k"]).reshape(NL, 512)
    shared["final_g"] = f(inputs["final_g"]).reshape(1, D)
    x = f(inputs["x"]); c = f(inputs["c"])
    pos = np.ascontiguousarray(np.asarray(inputs["positions"], dtype=np.int32))
    maps = []
    for b in range(8):
        m = dict(shared)
        m["x"] = x[b]; m["c"] = c[b:b + 1]; m["positions"] = pos[b:b + 1]
        maps.append(m)
    return maps


def kernel(**inputs):
    nc = build()
    maps = make_in_maps(inputs)
    res = run_bass_kernel_spmd(nc, maps, core_ids=list(range(8)))
    return np.stack([np.asarray(r["out"], dtype=np.float32) for r in res.results], axis=0)
```

```python
import contextlib
import math
import numpy as np
import concourse.bass as bass
import concourse.mybir as mybir
from concourse.bass_utils import run_bass_kernel_spmd

F32 = mybir.dt.float32
BF16 = mybir.dt.bfloat16
I32 = mybir.dt.int32
AF = mybir.ActivationFunctionType
ALU = mybir.AluOpType
AX = mybir.AxisListType

T = 4096
D = 1024
NL = 2
NE = 32
DE = 512
IN_COLS = 8960
EPS = 1e-6
D1_SEQ = False
PI = math.pi


class Sched:
    def __init__(self, nc, stack):
        self.nc = nc
        self.stack = stack
        self.eng = {"pe": nc.tensor, "act": nc.scalar, "dve": nc.vector, "pool": nc.gpsimd, "sp": nc.sync}
        self.esem = {}
        self.ecount = {}
        for e in self.eng:
            self.esem[e] = stack.enter_context(nc.semaphore("s_" + e))
            self.ecount[e] = 0
        self.sems = dict((id(s), s) for s in self.esem.values())
        self.seen = {e: {} for e in self.eng}
        self.lastw = {}
        self.reads = {}
        self.dsem = {}
        self.dcount = {}
        self.n_inst = 0
        self.n_wait = 0
        self.psum_keys = set()

    @property
    def cur_counts(self):
        return {id(self.esem[e]): (lambda e=e: self.ecount[e]) for e in ("act", "dve", "pe")}

    def _dma_sem(self, key):
        if key not in self.dsem:
            s = self.stack.enter_context(self.nc.semaphore("d%d" % len(self.dsem)))
            self.dsem[key] = s
            self.dcount[key] = 0
            self.sems[id(s)] = s
        return self.dsem[key]

    def _deps(self, e, reads, writes):
        evs = []
        for k in reads:
            w = self.lastw.get(k)
            if w is not None:
                evs.append(w)
        for k in writes:
            w = self.lastw.get(k)
            if w is not None and not (e != "dma" and w[2] == e):
                evs.append(w)
            for r in self.reads.get(k, ()):
                if e != "dma" and r[2] == e:
                    continue
                evs.append(r)
        return evs

    def _wait(self, e, evs):
        seen = self.seen[e]
        best = {}
        for (sid, val, _src) in evs:
            if seen.get(sid, 0) >= val:
                continue
            if best.get(sid, 0) < val:
                best[sid] = val
        for sid, val in best.items():
            if getattr(self, "coarse", False) and sid in self.cur_counts:
                val = max(val, self.cur_counts[sid]())
            self.eng[e].wait_ge(self.sems[sid], val)
            seen[sid] = val
            self.n_wait += 1

    def _record(self, ev, reads, writes):
        for k in reads:
            lst = self.reads.setdefault(k, [])
            lst[:] = [r for r in lst if r[0] != ev[0]]
            lst.append(ev)
        for k in writes:
            self.lastw[k] = ev
            self.reads[k] = []

    def op(self, e, fn, reads=(), writes=()):
        px = [k for k in reads if k in self.psum_keys and k not in writes]
        if px:
            writes = list(writes) + px
        evs = self._deps(e, reads, writes)
        if e == "pe":
            evs = [x for x in evs if x[2] != "pe"]
        self._wait(e, evs)
        inst = fn(self.eng[e])
        self.ecount[e] += 1
        inst.then_inc(self.esem[e], 1)
        ev = (id(self.esem[e]), self.ecount[e], e)
        self._record(ev, reads, writes)
        self.n_inst += 1
        return inst

    def dma(self, q, out, in_, reads=(), writes=(), **kw):
        evs = self._deps("dma", reads, writes)
        self._wait(q, evs)
        key = writes[0]
        s = self._dma_sem(key)
        inst = self.eng[q].dma_start(out=out, in_=in_, **kw)
        self.dcount[key] += 16
        inst.then_inc(s, 16)
        ev = (id(s), self.dcount[key], "dma")
        self._record(ev, reads, writes)
        self.n_inst += 1
        return inst

    def barrier(self):
        evs = [(id(self.esem[e]), self.ecount[e], e) for e in self.eng if self.ecount[e] > 0]
        evs += [(id(self.dsem[k]), self.dcount[k], "dma") for k in self.dsem if self.dcount[k] > 0]
        for e in self.eng:
            self._wait(e, [x for x in evs if x[2] != e or e == "dma"])
        self.nbar = getattr(self, "nbar", 0) + 1
        for e in self.eng:
            s_ = self.stack.enter_context(self.nc.semaphore("s_%s_%d" % (e, self.nbar)))
            self.esem[e] = s_
            self.sems[id(s_)] = s_
            self.ecount[e] = 0
        self.lastw = {}
        self.reads = {}


class Ctx:
    pass


_UID = [0]


def _un(name):
    _UID[0] += 1
    return "%s_u%d" % (name, _UID[0])


def _consts(S, nc, st, g):
    sb = lambda name, shape, dt=F32: st.enter_context(nc.sbuf_tensor(_un(name), list(shape), dt))
    g.ident = sb("ident", [128, 128])
    g.identb = sb("identb", [128, 128], BF16)
    g.ones = sb("ones", [128, 128])
    S.op("pool", lambda e: e.memset(g.ones[:], 1.0), writes=["ones"])
    S.op("pool", lambda e: e.memset(g.ident[:], 1.0), writes=["ident"])
    S.op("pool", lambda e: e.affine_select(out=g.ident[:], in_=g.ident[:], pattern=[[1, 128]],
                                           compare_op=ALU.is_equal, fill=0.0, base=0, channel_multiplier=-1),
         reads=["ident"], writes=["ident"])
    S.op("dve", lambda e: e.tensor_copy(out=g.identb[:], in_=g.ident[:]), reads=["ident"], writes=["identb"])
    g.eps = sb("epsc", [128, 1])
    S.op("pool", lambda e: e.memset(g.eps[:], EPS), writes=["epsc"])


def _rms_rstd(S, g, xt, xkey, junk, jkey, ss, rstd, tag):
    S.op("act", lambda e: e.activation(out=junk, in_=xt, func=AF.Square, accum_out=ss),
         reads=[xkey], writes=[jkey, tag + "ss"])
    S.op("dve", lambda e: e.tensor_scalar(out=ss, in0=ss, scalar1=1.0 / D, scalar2=EPS, op0=ALU.mult, op1=ALU.add),
         reads=[tag + "ss"], writes=[tag + "ss"])
    S.op("act", lambda e: e.activation(out=ss, in_=ss, func=AF.Sqrt), reads=[tag + "ss"], writes=[tag + "ss"])
    S.op("dve", lambda e: e.reciprocal(out=rstd, in_=ss), reads=[tag + "ss"], writes=[tag + "rstd"])


def _phase0(S, nc, g, io):
    with contextlib.ExitStack() as st:
        sb = lambda name, shape, dt=F32: st.enter_context(nc.sbuf_tensor(_un(name), list(shape), dt))
        cc = sb("p0_c", [128, 8])
        aw = [sb("p0_aw%d" % i, [128, 8, 512]) for i in range(2)]
        ab = sb("p0_ab", [1, 6 * D])
        row = sb("p0_row", [1, 6 * D])
        ps = [st.enter_context(nc.psum_tensor(_un("p0_ps%d" % i), [128, 512], F32)) for i in range(2)]
        S.psum_keys.update(["p0_ps0", "p0_ps1"])
        S.dma("sp", cc[:], io.c.rearrange("o (k p) -> p (o k)", p=128), writes=["p0_c"], allow_slow_non_contiguous=True)
        S.op("act", lambda e: e.activation(out=cc[:], in_=cc[:], func=AF.Silu), reads=["p0_c"], writes=["p0_c"])
        for l in range(NL):
            S.dma("sp", ab[:], io.ada_b[l:l + 1, :], writes=["p0_ab"])
            for nb in range(12):
                b = nb % 2
                S.dma("sp", aw[b][:], io.ada_w[l, :, nb * 512:(nb + 1) * 512].rearrange("(k p) n -> p k n", p=128),
                      writes=["p0_aw%d" % b])
                for k in range(8):
                    S.op("pe", lambda e, k=k, b=b: e.matmul(ps[b][0:1, :], lhsT=cc[:, k:k + 1], rhs=aw[b][:, k, :],
                                                           start=(k == 0), stop=(k == 7)),
                         reads=["p0_c", "p0_aw%d" % b], writes=["p0_ps%d" % b])
                S.op("dve", lambda e, b=b, nb=nb: e.tensor_tensor(out=row[0:1, nb * 512:(nb + 1) * 512], in0=ps[b][0:1, :],
                                                                 in1=ab[0:1, nb * 512:(nb + 1) * 512], op=ALU.add),
                     reads=["p0_ps%d" % b, "p0_ab"], writes=["p0_row"])
            S.dma("sp", io.modrow[l:l + 1, :], row[:], reads=["p0_row"], writes=["modrow%d" % l])
    S.barrier()


def _load_mod(S, nc, g, io, l, which, gain_ap, A, Akey, B, Bkey, G, Gkey, tmp, tkey):
    o = 0 if which == 1 else 3
    mr = io.modrow
    S.dma("sp", B, mr[l:l + 1, (o + 0) * D:(o + 1) * D].to_broadcast([128, D]), reads=["modrow%d" % l], writes=[Bkey])
    S.dma("sp", A, mr[l:l + 1, (o + 1) * D:(o + 2) * D].to_broadcast([128, D]), reads=["modrow%d" % l], writes=[Akey])
    if G is not None:
        S.dma("sp", G, mr[l:l + 1, (o + 2) * D:(o + 3) * D].to_broadcast([128, D]), reads=["modrow%d" % l], writes=[Gkey])
    S.dma("sp", tmp, gain_ap.to_broadcast([128, D]), writes=[tkey])
    S.op("dve", lambda e: e.scalar_tensor_tensor(out=A, in0=A, scalar=1.0, in1=tmp, op0=ALU.add, op1=ALU.mult),
         reads=[Akey, tkey], writes=[Akey])


def _moe_phase(S, nc, g, io, l, x_src, xs_key, x_dst, xd_key, final, pre=None):
    NPASS = 2
    TP = T // NPASS
    NT = TP // 128
    with contextlib.ExitStack() as st:
        sb = lambda name, shape, dt=F32: st.enter_context(nc.sbuf_tensor(_un(name), list(shape), dt))
        pst = lambda name, shape, dt=F32: (S.psum_keys.add(name), st.enter_context(nc.psum_tensor(_un(name), list(shape), dt)))[1]
        A2 = sb("m_A2", [128, D]); B2 = sb("m_B2", [128, D]); G2 = sb("m_G2", [128, D])
        FG = sb("m_FG", [128, D])
        h2T = sb("m_h2T", [128, 8, TP], BF16)
        yacc = sb("m_yacc", [128, NT, D])
        combH = sb("m_combH", [32, TP], BF16); combL = sb("m_combL", [32, TP], BF16)
        Wr = sb("m_Wr", [128, 8, 36])
        xt = [sb("m_xt%d" % i, [128, D]) for i in range(2)]
        hfs = [sb("m_hf%d" % i, [128, D]) for i in range(2)]
        hTfs = [sb("m_hTf%d" % i, [128, 8, 128]) for i in range(2)]
        smalls = [sb("m_small%d" % i, [128, 160]) for i in range(2)]
        sss = [sb("m%d_ss" % i, [128, 1]) for i in range(2)]; rstds = [sb("m%d_rstd" % i, [128, 1]) for i in range(2)]
        hf = hfs[0]; ss = sss[0]; rstd = rstds[0]
        w1 = [sb("m_w1_%d" % i, [128, 8, DE], BF16) for i in range(2)]
        w3 = [sb("m_w3_%d" % i, [128, 8, DE], BF16) for i in range(2)]
        w2 = [sb("m_w2_%d" % i, [128, 4, D], BF16) for i in range(2)]
        sil = [sb("m_sil%d" % i, [128, 512], BF16) for i in range(2)]
        tmp = [sb("m_tmp%d" % i, [128, 512], BF16) for i in range(2)]
        actT = [sb("m_act%d" % i, [128, 4, 512], BF16) for i in range(2)]
        p_h1 = [pst("m_ph1_%d" % i, [128, 512]) for i in range(2)]
        p_h3 = [pst("m_ph3_%d" % i, [128, 512]) for i in range(2)]
        p_cb = pst("m_pcb", [128, 512])
        p_y = [pst("m_py%d" % i, [128, 512]) for i in range(2)]
        p_tb = pst("m_ptb", [128, 8, 128], BF16)
        p_cbs = [(p_cb[:], "m_pcb"), (p_tb[:].rearrange("p a b -> p (a b)").bitcast(F32), "m_ptb")]

        _load_mod(S, nc, g, io, l, 2, io.norm2_g[l:l + 1, :], A2[:], "m_A2", B2[:], "m_B2", G2[:], "m_G2",
                  hf[:], "m_hf0")
        if final:
            S.dma("sp", FG[:], io.final_g.to_broadcast([128, D]), writes=["m_FG"])
        S.dma("sp", Wr[:, :, 0:4], io.router_g[l].rearrange("(k p) n -> p k n", p=128), writes=["m_Wr"])
        S.dma("sp", Wr[:, :, 4:36], io.router_e[l].rearrange("(k p) n -> p k n", p=128), writes=["m_Wr"])

        def load_expert(e):
            b = e % 2
            S.dma("pool", w1[b][:], io.moe_w1[l, e].rearrange("(k p) n -> p k n", p=128), writes=["m_w1_%d" % b])
            S.dma("pool", w3[b][:], io.moe_w3[l, e].rearrange("(k p) n -> p k n", p=128), writes=["m_w3_%d" % b])
            S.dma("pool", w2[b][:], io.moe_w2[l, e].rearrange("(k p) n -> p k n", p=128), writes=["m_w2_%d" % b])

        pre = list(pre) if pre is not None else []
        for ps_ in range(NPASS):
            t0 = ps_ * TP
            def d1_tile(i, par):
                xb = xt[par]; xk = "m_xt%d" % par
                hf_ = hfs[par]; hfk = "m_hf%d" % par
                hTf_ = hTfs[par]; hTk = "m_hTf%d" % par
                small = smalls[par]; K = "m_small%d" % par
                ss_ = sss[par]; rstd_ = rstds[par]; tg = "m%d_" % par
                py = p_y[par]; pyk = "m_py%d" % par
                plg, plk = (p_cb, "m_pcb") if par == 0 else (p_h1[0], "m_ph1_0")
                S.dma("sp", xb[:], x_src[t0 + i * 128:t0 + (i + 1) * 128, :], reads=[xs_key], writes=[xk])
                _rms_rstd(S, g, xb[:], xk, hf_[:], hfk, ss_[:], rstd_[:], tg)
                yield
                S.op("dve", lambda e: e.scalar_tensor_tensor(out=hf_[:], in0=xb[:], scalar=rstd_[:], in1=A2[:],
                                                             op0=ALU.mult, op1=ALU.mult),
                     reads=[xk, tg + "rstd", "m_A2"], writes=[hfk])
                S.op("dve", lambda e: e.tensor_tensor(out=hf_[:], in0=hf_[:], in1=B2[:], op=ALU.add),
                     reads=[hfk, "m_B2"], writes=[hfk])
                yield
                for hh in range(2):
                    for k in range(4):
                        kk = hh * 4 + k
                        S.op("pe", lambda e, k=k, kk=kk: e.transpose(py[:, k * 128:(k + 1) * 128],
                                                                     hf_[:, kk * 128:(kk + 1) * 128], g.ident[:]),
                             reads=[hfk, "ident"], writes=[pyk])
                    S.op("dve", lambda e, hh=hh: e.tensor_copy(out=hTf_[:, hh * 4:(hh + 1) * 4, :], in_=py[:]),
                         reads=[pyk], writes=[hTk])
                    yield
                S.op("act", lambda e: e.copy(out=h2T[:, :, i * 128:(i + 1) * 128], in_=hTf_[:]),
                     reads=[hTk], writes=["m_h2T"])
                for k in range(8):
                    S.op("pe", lambda e, k=k: e.matmul(plg[:, 0:36], lhsT=hTf_[:, k, :], rhs=Wr[:, k, :],
                                                       start=(k == 0), stop=(k == 7)),
                         reads=[hTk, "m_Wr"], writes=[plk])
                yield
                Lg = small[:, 0:36]
                gmax = small[:, 36:37]; oh = small[:, 40:44]; eg = small[:, 44:48]; sumg = small[:, 48:49]
                gw = small[:, 49:50]; esel = small[:, 52:60]; top8 = small[:, 60:68]; negm1 = small[:, 68:69]
                p2 = small[:, 69:70]; den = small[:, 70:71]; w1g = small[:, 71:72]; w2g = small[:, 72:73]
                c1 = small[:, 76:84]; c2 = small[:, 84:92]; comb = small[:, 96:128]; ngmax = small[:, 37:38]
                vop = lambda fn: S.op("dve", fn, reads=[K], writes=[K])
                S.op("dve", lambda e: e.tensor_copy(out=Lg, in_=plg[:, 0:36]), reads=[plk], writes=[K])
                vop(lambda e: e.tensor_reduce(out=gmax, in_=small[:, 0:4], axis=AX.X, op=ALU.max))
                vop(lambda e: e.tensor_scalar(out=oh, in0=small[:, 0:4], scalar1=gmax, scalar2=None, op0=ALU.is_equal))
                vop(lambda e: e.tensor_scalar(out=ngmax, in0=gmax, scalar1=-1.0, scalar2=None, op0=ALU.mult))
                yield
                S.op("act", lambda e: e.activation(out=eg, in_=small[:, 0:4], func=AF.Exp, bias=ngmax, accum_out=sumg),
                     reads=[K], writes=[K])
                vop(lambda e: e.reciprocal(out=gw, in_=sumg))
                vop(lambda e: e.tensor_scalar(out=esel, in0=small[:, 4:12], scalar1=small[:, 40:41], scalar2=None,
                                              op0=ALU.mult))
                yield
                for gi in range(1, 4):
                    vop(lambda e, gi=gi: e.scalar_tensor_tensor(out=esel, in0=small[:, 4 + 8 * gi:12 + 8 * gi],
                                                               scalar=small[:, 40 + gi:41 + gi], in1=esel,
                                                               op0=ALU.mult, op1=ALU.add))
                    yield
                vop(lambda e: e.max(out=top8, in_=esel))
                vop(lambda e: e.tensor_scalar(out=negm1, in0=small[:, 60:61], scalar1=-1.0, scalar2=None, op0=ALU.mult))
                yield
                S.op("act", lambda e: e.activation(out=p2, in_=small[:, 61:62], func=AF.Exp, bias=negm1),
                     reads=[K], writes=[K])
                vop(lambda e: e.tensor_scalar(out=den, in0=p2, scalar1=1.0, scalar2=None, op0=ALU.add))
                yield
                vop(lambda e: e.reciprocal(out=den, in_=den))
                yield
                vop(lambda e: e.tensor_tensor(out=w1g, in0=den, in1=gw, op=ALU.mult))
                yield
                vop(lambda e: e.tensor_tensor(out=w2g, in0=w1g, in1=p2, op=ALU.mult))
                vop(lambda e: e.tensor_scalar(out=c1, in0=esel, scalar1=small[:, 60:61], scalar2=w1g,
                                              op0=ALU.is_equal, op1=ALU.mult))
                yield
                vop(lambda e: e.tensor_scalar(out=c2, in0=esel, scalar1=small[:, 61:62], scalar2=w2g,
                                              op0=ALU.is_equal, op1=ALU.mult))
                yield
                vop(lambda e: e.tensor_tensor(out=c1, in0=c1, in1=c2, op=ALU.add))
                yield
                for gi in range(4):
                    vop(lambda e, gi=gi: e.tensor_scalar(out=small[:, 96 + 8 * gi:104 + 8 * gi], in0=c1,
                                                        scalar1=small[:, 40 + gi:41 + gi], scalar2=None, op0=ALU.mult))
                yield
                S.op("pe", lambda e: e.transpose(plg[0:32, 128:256], comb, g.ident[:]),
                     reads=[K, "ident"], writes=[plk])
                S.op("dve", lambda e: e.tensor_copy(out=combH[:, i * 128:(i + 1) * 128], in_=plg[0:32, 128:256]),
                     reads=[plk], writes=["m_combH"])
                S.op("dve", lambda e: e.tensor_tensor(out=combL[:, i * 128:(i + 1) * 128], in0=plg[0:32, 128:256],
                                                      in1=combH[:, i * 128:(i + 1) * 128], op=ALU.subtract),
                     reads=[plk, "m_combH"], writes=["m_combL"])

            for i in range(0, NT, 2):
                gens = [d1_tile(i, 0), d1_tile(i + 1, 1)]
                if D1_SEQ:
                    for gn in gens:
                        for _ in gn:
                            pass
                    gens = []
                while gens:
                    for gn in list(gens):
                        try:
                            next(gn)
                        except StopIteration:
                            gens.remove(gn)
            units = [(ex, blk) for ex in range(NE) for blk in range(TP // 512)]

            def front(ex, blk, it, fcs=range(4)):
                b = ex % 2; c0 = blk * 512; ab = it % 2
                pcb, pck = p_cbs[it % 2]
                wk = ["m_w1_%d" % b, "m_w3_%d" % b]
                if 0 in fcs:
                    S.op("pe", lambda e: e.matmul(pcb, lhsT=g.identb[0:32, ex:ex + 1].to_broadcast([32, 128]),
                                                  rhs=combH[:, c0:c0 + 512], start=True, stop=False),
                         reads=["identb", "m_combH"], writes=[pck])
                    S.op("pe", lambda e: e.matmul(pcb, lhsT=g.identb[0:32, ex:ex + 1].to_broadcast([32, 128]),
                                                  rhs=combL[:, c0:c0 + 512], start=False, stop=True),
                         reads=["identb", "m_combL"], writes=[pck])
                for fc in fcs:
                    pb = fc % 2
                    for k in range(8):
                        S.op("pe", lambda e, k=k, fc=fc, pb=pb: e.matmul(
                            p_h1[pb][:], lhsT=w1[b][:, k, fc * 128:(fc + 1) * 128], rhs=h2T[:, k, c0:c0 + 512],
                            start=(k == 0), stop=(k == 7)), reads=[wk[0], "m_h2T"], writes=["m_ph1_%d" % pb])
                    for k in range(8):
                        S.op("pe", lambda e, k=k, fc=fc, pb=pb: e.matmul(
                            p_h3[pb][:], lhsT=w3[b][:, k, fc * 128:(fc + 1) * 128], rhs=h2T[:, k, c0:c0 + 512],
                            start=(k == 0), stop=(k == 7)), reads=[wk[1], "m_h2T"], writes=["m_ph3_%d" % pb])
                    S.op("act", lambda e, pb=pb: e.activation(out=sil[pb][:], in_=p_h1[pb][:], func=AF.Silu),
                         reads=["m_ph1_%d" % pb], writes=["m_sil%d" % pb])
                    S.op("dve", lambda e, pb=pb: e.tensor_tensor(out=tmp[pb][:], in0=sil[pb][:], in1=p_h3[pb][:], op=ALU.mult),
                         reads=["m_sil%d" % pb, "m_ph3_%d" % pb], writes=["m_tmp%d" % pb])
                    S.op("dve", lambda e, pb=pb, fc=fc: e.tensor_tensor(out=actT[ab][:, fc, :], in0=tmp[pb][:], in1=pcb, op=ALU.mult),
                         reads=["m_tmp%d" % pb, pck], writes=["m_act%d" % ab])

            def back(ex, blk, it, tls=range(4)):
                b = ex % 2; ab = it % 2
                for tl in tls:
                    ti = blk * 4 + tl
                    for hh in range(2):
                        for fc in range(4):
                            S.op("pe", lambda e, fc=fc, tl=tl, hh=hh: e.matmul(
                                p_y[hh][:], lhsT=actT[ab][:, fc, tl * 128:(tl + 1) * 128],
                                rhs=w2[b][:, fc, hh * 512:(hh + 1) * 512], start=(fc == 0), stop=(fc == 3)),
                                 reads=["m_act%d" % ab, "m_w2_%d" % b], writes=["m_py%d" % hh])
                        ya = yacc[:, ti, hh * 512:(hh + 1) * 512]
                        yk = "m_yacc%d" % ti
                        if ex == 0:
                            S.op("dve", lambda e, ya=ya, hh=hh: e.tensor_copy(out=ya, in_=p_y[hh][:]),
                                 reads=["m_py%d" % hh], writes=[yk])
                        else:
                            S.op("dve", lambda e, ya=ya, hh=hh: e.tensor_tensor(out=ya, in0=ya, in1=p_y[hh][:], op=ALU.add),
                                 reads=["m_py%d" % hh, yk], writes=[yk])

            load_expert(0)
            load_expert(1)
            for it, (ex, blk) in enumerate(units):
                for st_ in range(4):
                    front(ex, blk, it, fcs=[st_])
                    if it > 0:
                        back(units[it - 1][0], units[it - 1][1], it - 1, tls=[st_])
                if it > 0:
                    pex, pblk = units[it - 1]
                    if pblk == TP // 512 - 1 and pex + 2 < NE:
                        load_expert(pex + 2)
                        if pre:
                            pre.pop(0)()
            back(units[-1][0], units[-1][1], len(units) - 1)
            while pre:
                pre.pop(0)()
            S.coarse = False
            for i in range(NT):
                xb = xt[i % 2]; xk = "m_xt%d" % (i % 2)
                S.dma("sp", xb[:], x_src[t0 + i * 128:t0 + (i + 1) * 128, :], reads=[xs_key], writes=[xk])
                S.op("dve", lambda e, i=i: e.tensor_tensor(out=hf[:], in0=yacc[:, i, :], in1=G2[:], op=ALU.mult),
                     reads=["m_yacc%d" % i, "m_G2"], writes=["m_hf0"])
                S.op("dve", lambda e, xb=xb: e.tensor_tensor(out=xb[:], in0=xb[:], in1=hf[:], op=ALU.add),
                     reads=["m_hf0", xk], writes=[xk])
                if final:
                    _rms_rstd(S, g, xb[:], xk, hf[:], "m_hf0", ss[:], rstd[:], "m0_")
                    S.op("dve", lambda e, xb=xb: e.scalar_tensor_tensor(out=xb[:], in0=xb[:], scalar=rstd[:], in1=FG[:],
                                                                        op0=ALU.mult, op1=ALU.mult),
                         reads=[xk, "m0_rstd", "m_FG"], writes=[xk])
                S.dma("sp", x_dst[t0 + i * 128:t0 + (i + 1) * 128, :], xb[:], reads=[xk], writes=[xd_key])
    S.barrier()


def build(mixer=True, nlayers=NL, moe=True, nblocks=8, dbg_blk=None):
    nc = bass.Bass("TRN2", target_bir_lowering=False)
    io = Ctx()
    din = lambda name, shape, dt=F32: nc.dram_tensor(name, list(shape), dt, kind="ExternalInput").ap()
    io.x = din("x", [T, D]); io.c = din("c", [1, D]); io.pos = din("positions", [1, T], I32)
    io.ada_w = din("ada_w", [NL, D, 6 * D]); io.ada_b = din("ada_b", [NL, 6 * D])
    io.norm1_g = din("norm1_g", [NL, D]); io.norm2_g = din("norm2_g", [NL, D])
    io.w_in = din("w_in", [NL, D, IN_COLS])
    io.hg_lb_table = din("hg_lb_table", [NL, 512]); io.hg_norm_w = din("hg_norm_w", [NL, 128])
    io.rw_mu = din("rw_mu", [NL, 1792]); io.rw_w0 = din("rw_w0", [NL, 512]); io.rw_w2 = din("rw_w2", [NL, 64, 512])
    io.rw_a0 = din("rw_a0", [NL, 512]); io.rw_a2 = din("rw_a2", [NL, 64, 512]); io.rw_g2 = din("rw_g2", [NL, 128, 512])
    io.rw_k_k = din("rw_k_k", [NL, 512]); io.rw_k_a = din("rw_k_a", [NL, 512]); io.rw_r_k = din("rw_r_k", [NL, 512])
    io.rw_ln_w = din("rw_ln_w", [NL, 512]); io.rw_ln_b = din("rw_ln_b", [NL, 512])
    io.br_hg = din("br_hg", [NL, 512, D]); io.br_ret = din("br_ret", [NL, 512, D]); io.br_rw = din("br_rw", [NL, 512, D])
    io.w_out = din("w_out", [NL, D, D])
    io.router_g = din("router_g", [NL, D, 4]); io.router_e = din("router_e", [NL, D, 32])
    io.moe_w1 = din("moe_w1", [NL, NE, D, DE]); io.moe_w3 = din("moe_w3", [NL, NE, D, DE])
    io.moe_w2 = din("moe_w2", [NL, NE, DE, D])
    io.final_g = din("final_g", [1, D])
    io.out = nc.dram_tensor("out", [T, D], F32, kind="ExternalOutput").ap()
    io.modrow = nc.dram_tensor("modrow", [NL, 6 * D], F32, kind="Internal").ap()
    io.xa = nc.dram_tensor("xa", [T, D], F32, kind="Internal").ap()
    io.xb = nc.dram_tensor("xb", [T, D], F32, kind="Internal").ap()
    io.wbf = nc.dram_tensor("wbf", [NL, 23, 128, 4096], BF16, kind="Internal").ap()
    g = Ctx()
    with contextlib.ExitStack() as st:
        S = Sched(nc, st)
        _consts(S, nc, st, g)
        dbg = None
        if dbg_blk is not None:
            dout = nc.dram_tensor("dbg_y", [3, 128, 4, 512], F32, kind="ExternalOutput").ap()
            dbg = (dout, dbg_blk)
        if mixer:
            for f_ in _precast_mixer_weights(S, io, 0):
                f_()
        _phase0(S, nc, g, io)
        cur, ck = io.x, "x_in"
        for l in range(nlayers):
            last = (l == nlayers - 1)
            if mixer:
                mdst, mk = (io.out, "out") if (last and not moe) else (io.xa, "xa")
                _mixer_phase(S, nc, g, io, l, cur, ck, mdst, mk, nblocks=nblocks, dbg=dbg if l == 0 else None)
                cur, ck = mdst, mk
            if moe:
                dst, dk = (io.out, "out") if last else (io.xb, "xb")
                pre = _precast_mixer_weights(S, io, l + 1) if (mixer and not last) else None
                _moe_phase(S, nc, g, io, l, cur, ck, dst, dk, final=last, pre=pre)
                cur, ck = dst, dk
        S.barrier()
        print("instructions", S.n_inst, "waits", S.n_wait, "dma sems", len(S.dsem), "barriers", getattr(S, "nbar", 0))
    return nc


def _mixer_consts(S, nc, st, g):
    sb = lambda name, shape, dt=F32: st.enter_context(nc.sbuf_tensor(_un(name), list(shape), dt))
    g.blk = sb("c_blk", [128, 128])
    S.op("pool", lambda e: e.memset(g.blk[:], 0.0), writes=["c_blk"])
    S.op("pool", lambda e: e.memset(g.blk[0:64, 0:64], 1.0), reads=["c_blk"], writes=["c_blk"])
    S.op("pool", lambda e: e.memset(g.blk[64:128, 64:128], 1.0), reads=["c_blk"], writes=["c_blk"])
    g.mge = sb("c_mge", [128, 64])
    g.m4 = sb("c_m4", [128, 256])
    S.op("pool", lambda e: e.memset(g.mge[:], 1.0), writes=["c_mge"])
    S.op("pool", lambda e: e.memset(g.m4[:], 1.0), writes=["c_m4"])
    for hh in range(2):
        rows = slice(hh * 64, hh * 64 + 64)
        S.op("pool", lambda e, rows=rows: e.affine_select(out=g.mge[rows, :], in_=g.mge[rows, :], pattern=[[1, 64]],
                                                          compare_op=ALU.is_ge, fill=0.0, base=0, channel_multiplier=-1),
             reads=["c_mge"], writes=["c_mge"])
        for q in range(4):
            op = ALU.is_gt if q % 2 == 0 else ALU.is_ge
            S.op("pool", lambda e, rows=rows, q=q, op=op: e.affine_select(
                out=g.m4[rows, q * 64:(q + 1) * 64], in_=g.m4[rows, q * 64:(q + 1) * 64], pattern=[[1, 64]],
                compare_op=op, fill=0.0, base=0, channel_multiplier=-1), reads=["c_m4"], writes=["c_m4"])
    g.blkb = sb("c_blkb", [128, 128], BF16); g.onesb = sb("c_onesb", [128, 128], BF16)
    S.op("dve", lambda e: e.tensor_copy(out=g.blkb[:], in_=g.blk[:]), reads=["c_blk"], writes=["c_blkb"])
    S.op("dve", lambda e: e.tensor_copy(out=g.onesb[:], in_=g.ones[:]), reads=["ones"], writes=["c_onesb"])
    g.mgei = sb("c_mgei", [128, 64], I32)
    S.op("dve", lambda e: e.tensor_copy(out=g.mgei[:], in_=g.mge[:]), reads=["c_mge"], writes=["c_mgei"])
    g.rm = sb("c_rm", [128, 512])
    S.op("pool", lambda e: e.memset(g.rm[:], 1.0), writes=["c_rm"])
    S.op("pool", lambda e: e.memset(g.rm[:].rearrange("p (c j) -> p c j", j=64)[:, :, 0:1], 0.0),
         reads=["c_rm"], writes=["c_rm"])
    g.J = sb("c_J", [128, 128]); g.Ji = sb("c_Ji", [128, 128], I32)
    g.DF = sb("c_DF", [128, 128])
    S.op("pool", lambda e: e.iota(g.Ji[:], pattern=[[1, 128]], base=0, channel_multiplier=0), writes=["c_Ji"])
    S.op("dve", lambda e: e.tensor_copy(out=g.J[:], in_=g.Ji[:]), reads=["c_Ji"], writes=["c_J"])
    S.op("pool", lambda e: e.iota(g.Ji[:], pattern=[[1, 128]], base=0, channel_multiplier=-1), reads=["c_Ji"], writes=["c_Ji"])
    S.op("dve", lambda e: e.tensor_copy(out=g.DF[:], in_=g.Ji[:]), reads=["c_Ji"], writes=["c_DF"])
    g.XI = sb("c_XI", [128, 4, 128]); g.ZE = sb("c_ZE", [128, 4, 128]); g.DM = sb("c_DM", [128, 4, 128])
    g.lng = [math.log(1.0 - 2.0 ** (-5.0 - h)) for h in range(4)]
    g.cb = sb("c_cb", [128, 16])
    for h in range(4):
        lg = g.lng[h]
        S.op("pool", lambda e, h=h, lg=lg: e.memset(g.cb[:, h:h + 1], lg), reads=["c_cb"], writes=["c_cb"])
        S.op("pool", lambda e, h=h, lg=lg: e.memset(g.cb[:, 4 + h:5 + h], 127.0 * lg), reads=["c_cb"], writes=["c_cb"])
    for h in range(4):
        lg = g.lng[h]
        S.op("act", lambda e, h=h, lg=lg: e.activation(out=g.XI[:, h, :], in_=g.J[:], func=AF.Exp, scale=lg, bias=g.cb[:, h:h + 1]),
             reads=["c_J", "c_cb"], writes=["c_XI"])
        S.op("act", lambda e, h=h, lg=lg: e.activation(out=g.ZE[:, h, :], in_=g.J[:], func=AF.Exp, scale=-lg, bias=g.cb[:, 4 + h:5 + h]),
             reads=["c_J", "c_cb"], writes=["c_ZE"])
        S.op("act", lambda e, h=h, lg=lg: e.activation(out=g.DM[:, h, :], in_=g.DF[:], func=AF.Exp, scale=lg),
             reads=["c_DF"], writes=["c_DM"])
        S.op("pool", lambda e, h=h: e.affine_select(out=g.DM[:, h, :], in_=g.DM[:, h, :], pattern=[[1, 128]],
                                                    compare_op=ALU.is_ge, fill=0.0, base=0, channel_multiplier=-1),
             reads=["c_DM"], writes=["c_DM"])
    g.invf = sb("c_invf", [128, 1]); g.pm = sb("c_pm", [128, 1], I32); g.sgn = sb("c_sgn", [128, 1])
    for hh in range(2):
        rows = slice(hh * 64, hh * 64 + 64)
        S.op("pool", lambda e, rows=rows: e.iota(g.pm[rows, :], pattern=[[0, 1]], base=0, channel_multiplier=1),
             reads=["c_pm"], writes=["c_pm"])
    S.op("dve", lambda e: e.tensor_copy(out=g.invf[:], in_=g.pm[:]), reads=["c_pm"], writes=["c_invf"])
    S.op("act", lambda e: e.activation(out=g.invf[:], in_=g.invf[:], func=AF.Exp, scale=-math.log(10000.0) / 64.0),
         reads=["c_invf"], writes=["c_invf"])
    S.op("pool", lambda e: e.memset(g.sgn[0:64, :], -1.0), reads=["c_sgn"], writes=["c_sgn"])
    S.op("pool", lambda e: e.memset(g.sgn[64:128, :], 1.0), reads=["c_sgn"], writes=["c_sgn"])


def _mixer_wlist(io, l):
    wsrc = lambda c0, n=512: (io.w_in[l, :, c0:c0 + n], 8, n)
    wlist = [wsrc(0), wsrc(512), wsrc(1024), wsrc(1536), wsrc(2048), wsrc(2560), wsrc(3072), wsrc(3584),
             wsrc(4096 + 1536, 256), wsrc(4096), wsrc(4608), wsrc(5120)]
    for b_, brn_ in enumerate((io.br_hg, io.br_ret, io.br_rw)):
        wlist += [(brn_[l], 4, 1024), wsrc(5888 + b_ * 1024), wsrc(5888 + b_ * 1024 + 512)]
    wlist += [(io.w_out[l, :, 0:512], 8, 512), (io.w_out[l, :, 512:1024], 8, 512)]
    return wlist


def _precast_mixer_weights(S, io, l):
    def mk(j, src, kp, ncols):
        def f():
            dst = io.wbf[l, j, :, 0:kp * ncols].rearrange("p (k n) -> p k n", k=kp)
            S.dma("pool", dst, src.rearrange("(k p) n -> p k n", p=128), writes=["wbf%d" % l])
        return f
    return [mk(j, src, kp, ncols) for j, (src, kp, ncols) in enumerate(_mixer_wlist(io, l))]


def _mixer_phase(S, nc, g, io, l, x_src, xs_key, x_dst, xd_key, nblocks=8, dbg=None):
    BT = 512
    E05 = math.exp(-0.5)
    with contextlib.ExitStack() as st:
        sb = lambda name, shape, dt=F32: st.enter_context(nc.sbuf_tensor(_un(name), list(shape), dt))
        pst_ = lambda name, shape, dt=F32: (S.psum_keys.add(name), st.enter_context(nc.psum_tensor(_un(name), list(shape), dt)))[1]
        V = lambda fn, r, w: S.op("dve", fn, reads=r, writes=w)
        A = lambda fn, r, w: S.op("act", fn, reads=r, writes=w)
        P = lambda fn, r, w: S.op("pe", fn, reads=r, writes=w)
        G = lambda fn, r, w: S.op("pool", fn, reads=r, writes=w)
        _mixer_consts(S, nc, st, g)
        A1 = sb("x_A1", [128, D]); B1 = sb("x_B1", [128, D])
        xt = [sb("x_xt%d" % i, [128, D]) for i in range(2)]
        hf = sb("x_hf", [128, D]); hb = sb("x_hb", [128, D], BF16)
        ss = sb("x_ss", [128, 1]); rstd = sb("x_rstd", [128, 1])
        hT = sb("x_hT", [128, 8, BT], BF16)
        wb = [sb("x_wb%d" % i, [128, 4096], BF16) for i in range(3)]
        yT = [sb("x_yT%d" % i, [128, 4, BT], BF16) for i in range(3)]
        ymix = sb("x_ymix", [128, 8, BT])
        ymixb = sb("x_ymixb", [128, 8, BT], BF16)
        cols = sb("x_cols", [128, 64])
        lbt = sb("x_lbt", [128, 8])
        f32t = [sb("x_f%d" % i, [128, BT]) for i in range(6)]
        f513 = sb("x_f513", [128, BT + 1])
        posi = f32t[5][:].bitcast(I32)
        carry = sb("x_carry", [128, 16])
        bq = [sb("x_bq%d" % i, [128, 4, BT], BF16) for i in range(6)]
        vtm = sb("x_vtm", [128, 4, 512], BF16)
        Sst = sb("x_S", [128, 4, 128]); Sbf = sb("x_Sbf", [128, 4, 128], BF16)
        Rst = sb("x_R", [128, 4, 128]); Rbf = sb("x_Rbf", [128, 4, 128], BF16)
        Wst = sb("x_Wst", [128, 4, 64])
        dS = sb("x_dS", [128, 4, 8]); dSr = sb("x_dSr", [128, 4, 8])
        sc4 = sb("x_sc4", [128, 4, 128], BF16)
        schg = sb("x_schg", [128, 4, 64], BF16)
        ktm = sb("x_ktm", [128, 4, 128], BF16)
        cosT = sb("x_cos", [128, BT]); sinT = sb("x_sin", [128, BT])
        lora = [sb("x_lora0", [128, BT], BF16), sb("x_lora1", [128, BT], BF16)]
        w2a2 = sb("x_w2a2", [128, 512], BF16); g2t = sb("x_g2", [128, 512], BF16)
        rw = [sb("x_rw%d" % i, [128, BT]) for i in range(3)]
        ARs = [sb("x_AR%d" % p, [128, 8, 128], BF16) for p in range(4)]
        bts = [sb("x_bt%d" % p, [128, BT], BF16) for p in range(4)]
        kts = [sb("x_kt%d" % p, [128, BT], BF16) for p in range(4)]
        vbs = [sb("x_vb%d" % p, [128, BT], BF16) for p in range(4)]
        bons = [sb("x_bon%d" % p, [128, BT], BF16) for p in range(4)]
        ABKs = [sb("x_ABK%d" % p, [128, 256], BF16) for p in range(4)]
        MTs = [sb("x_MT%d" % p, [128, 128], BF16) for p in range(4)]
        MNs = [[sb("x_MN%d_%d" % (p, i), [128, 128], BF16) for i in range(2)] for p in range(4)]
        NPs = [[MTs[p], sb("x_NP%d_1" % p, [128, 128], BF16)] for p in range(4)]
        Xs = [sb("x_X%d" % p, [128, 128], BF16) for p in range(4)]
        Wbf = sb("x_Wbf", [128, 4, 64], BF16)
        TMss = [sb("x_TMs%d" % p, [128, 192], BF16) for p in range(4)]
        Wsbs = [sb("x_Wsb%d" % p, [128, 64], BF16) for p in range(4)]
        Usbs = [sb("x_Usb%d" % p, [128, 64], BF16) for p in range(4)]
        tSs = [sb("x_tS%d" % p, [128, 64]) for p in range(4)]
        pz = [pst_("x_pz%d" % i, [128, 512]) for i in range(2)]
        ptb = pst_("x_ptb", [128, 8, 128], BF16)
        pw = [pst_("x_pw%d" % i, [128, 512]) for i in range(4)]
        psc, po, pss = pw[0], pw[1], pw[2]

        _load_mod(S, nc, g, io, l, 1, io.norm1_g[l:l + 1, :], A1[:], "x_A1", B1[:], "x_B1", None, None, hf[:], "x_hf")
        CK = "x_cols"
        colv = lambda v, n: v.rearrange("o (j p) -> p (o j)", p=128)
        nslow = dict(allow_slow_non_contiguous=True)
        S.dma("sp", cols[:, 0:14], colv(io.rw_mu[l:l + 1, :], 14), writes=[CK], **nslow)
        for i, nm in enumerate(["rw_w0", "rw_a0", "rw_k_k", "rw_k_a", "rw_r_k", "rw_ln_w", "rw_ln_b"]):
            S.dma("sp", cols[:, 16 + 4 * i:20 + 4 * i], colv(getattr(io, nm)[l:l + 1, :], 4), reads=[CK], writes=[CK], **nslow)
        c_w0, c_a0, c_kk, c_ka, c_rk, c_lw, c_lb = [lambda p, i=i: cols[:, 16 + 4 * i + p:17 + 4 * i + p] for i in range(7)]
        c_omka = lambda p: cols[:, 44 + p:45 + p]
        V(lambda e: e.tensor_scalar(out=cols[:, 44:48], in0=cols[:, 28:32], scalar1=-1.0, scalar2=1.0, op0=ALU.mult, op1=ALU.add), [CK], [CK])
        S.dma("sp", cols[:, 48:49], io.hg_norm_w[l:l + 1, :].rearrange("o p -> p o"), reads=[CK], writes=[CK], **nslow)
        S.dma("sp", lbt[:, 0:4], colv(io.hg_lb_table[0:1, :], 4), writes=["x_lbt"], **nslow)
        S.dma("sp", lbt[:, 4:8], colv(io.hg_lb_table[1:2, :], 4), reads=["x_lbt"], writes=["x_lbt"], **nslow)
        c_lbv = lambda h: cols[:, 52 + h:53 + h]
        c_oml = lambda h: cols[:, 56 + h:57 + h]
        if l == 0:
            V(lambda e: e.memset(cols[:, 52:56], 0.0), [CK], [CK])
        else:
            V(lambda e: e.tensor_tensor(out=cols[:, 52:56], in0=lbt[:, 4:8], in1=lbt[:, 0:4], op=ALU.subtract), ["x_lbt", CK], [CK])
            A(lambda e: e.activation(out=cols[:, 52:56], in_=cols[:, 52:56], func=AF.Sigmoid), [CK], [CK])
        V(lambda e: e.tensor_scalar(out=cols[:, 56:60], in0=cols[:, 52:56], scalar1=-1.0, scalar2=1.0, op0=ALU.mult, op1=ALU.add), [CK], [CK])
        S.dma("pool", w2a2[0:64, :], io.rw_w2[l], writes=["x_w2a2"])
        S.dma("pool", w2a2[64:128, :], io.rw_a2[l], reads=["x_w2a2"], writes=["x_w2a2"])
        S.dma("pool", g2t[:], io.rw_g2[l], writes=["x_g2"])
        for p in range(4):
            G(lambda e, p=p: e.memset(Wbf[:, p, :], 0.0), [], ["x_Wbf%d" % p])
            G(lambda e, p=p: e.memset(Wst[:, p, :], 0.0), [], ["x_Wst%d" % p])
            G(lambda e, p=p: e.memset(MTs[p][:], 0.0), [], ["x_MT%d" % p])
        G(lambda e: e.memset(carry[:], 0.0), [], ["x_carry"])
        for h_ in range(4):
            G(lambda e, h_=h_: e.memset(Rst[:, h_, :], 0.0), [], ["x_R%d" % h_])
            G(lambda e, h_=h_: e.memset(Rbf[:, h_, :], 0.0), [], ["x_Rbf%d" % h_])
        for h_ in range(4):
            G(lambda e, h_=h_: e.memset(schg[:, h_, :], 0.0), [], ["x_schg%d" % h_])
            G(lambda e, h_=h_: e.memset(Sst[:, h_, :], 0.0), [], ["x_S%d" % h_])
            G(lambda e, h_=h_: e.memset(Sbf[:, h_, :], 0.0), [], ["x_Sbf%d" % h_])

        wlist = _mixer_wlist(io, l)
        NW = len(wlist)
        wst = {"issued": 0, "got": 0, "rel": -1, "total": NW * nblocks}

        def w_issue():
            j = wst["issued"]; wst["issued"] += 1
            src, kparts, ncols = wlist[j % NW]
            i = j % 3
            S.dma("sp", wb[i][:, 0:kparts * ncols], io.wbf[l, j % NW, :, 0:kparts * ncols],
                  reads=["wbf%d" % l], writes=["x_wb%d" % i])

        def w_pump():
            while wst["issued"] < wst["total"] and wst["issued"] <= wst["got"] + 1 and wst["issued"] - 3 <= wst["rel"]:
                w_issue()

        def w_get():
            j = wst["got"]; wst["got"] += 1
            while wst["issued"] <= j:
                assert wst["issued"] - 3 <= wst["rel"], "weight buffer still live"
                w_issue()
            src, kparts, ncols = wlist[j % NW]
            i = j % 3
            view = wb[i][:, 0:kparts * ncols].rearrange("p (k n) -> p k n", k=kparts)
            return view, "x_wb%d" % i

        def w_rel(n=1):
            wst["rel"] += n
            w_pump()

        pzc = [0]

        def proj_fm(W, wk, c0, nc_=128, row0=0, same=False):
            if same:
                i = (pzc[0] - 1) % 2
            else:
                i = pzc[0] % 2
                pzc[0] += 1
            for k in range(8):
                P(lambda e, k=k, i=i: e.matmul(pz[i][row0:row0 + nc_, :], lhsT=W[:, k, c0:c0 + nc_], rhs=hT[:, k, :],
                                               start=(k == 0), stop=(k == 7)), [wk, "x_hT"], ["x_pz%d" % i])
            return pz[i], "x_pz%d" % i

        def wgrp(c0, ncols=512):
            return w_get()

        for blk in range(nblocks):
            t0 = blk * BT
            for i in range(4):
                xb = xt[i % 2]; xk = "x_xt%d" % (i % 2)
                S.dma("sp", xb[:], x_src[t0 + i * 128:t0 + (i + 1) * 128, :], reads=[xs_key], writes=[xk])
                _rms_rstd(S, g, xb[:], xk, hf[:], "x_hf", ss[:], rstd[:], "x_")
                V(lambda e, xb=xb: e.scalar_tensor_tensor(out=hf[:], in0=xb[:], scalar=rstd[:], in1=A1[:], op0=ALU.mult, op1=ALU.mult),
                  [xk, "x_rstd", "x_A1"], ["x_hf"])
                V(lambda e: e.tensor_tensor(out=hb[:], in0=hf[:], in1=B1[:], op=ALU.add), ["x_hf", "x_B1"], ["x_hb"])
                for k in range(8):
                    P(lambda e, k=k: e.transpose(ptb[:, k, :], hb[:, k * 128:(k + 1) * 128], g.identb[:]), ["x_hb", "identb"], ["x_ptb"])
                A(lambda e, i=i: e.copy(out=hT[:, :, i * 128:(i + 1) * 128], in_=ptb[:]), ["x_ptb"], ["x_hT"])

            qsil, qh, qs_, kh, kt_, sgt = bq
            qk = ["x_bq%d" % i for i in range(6)]
            W, wk = wgrp(0)
            for h in range(4):
                pzt, pk = proj_fm(W, wk, h * 128)
                A(lambda e, h=h, pzt=pzt: e.activation(out=qsil[:, h, :], in_=pzt[:], func=AF.Silu), [pk], [qk[0]])
            w_rel()
            W, wk = wgrp(512)
            lf, cum, tmpa, tmpb, kf = f32t[0], f32t[1], f32t[2], f32t[3], f32t[4]
            c3 = lambda tl: tl[:].rearrange("p (c j) -> p c j", j=64)
            for h in range(4):
                pzt, pk = proj_fm(W, wk, h * 128)
                A(lambda e, pzt=pzt: e.activation(out=tmpa[:], in_=pzt[:], func=AF.Sigmoid), [pk], ["x_f2"])
                V(lambda e, h=h: e.tensor_scalar(out=tmpa[:], in0=tmpa[:], scalar1=c_oml(h), scalar2=c_lbv(h), op0=ALU.mult, op1=ALU.add),
                  ["x_f2", CK], ["x_f2"])
                A(lambda e: e.activation(out=lf[:], in_=tmpa[:], func=AF.Ln), ["x_f2"], ["x_f0"])
                V(lambda e: e.tensor_scalar(out=kf[:], in0=tmpa[:], scalar1=-1.0, scalar2=1.0, op0=ALU.mult, op1=ALU.add), ["x_f2"], ["x_f4"])
                V(lambda e: e.tensor_tensor_scan(out=cum[:], data0=g.rm[:], data1=lf[:], initial=0.0, op0=ALU.mult, op1=ALU.add),
                  ["c_rm", "x_f0"], ["x_f1"])
                V(lambda e: e.tensor_tensor(out=c3(tmpa), in0=c3(cum), in1=c3(cum)[:, :, 31:32].to_broadcast([128, 8, 64]), op=ALU.subtract),
                  ["x_f1"], ["x_f2"])
                A(lambda e: e.activation(out=tmpb[:], in_=tmpa[:], func=AF.Exp), ["x_f2"], ["x_f3"])
                V(lambda e, h=h: e.scalar_tensor_tensor(out=qh[:, h, :], in0=qsil[:, h, :], scalar=128.0 ** -0.5, in1=tmpb[:], op0=ALU.mult, op1=ALU.mult),
                  [qk[0], "x_f3"], [qk[1]])
                A(lambda e: e.activation(out=tmpb[:], in_=tmpa[:], func=AF.Exp, scale=-1.0), ["x_f2"], ["x_f3"])
                V(lambda e, h=h: e.tensor_tensor(out=kh[:, h, :], in0=kf[:], in1=tmpb[:], op=ALU.mult), ["x_f4", "x_f3"], [qk[3]])
                A(lambda e: e.activation(out=tmpb[:], in_=cum[:], func=AF.Exp), ["x_f1"], ["x_f3"])
                V(lambda e, h=h: e.scalar_tensor_tensor(out=qs_[:, h, :], in0=qsil[:, h, :], scalar=128.0 ** -0.5, in1=tmpb[:], op0=ALU.mult, op1=ALU.mult),
                  [qk[0], "x_f3"], [qk[2]])
                V(lambda e, h=h: e.tensor_copy(out=dS[:, h, :], in_=c3(tmpb)[:, :, 63]), ["x_f3"], ["x_dS"])
                V(lambda e: e.tensor_tensor(out=c3(tmpa), in0=c3(cum)[:, :, 63:64].to_broadcast([128, 8, 64]), in1=c3(cum), op=ALU.subtract),
                  ["x_f1"], ["x_f2"])
                A(lambda e: e.activation(out=tmpb[:], in_=tmpa[:], func=AF.Exp), ["x_f2"], ["x_f3"])
                V(lambda e, h=h: e.tensor_tensor(out=kt_[:, h, :], in0=kf[:], in1=tmpb[:], op=ALU.mult), ["x_f4", "x_f3"], [qk[4]])
            w_rel()
            W, wk = wgrp(1024)
            for i in range(4):
                pi_ = pzc[0] % 2; pzc[0] += 1
                for k in range(8):
                    P(lambda e, k=k, i=i, pi_=pi_: e.matmul(pz[pi_][:], lhsT=hT[:, k, i * 128:(i + 1) * 128], rhs=W[:, k, :], start=(k == 0), stop=(k == 7)),
                      [wk, "x_hT"], ["x_pz%d" % pi_])
                A(lambda e, i=i, pi_=pi_: e.copy(out=vtm[:, i, :], in_=pz[pi_][:]), ["x_pz%d" % pi_], ["x_vtm"])
            w_rel()
            W, wk = wgrp(1536)
            for h in range(4):
                pzt, pk = proj_fm(W, wk, h * 128)
                A(lambda e, h=h, pzt=pzt: e.activation(out=sgt[:, h, :], in_=pzt[:], func=AF.Silu), [pk], [qk[5]])
            w_rel()
            def rope_gen():
                S.dma("sp", posi, io.pos[0:1, t0:t0 + BT].to_broadcast([128, BT]), writes=["x_f5"])
                ang, rr, nf, mm = f32t[0], f32t[1], f32t[2], f32t[3]
                V(lambda e: e.tensor_copy(out=ang[:], in_=posi), ["x_f5"], ["x_f0"])
                yield
                V(lambda e: e.tensor_scalar(out=ang[:], in0=ang[:], scalar1=g.invf[:], scalar2=None, op0=ALU.mult), ["x_f0", "c_invf"], ["x_f0"])
                yield
                for which in range(2):
                    dst, dk_ = (sinT, "x_sin") if which == 0 else (cosT, "x_cos")
                    off = 0.0 if which == 0 else PI / 2
                    V(lambda e, off=off: e.tensor_scalar(out=rr[:], in0=ang[:], scalar1=off, scalar2=None, op0=ALU.add), ["x_f0"], ["x_f1"])
                    yield
                    V(lambda e: e.tensor_scalar(out=posi, in0=rr[:], scalar1=1.0 / (2 * PI), scalar2=None, op0=ALU.mult), ["x_f1"], ["x_f5"])
                    yield
                    V(lambda e: e.tensor_copy(out=nf[:], in_=posi), ["x_f5"], ["x_f2"])
                    yield
                    V(lambda e: e.scalar_tensor_tensor(out=rr[:], in0=nf[:], scalar=-2 * PI, in1=rr[:], op0=ALU.mult, op1=ALU.add), ["x_f2", "x_f1"], ["x_f1"])
                    yield
                    V(lambda e: e.tensor_scalar(out=mm[:], in0=rr[:], scalar1=PI, scalar2=None, op0=ALU.is_gt), ["x_f1"], ["x_f3"])
                    yield
                    V(lambda e: e.scalar_tensor_tensor(out=rr[:], in0=mm[:], scalar=-2 * PI, in1=rr[:], op0=ALU.mult, op1=ALU.add), ["x_f3", "x_f1"], ["x_f1"])
                    yield
                    V(lambda e: e.tensor_scalar(out=mm[:], in0=rr[:], scalar1=-PI, scalar2=None, op0=ALU.is_lt), ["x_f1"], ["x_f3"])
                    yield
                    V(lambda e: e.scalar_tensor_tensor(out=rr[:], in0=mm[:], scalar=2 * PI, in1=rr[:], op0=ALU.mult, op1=ALU.add), ["x_f3", "x_f1"], ["x_f1"])
                    yield
                    V(lambda e: e.tensor_scalar(out=rr[:], in0=rr[:], scalar1=3.1415925, scalar2=-3.1415925, op0=ALU.min, op1=ALU.max), ["x_f1"], ["x_f1"])
                    yield
                    if which == 0:
                        A(lambda e, dst=dst: e.activation(out=dst[:], in_=rr[:], func=AF.Sin, scale=g.sgn[:]), ["x_f1", "c_sgn"], [dk_])
                        yield
                    else:
                        A(lambda e, dst=dst: e.activation(out=dst[:], in_=rr[:], func=AF.Sin), ["x_f1"], [dk_])
                        yield
                yield

            rg = rope_gen()
            for c in range(8):
                par = c % 2; rows = slice(par * 64, par * 64 + 64); cs = slice(c * 64, c * 64 + 64); tl = c // 2
                for h in range(4):
                    P(lambda e, h=h: e.matmul(pz[0][rows, h * 64:(h + 1) * 64], lhsT=kh[:, h, cs], rhs=qh[:, h, cs], start=True, stop=True),
                      [qk[3], qk[1]], ["x_pz0"])
                for h in range(4):
                    V(lambda e, h=h: e.copy_predicated(out=schg[rows, h, :], mask=g.mgei[rows, :], data=pz[0][rows, h * 64:(h + 1) * 64]),
                      ["x_pz0", "c_mgei"], ["x_schg%d" % h])
                for h in range(4):
                    P(lambda e, h=h: e.matmul(pw[h][:, cs], lhsT=vtm[rows, tl, h * 128:(h + 1) * 128], rhs=schg[rows, h, :], start=True, stop=False),
                      ["x_vtm", "x_schg%d" % h], ["x_pw%d" % h])
                    P(lambda e, h=h: e.matmul(pw[h][:, cs], lhsT=Sbf[:, h, :], rhs=qs_[:, h, cs], start=False, stop=True),
                      ["x_Sbf%d" % h, qk[2]], ["x_pw%d" % h])
                for h in range(4):
                    P(lambda e, h=h: e.transpose(ptb[rows, h, :], kt_[:, h, cs], g.identb[:]), [qk[4], "identb"], ["x_ptb"])
                for h in range(4):
                    A(lambda e, h=h: e.copy(out=ktm[rows, h, :], in_=ptb[rows, h, :]), ["x_ptb"], ["x_ktm%d" % h])
                for h in range(4):
                    P(lambda e, h=h: e.matmul(pz[1][:, h * 128:(h + 1) * 128], lhsT=ktm[rows, h, :], rhs=vtm[rows, tl, h * 128:(h + 1) * 128], start=True, stop=True),
                      ["x_ktm%d" % h, "x_vtm"], ["x_pz1"])
                for h in range(4):
                    V(lambda e, h=h: e.scalar_tensor_tensor(out=Sst[:, h, :], in0=Sst[:, h, :], scalar=dS[:, h, c:c + 1], in1=pz[1][:, h * 128:(h + 1) * 128], op0=ALU.mult, op1=ALU.add),
                      ["x_S%d" % h, "x_dS", "x_pz1"], ["x_S%d" % h])
                for h in range(4):
                    A(lambda e, h=h: e.copy(out=Sbf[:, h, :], in_=Sst[:, h, :]), ["x_S%d" % h], ["x_Sbf%d" % h])
                for _ in range(4):
                    next(rg, None)
            for _ in rg:
                pass
            for h in range(4):
                A(lambda e, h=h: e.activation(out=bq[0][:, h, :], in_=pw[h][:], func=AF.Square), ["x_pw%d" % h], ["x_bq0"])
            for h in range(4):
                P(lambda e, h=h: e.matmul(pz[h % 2][:], lhsT=g.onesb[:], rhs=bq[0][:, h, :], start=True, stop=True), ["c_onesb", "x_bq0"], ["x_pz%d" % (h % 2)])
                V(lambda e, h=h: e.tensor_scalar(out=f32t[h][:], in0=pz[h % 2][:], scalar1=1.0 / 128, scalar2=EPS, op0=ALU.mult, op1=ALU.add), ["x_pz%d" % (h % 2)], ["x_f%d" % h])
            for h in range(4):
                A(lambda e, h=h: e.activation(out=f32t[h][:], in_=f32t[h][:], func=AF.Sqrt), ["x_f%d" % h], ["x_f%d" % h])
            for h in range(4):
                V(lambda e, h=h: e.reciprocal(out=f32t[h][:], in_=f32t[h][:]), ["x_f%d" % h], ["x_f%d" % h])
            for h in range(4):
                V(lambda e, h=h: e.tensor_tensor(out=f32t[h][:], in0=pw[h][:], in1=f32t[h][:], op=ALU.mult), ["x_pw%d" % h, "x_f%d" % h], ["x_f%d" % h])
            for h in range(4):
                V(lambda e, h=h: e.scalar_tensor_tensor(out=yT[0][:, h, :], in0=f32t[h][:], scalar=cols[:, 48:49], in1=sgt[:, h, :], op0=ALU.mult, op1=ALU.mult),
                  ["x_f%d" % h, CK, qk[5]], ["x_yT0"])

            qr, qx, kr, kz, sgr = bq[0], bq[1], bq[2], bq[3], bq[4]
            c4 = lambda ap: ap.rearrange("p (c j) -> p c j", j=128)
            for isk in range(2):
                if isk == 1:
                    w_rel()
                W, wk = wgrp(2048 + isk * 512)
                for h in range(4):
                    pzt, pk = proj_fm(W, wk, h * 128)
                    V(lambda e, pzt=pzt: e.tensor_tensor(out=f32t[4][:], in0=pzt[:], in1=cosT[:], op=ALU.mult), [pk, "x_cos"], ["x_f4"])
                    proj_fm(W, wk, h * 128 + 64, 64, 0)
                    pzr, pkr = proj_fm(W, wk, h * 128, 64, 64, same=True)
                    V(lambda e, pzr=pzr: e.tensor_tensor(out=f32t[5][:], in0=pzr[:], in1=sinT[:], op=ALU.mult), [pkr, "x_sin"], ["x_f5"])
                    V(lambda e: e.tensor_tensor(out=f32t[4][:], in0=f32t[4][:], in1=f32t[5][:], op=ALU.add), ["x_f4", "x_f5"], ["x_f4"])
                    if isk == 0:
                        A(lambda e, h=h: e.mul(out=qr[:, h, :], in_=f32t[4][:], mul=128.0 ** -0.5), ["x_f4"], [qk[0]])
                        V(lambda e, h=h: e.scalar_tensor_tensor(out=c4(qx[:, h, :]), in0=c4(f32t[4][:]), scalar=128.0 ** -0.5,
                                                                in1=g.XI[:, h:h + 1, :].to_broadcast([128, 4, 128]), op0=ALU.mult, op1=ALU.mult),
                          ["x_f4", "c_XI"], [qk[1]])
                    else:
                        A(lambda e, h=h: e.copy(out=kr[:, h, :], in_=f32t[4][:]), ["x_f4"], [qk[2]])
                        V(lambda e, h=h: e.tensor_tensor(out=c4(kz[:, h, :]), in0=c4(f32t[4][:]), in1=g.ZE[:, h:h + 1, :].to_broadcast([128, 4, 128]), op=ALU.mult),
                          ["x_f4", "c_ZE"], [qk[3]])
            w_rel()
            W, wk = wgrp(3072)
            for i in range(4):
                pi_ = pzc[0] % 2; pzc[0] += 1
                for k in range(8):
                    P(lambda e, k=k, i=i, pi_=pi_: e.matmul(pz[pi_][:], lhsT=hT[:, k, i * 128:(i + 1) * 128], rhs=W[:, k, :], start=(k == 0), stop=(k == 7)),
                      [wk, "x_hT"], ["x_pz%d" % pi_])
                A(lambda e, i=i, pi_=pi_: e.copy(out=vtm[:, i, :], in_=pz[pi_][:]), ["x_pz%d" % pi_], ["x_vtm"])
            w_rel()
            W, wk = wgrp(3584)
            for h in range(4):
                pzt, pk = proj_fm(W, wk, h * 128)
                A(lambda e, h=h, pzt=pzt: e.activation(out=sgr[:, h, :], in_=pzt[:], func=AF.Silu), [pk], [qk[4]])
            w_rel()
            gam = [math.exp(128.0 * g.lng[h]) for h in range(4)]
            scr = sc4
            for c in range(4):
                cs = slice(c * 128, c * 128 + 128)
                for h in range(4):
                    P(lambda e, h=h: e.matmul(pz[0][:, h * 128:(h + 1) * 128], lhsT=kr[:, h, cs], rhs=qr[:, h, cs], start=True, stop=True), [qk[2], qk[0]], ["x_pz0"])
                for h in range(4):
                    V(lambda e, h=h: e.tensor_tensor(out=scr[:, h, :], in0=pz[0][:, h * 128:(h + 1) * 128], in1=g.DM[:, h, :], op=ALU.mult), ["x_pz0", "c_DM"], ["x_sc4_%d" % h])
                for h in range(4):
                    P(lambda e, h=h: e.matmul(pw[h][:, cs], lhsT=vtm[:, c, h * 128:(h + 1) * 128], rhs=scr[:, h, :], start=True, stop=False),
                      ["x_vtm", "x_sc4_%d" % h], ["x_pw%d" % h])
                    P(lambda e, h=h: e.matmul(pw[h][:, cs], lhsT=Rbf[:, h, :], rhs=qx[:, h, cs], start=False, stop=True), ["x_Rbf%d" % h, qk[1]], ["x_pw%d" % h])
                for h in range(4):
                    P(lambda e, h=h: e.transpose(ptb[:, h, :], kz[:, h, cs], g.identb[:]), [qk[3], "identb"], ["x_ptb"])
                for h in range(4):
                    A(lambda e, h=h: e.copy(out=ktm[:, h, :], in_=ptb[:, h, :]), ["x_ptb"], ["x_ktm%d" % h])
                for h in range(4):
                    P(lambda e, h=h: e.matmul(pz[1][:, h * 128:(h + 1) * 128], lhsT=ktm[:, h, :], rhs=vtm[:, c, h * 128:(h + 1) * 128], start=True, stop=True),
                      ["x_ktm%d" % h, "x_vtm"], ["x_pz1"])
                for h in range(4):
                    V(lambda e, h=h: e.scalar_tensor_tensor(out=Rst[:, h, :], in0=Rst[:, h, :], scalar=gam[h], in1=pz[1][:, h * 128:(h + 1) * 128], op0=ALU.mult, op1=ALU.add),
                      ["x_R%d" % h, "x_pz1"], ["x_R%d" % h])
                for h in range(4):
                    A(lambda e, h=h: e.copy(out=Rbf[:, h, :], in_=Rst[:, h, :]), ["x_R%d" % h], ["x_Rbf%d" % h])
            for h in range(4):
                A(lambda e, h=h: e.activation(out=bq[0][:, h, :], in_=pw[h][:], func=AF.Square), ["x_pw%d" % h], ["x_bq0"])
            for h in range(4):
                P(lambda e, h=h: e.matmul(pz[h % 2][:], lhsT=g.onesb[:], rhs=bq[0][:, h, :], start=True, stop=True), ["c_onesb", "x_bq0"], ["x_pz%d" % (h % 2)])
                V(lambda e, h=h: e.tensor_scalar(out=f32t[h][:], in0=pz[h % 2][:], scalar1=1.0 / 128, scalar2=EPS, op0=ALU.mult, op1=ALU.add), ["x_pz%d" % (h % 2)], ["x_f%d" % h])
            for h in range(4):
                A(lambda e, h=h: e.activation(out=f32t[h][:], in_=f32t[h][:], func=AF.Sqrt), ["x_f%d" % h], ["x_f%d" % h])
            for h in range(4):
                V(lambda e, h=h: e.reciprocal(out=f32t[h][:], in_=f32t[h][:]), ["x_f%d" % h], ["x_f%d" % h])
            for h in range(4):
                V(lambda e, h=h: e.tensor_tensor(out=f32t[h][:], in0=pw[h][:], in1=f32t[h][:], op=ALU.mult), ["x_pw%d" % h, "x_f%d" % h], ["x_f%d" % h])
            for h in range(4):
                V(lambda e, h=h: e.tensor_tensor(out=yT[1][:, h, :], in0=f32t[h][:], in1=sgr[:, h, :], op=ALU.mult), ["x_f%d" % h, qk[4]], ["x_yT1"])

            def shifted(pzt, pk, j, dst, dkey):
                A(lambda e: e.copy(out=f513[:, 1:BT + 1], in_=pzt[:]), [pk], ["x_f513"])
                A(lambda e: e.copy(out=f513[:, 0:1], in_=carry[:, j:j + 1]), ["x_carry", "x_f513"], ["x_f513"])
                V(lambda e: e.tensor_tensor(out=dst[:], in0=f513[:, 0:BT], in1=f513[:, 1:BT + 1], op=ALU.subtract), ["x_f513"], [dkey])
                V(lambda e: e.scalar_tensor_tensor(out=dst[:], in0=dst[:], scalar=cols[:, j:j + 1], in1=f513[:, 1:BT + 1], op0=ALU.mult, op1=ALU.add),
                  [dkey, CK, "x_f513"], [dkey])
                A(lambda e: e.copy(out=carry[:, j:j + 1], in_=f513[:, BT:BT + 1]), ["x_f513"], ["x_carry"])

            W, wk = wgrp(4096 + 1536, 256)
            pzt, pk = proj_fm(W, wk, 0)
            shifted(pzt, pk, 12, lora[0], "x_lora0")
            A(lambda e: e.activation(out=lora[0][0:64, :], in_=lora[0][0:64, :], func=AF.Tanh), ["x_lora0"], ["x_lora0"])
            pzt, pk = proj_fm(W, wk, 128)
            shifted(pzt, pk, 13, lora[1], "x_lora1")
            A(lambda e: e.activation(out=lora[1][:], in_=lora[1][:], func=AF.Sigmoid), ["x_lora1"], ["x_lora1"])
            w_rel()
            Wr_, wkr = wgrp(4096)
            Wk_, wkk = wgrp(4096 + 512)
            Wv_, wkv = wgrp(4096 + 1024)
            rs, ks, vs = rw
            rk = ["x_rw0", "x_rw1", "x_rw2"]
            ar3 = lambda tl: tl[:].rearrange("p (c j) -> p c j", j=64)
            t0_, t1_, t2_, t3_ = f32t[0], f32t[1], f32t[2], f32t[3]
            pq = pw[3]; PQ = ["x_pw3"]
            for p in range(4):
                pc = slice(p * 128, p * 128 + 128)
                AR = ARs[p]; bt = bts[p]; kt = kts[p]; vb = vbs[p]; bon = bons[p]
                KAR = "x_AR%d" % p; KBT = "x_bt%d" % p; KKT = "x_kt%d" % p; KVB = "x_vb%d" % p; KBON = "x_bon%d" % p
                pzt, pk = proj_fm(Wr_, wkr, p * 128); shifted(pzt, pk, p, rs, rk[0])
                pzt, pk = proj_fm(Wk_, wkk, p * 128); shifted(pzt, pk, 4 + p, ks, rk[1])
                pzt, pk = proj_fm(Wv_, wkv, p * 128); shifted(pzt, pk, 8 + p, vs, rk[2])
                A(lambda e, vb=vb: e.copy(out=vb[:], in_=vs[:]), [rk[2]], [KVB])
                P(lambda e, pc=pc: e.matmul(pq[:], lhsT=w2a2[0:64, pc], rhs=lora[0][0:64, :], start=True, stop=True), ["x_w2a2", "x_lora0"], PQ)
                A(lambda e, p=p: e.activation(out=t0_[:], in_=pq[:], func=AF.Sigmoid, bias=c_w0(p)), PQ + [CK], ["x_f0"])
                P(lambda e, pc=pc: e.matmul(pq[:], lhsT=w2a2[64:128, pc], rhs=lora[0][64:128, :], start=True, stop=True), ["x_w2a2", "x_lora0"], PQ)
                A(lambda e, p=p: e.activation(out=t1_[:], in_=pq[:], func=AF.Sigmoid, bias=c_a0(p)), PQ + [CK], ["x_f1"])
                V(lambda e, p=p: e.tensor_scalar(out=t2_[:], in0=ks[:], scalar1=c_kk(p), scalar2=None, op0=ALU.mult), [rk[1], CK], ["x_f2"])
                A(lambda e, kt=kt: e.activation(out=kt[:], in_=t2_[:], func=AF.Square), ["x_f2"], [KKT])
                P(lambda e, kt=kt: e.matmul(pq[:], lhsT=g.blkb[:], rhs=kt[:], start=True, stop=True), ["c_blkb", KKT], PQ)
                V(lambda e: e.tensor_scalar(out=t3_[:], in0=pq[:], scalar1=1e-24, scalar2=None, op0=ALU.max), PQ, ["x_f3"])
                A(lambda e: e.activation(out=t3_[:], in_=t3_[:], func=AF.Sqrt), ["x_f3"], ["x_f3"])
                V(lambda e: e.reciprocal(out=t3_[:], in_=t3_[:]), ["x_f3"], ["x_f3"])
                V(lambda e: e.tensor_tensor(out=t2_[:], in0=t2_[:], in1=t3_[:], op=ALU.mult), ["x_f2", "x_f3"], ["x_f2"])
                V(lambda e, p=p: e.tensor_scalar(out=t3_[:], in0=t1_[:], scalar1=c_ka(p), scalar2=c_omka(p), op0=ALU.mult, op1=ALU.add), ["x_f1", CK], ["x_f3"])
                V(lambda e: e.tensor_tensor(out=ks[:], in0=ks[:], in1=t3_[:], op=ALU.mult), [rk[1], "x_f3"], [rk[1]])
                V(lambda e, p=p, bt=bt: e.scalar_tensor_tensor(out=bt[:], in0=rs[:], scalar=c_rk(p), in1=ks[:], op0=ALU.mult, op1=ALU.mult), [rk[0], CK, rk[1]], [KBT])
                P(lambda e, bt=bt: e.matmul(pq[:], lhsT=g.blkb[:], rhs=bt[:], start=True, stop=True), ["c_blkb", KBT], PQ)
                V(lambda e, bon=bon: e.tensor_tensor(out=bon[:], in0=pq[:], in1=vs[:], op=ALU.mult), PQ + [rk[2]], [KBON])
                cs_, u_ = f32t[4], f32t[5]
                V(lambda e: e.tensor_tensor_scan(out=cs_[:], data0=g.rm[:], data1=t0_[:], initial=0.0, op0=ALU.mult, op1=ALU.add), ["c_rm", "x_f0"], ["x_f4"])
                A(lambda e: e.activation(out=u_[:], in_=cs_[:], func=AF.Exp, scale=-E05), ["x_f4"], ["x_f5"])
                V(lambda e, AR=AR: e.tensor_tensor(out=AR[:, :, 64:128], in0=ar3(rs), in1=ar3(u_), op=ALU.mult), [rk[0], "x_f5"], [KAR])
                V(lambda e, p=p: e.tensor_copy(out=dSr[:, p, :], in_=c3(u_)[:, :, 63]), ["x_f5"], ["x_dSr"])
                A(lambda e: e.activation(out=u_[:], in_=cs_[:], func=AF.Exp, scale=E05), ["x_f4"], ["x_f5"])
                V(lambda e, kt=kt: e.tensor_tensor(out=kt[:], in0=ks[:], in1=u_[:], op=ALU.mult), [rk[1], "x_f5"], [KKT])
                V(lambda e: e.tensor_tensor(out=t3_[:], in0=t2_[:], in1=t1_[:], op=ALU.mult), ["x_f2", "x_f1"], ["x_f3"])
                V(lambda e, bt=bt: e.tensor_tensor(out=bt[:], in0=t3_[:], in1=u_[:], op=ALU.mult), ["x_f3", "x_f5"], [KBT])
                V(lambda e: e.tensor_tensor(out=cs_[:], in0=cs_[:], in1=t0_[:], op=ALU.subtract), ["x_f4", "x_f0"], ["x_f4"])
                A(lambda e: e.activation(out=u_[:], in_=cs_[:], func=AF.Exp, scale=-E05), ["x_f4"], ["x_f5"])
                V(lambda e, AR=AR: e.scalar_tensor_tensor(out=AR[:, :, 0:64], in0=ar3(t2_), scalar=-1.0, in1=ar3(u_), op0=ALU.mult, op1=ALU.mult), ["x_f2", "x_f5"], [KAR])
            w_rel(3)
            HH = [slice(0, 64), slice(64, 128)]
            PR = range(4)
            ka = lambda p: "x_pw%d" % p
            kb = lambda p: "x_pw%d" % p
            for c in range(8):
                cs = slice(c * 64, c * 64 + 64)
                for p in PR:
                    for rows in HH:
                        for q, (lt, lk) in enumerate(((bts[p], "x_bt%d" % p), (kts[p], "x_kt%d" % p))):
                            P(lambda e, rows=rows, q=q, lt=lt, p=p: e.matmul(pw[p][rows, q * 128:(q + 1) * 128], lhsT=lt[rows, cs], rhs=ARs[p][rows, c, :], start=True, stop=True),
                              [lk, "x_AR%d" % p], [ka(p)])
                for p in PR:
                    V(lambda e, p=p: e.tensor_tensor(out=ABKs[p][:], in0=pw[p][:, 0:256], in1=g.m4[:], op=ALU.mult), [ka(p), "c_m4"], ["x_ABK%d" % p])
                for p in PR:
                    for rows in HH:
                        for q, (src, sk) in enumerate(((bts[p], "x_bt%d" % p), (kts[p], "x_kt%d" % p), (vbs[p], "x_vb%d" % p))):
                            P(lambda e, rows=rows, q=q, src=src, p=p: e.matmul(pw[p][rows, 256 + q * 64:256 + (q + 1) * 64], lhsT=src[rows, cs], rhs=g.identb[rows, rows], start=True, stop=True),
                              [sk, "identb"], [kb(p)])
                for p in PR:
                    A(lambda e, p=p: e.copy(out=TMss[p][:], in_=pw[p][:, 256:448]), [kb(p)], ["x_TMs%d" % p])
                for p in PR:
                    A(lambda e, p=p: e.copy(out=MTs[p][0:64, 0:64], in_=ABKs[p][0:64, 0:64]), ["x_ABK%d" % p], ["x_MT%d" % p])
                    A(lambda e, p=p: e.copy(out=MTs[p][64:128, 64:128], in_=ABKs[p][64:128, 0:64]), ["x_ABK%d" % p], ["x_MT%d" % p])
                for p in PR:
                    P(lambda e, p=p: e.transpose(ptb[:, p, :], MTs[p][:], g.identb[:]), ["x_MT%d" % p, "identb"], ["x_ptb"])
                for p in PR:
                    A(lambda e, p=p: e.copy(out=MNs[p][0][:], in_=ptb[:, p, :]), ["x_ptb"], ["x_MN%d_0" % p])
                    V(lambda e, p=p: e.tensor_tensor(out=Xs[p][:], in0=MTs[p][:], in1=g.identb[:], op=ALU.add), ["x_MT%d" % p, "identb"], ["x_X%d" % p])
                curP = [(MTs[p], "x_MT%d" % p) for p in PR]
                curT = [(MNs[p][0], "x_MN%d_0" % p) for p in PR]
                for lev in range(1, 6):
                    nT = [(MNs[p][lev % 2], "x_MN%d_%d" % (p, lev % 2)) for p in PR]
                    nP = [(NPs[p][lev % 2], ("x_NP%d_1" % p) if lev % 2 == 1 else ("x_MT%d" % p)) for p in PR]
                    for p in PR:
                        P(lambda e, p=p, a_=curP[p][0], b_=curT[p][0]: e.matmul(pw[p][:, 0:128], lhsT=a_[:], rhs=b_[:], start=True, stop=True), [curP[p][1], curT[p][1]], [ka(p)])
                        if lev < 5:
                            P(lambda e, p=p, a_=curP[p][0], b_=curT[p][0]: e.matmul(pw[p][:, 128:256], lhsT=b_[:], rhs=a_[:], start=True, stop=True), [curP[p][1], curT[p][1]], [ka(p)])
                    for p in PR:
                        A(lambda e, p=p, t_=nT[p][0]: e.copy(out=t_[:], in_=pw[p][:, 0:128]), [ka(p)], [nT[p][1]])
                        if lev < 5:
                            V(lambda e, p=p, t_=nP[p][0]: e.tensor_copy(out=t_[:], in_=pw[p][:, 128:256]), [ka(p)], [nP[p][1]])
                    for p in PR:
                        P(lambda e, p=p, t_=nT[p][0]: e.matmul(pw[p][:, 256:384], lhsT=t_[:], rhs=Xs[p][:], start=True, stop=True), [nT[p][1], "x_X%d" % p], [kb(p)])
                    for p in PR:
                        V(lambda e, p=p: e.tensor_tensor(out=Xs[p][:], in0=Xs[p][:], in1=pw[p][:, 256:384], op=ALU.add), ["x_X%d" % p, kb(p)], ["x_X%d" % p])
                    if lev < 5:
                        curP = nP
                    curT = nT
                for p in PR:
                    for rows in HH:
                        P(lambda e, rows=rows, p=p: e.matmul(pw[p][rows, 384:448], lhsT=ARs[p][rows, c, 0:64], rhs=Wbf[rows, p, :], start=True, stop=False), ["x_AR%d" % p, "x_Wbf%d" % p], [kb(p)])
                        P(lambda e, rows=rows, p=p: e.matmul(pw[p][rows, 384:448], lhsT=ABKs[p][rows, 128:192], rhs=TMss[p][rows, 128:192], start=False, stop=True), ["x_ABK%d" % p, "x_TMs%d" % p], [kb(p)])
                for p in PR:
                    V(lambda e, p=p: e.tensor_copy(out=Wsbs[p][:], in_=pw[p][:, 384:448]), [kb(p)], ["x_Wsb%d" % p])
                for p in PR:
                    P(lambda e, p=p: e.matmul(pw[p][:, 448:512], lhsT=Xs[p][:], rhs=Wsbs[p][:], start=True, stop=True), ["x_X%d" % p, "x_Wsb%d" % p], [kb(p)])
                for p in PR:
                    A(lambda e, p=p: e.copy(out=Usbs[p][:], in_=pw[p][:, 448:512]), [kb(p)], ["x_Usb%d" % p])
                for p in PR:
                    for rows in HH:
                        P(lambda e, rows=rows, p=p: e.matmul(pw[p][rows, 256:320], lhsT=Wbf[rows, p, :], rhs=ARs[p][rows, c, 64:128], start=True, stop=False), ["x_Wbf%d" % p, "x_AR%d" % p], [kb(p)])
                        P(lambda e, rows=rows, p=p: e.matmul(pw[p][rows, 256:320], lhsT=Usbs[p][rows, :], rhs=ABKs[p][rows, 64:128], start=False, stop=False), ["x_Usb%d" % p, "x_ABK%d" % p], [kb(p)])
                        P(lambda e, rows=rows, p=p: e.matmul(pw[p][rows, 256:320], lhsT=TMss[p][rows, 128:192], rhs=ABKs[p][rows, 192:256], start=False, stop=True), ["x_TMs%d" % p, "x_ABK%d" % p], [kb(p)])
                for p in PR:
                    A(lambda e, p=p: e.copy(out=ymix[:, p, cs], in_=pw[p][:, 256:320]), [kb(p)], ["x_ymix%d" % p])
                for p in PR:
                    for rows in HH:
                        P(lambda e, rows=rows, p=p: e.matmul(pw[p][rows, 320:384], lhsT=TMss[p][rows, 0:64], rhs=Usbs[p][rows, :], start=True, stop=False), ["x_TMs%d" % p, "x_Usb%d" % p], [kb(p)])
                        P(lambda e, rows=rows, p=p: e.matmul(pw[p][rows, 320:384], lhsT=TMss[p][rows, 64:128], rhs=TMss[p][rows, 128:192], start=False, stop=True), ["x_TMs%d" % p], [kb(p)])
                for p in PR:
                    V(lambda e, p=p: e.tensor_tensor(out=tSs[p][:], in0=Wst[:, p, :], in1=pw[p][:, 320:384], op=ALU.add), ["x_Wst%d" % p, kb(p)], ["x_tS%d" % p])
                    V(lambda e, p=p: e.tensor_scalar(out=Wst[:, p, :], in0=tSs[p][:], scalar1=dSr[:, p, c:c + 1], scalar2=None, op0=ALU.mult), ["x_tS%d" % p, "x_dSr"], ["x_Wst%d" % p])
                    A(lambda e, p=p: e.copy(out=Wbf[:, p, :], in_=Wst[:, p, :]), ["x_Wst%d" % p], ["x_Wbf%d" % p])
            for p in range(4):
                pc = slice(p * 128, p * 128 + 128)
                ta = f32t[2 * (p % 2)]; tak = "x_f%d" % (2 * (p % 2)); tb_ = f32t[2 * (p % 2) + 1]; tbk = "x_f%d" % (2 * (p % 2) + 1)
                pe_ = pw[p]; PEK = ["x_pw%d" % p]
                OK_ = "x_ymix%d" % p
                P(lambda e, p=p, pe_=pe_: e.matmul(pe_[:], lhsT=g.blk[:], rhs=ymix[:, p, :], start=True, stop=True), ["c_blk", OK_], PEK)
                V(lambda e, p=p, pe_=pe_, ta=ta: e.scalar_tensor_tensor(out=ta[:], in0=pe_[:], scalar=-1.0 / 64, in1=ymix[:, p, :], op0=ALU.mult, op1=ALU.add), PEK + [OK_], [tak])
                A(lambda e, ta=ta, p=p: e.activation(out=bts[p][:], in_=ta[:], func=AF.Square), [tak], ["x_bt%d" % p])
                P(lambda e, pe_=pe_, p=p: e.matmul(pe_[:], lhsT=g.blkb[:], rhs=bts[p][:], start=True, stop=True), ["c_blkb", "x_bt%d" % p], PEK)
                V(lambda e, pe_=pe_, tb_=tb_: e.tensor_scalar(out=tb_[:], in0=pe_[:], scalar1=1.0 / 64, scalar2=64e-5, op0=ALU.mult, op1=ALU.add), PEK, [tbk])
                A(lambda e, tb_=tb_: e.activation(out=tb_[:], in_=tb_[:], func=AF.Sqrt), [tbk], [tbk])
                V(lambda e, tb_=tb_: e.reciprocal(out=tb_[:], in_=tb_[:]), [tbk], [tbk])
                V(lambda e, ta=ta, tb_=tb_: e.tensor_tensor(out=ta[:], in0=ta[:], in1=tb_[:], op=ALU.mult), [tak, tbk], [tak])
                V(lambda e, p=p, ta=ta: e.tensor_scalar(out=ta[:], in0=ta[:], scalar1=c_lw(p), scalar2=c_lb(p), op0=ALU.mult, op1=ALU.add), [tak, CK], [tak])
                V(lambda e, p=p, ta=ta: e.tensor_tensor(out=ta[:], in0=ta[:], in1=bons[p][:], op=ALU.add), [tak, "x_bon%d" % p], [tak])
                P(lambda e, pc=pc, pe_=pe_: e.matmul(pe_[:], lhsT=g2t[:, pc], rhs=lora[1][:], start=True, stop=True), ["x_g2", "x_lora1"], PEK)
                V(lambda e, p=p, ta=ta, pe_=pe_: e.tensor_tensor(out=yT[2][:, p, :], in0=ta[:], in1=pe_[:], op=ALU.mult), [tak] + PEK, ["x_yT2"])

            if dbg is not None and blk == dbg[1]:
                for b in range(3):
                    S.dma("pool", dbg[0][b], yT[b][:], reads=["x_yT%d" % b], writes=["dbg"])

            for b, brn in enumerate((io.br_hg, io.br_ret, io.br_rw)):
                BR, bk = w_get()
                for gh in range(2):
                    W, wk = w_get()
                    for dl in range(4):
                        dc = gh * 4 + dl
                        pzt, pk = proj_fm(W, wk, dl * 128)
                        db = dl % 2
                        sgm = f32t[2 * db]; sgk = "x_f%d" % (2 * db); prd = f32t[2 * db + 1]; prk = "x_f%d" % (2 * db + 1)
                        pbr = pw[db]; pbk = "x_pw%d" % db
                        A(lambda e, pzt=pzt, sgm=sgm: e.activation(out=sgm[:], in_=pzt[:], func=AF.Sigmoid), [pk], [sgk])
                        for k in range(4):
                            P(lambda e, k=k, dc=dc, b=b, BR=BR, pbr=pbr: e.matmul(pbr[:], lhsT=BR[:, k, dc * 128:(dc + 1) * 128], rhs=yT[b][:, k, :], start=(k == 0), stop=(k == 3)),
                              [bk, "x_yT%d" % b], [pbk])
                        ymk = "x_ymix%d" % dc
                        if b == 0:
                            V(lambda e, dc=dc, sgm=sgm, pbr=pbr: e.tensor_tensor(out=ymix[:, dc, :], in0=sgm[:], in1=pbr[:], op=ALU.mult), [sgk, pbk], [ymk])
                        else:
                            V(lambda e, sgm=sgm, pbr=pbr, prd=prd: e.tensor_tensor(out=prd[:], in0=sgm[:], in1=pbr[:], op=ALU.mult), [sgk, pbk], [prk])
                            if b == 1:
                                V(lambda e, dc=dc, prd=prd: e.tensor_tensor(out=ymix[:, dc, :], in0=ymix[:, dc, :], in1=prd[:], op=ALU.add), [ymk, prk], [ymk])
                            else:
                                V(lambda e, dc=dc, prd=prd: e.tensor_tensor(out=ymixb[:, dc, :], in0=ymix[:, dc, :], in1=prd[:], op=ALU.add), [ymk, prk], ["x_ymixb"])
                    if gh == 1:
                        w_rel(3)
            Wo = [w_get() for hh in range(2)]
            for hh in range(2):
                S.dma("sp", f32t[hh][:], io.modrow[l:l + 1, 2 * D + hh * 512:2 * D + (hh + 1) * 512].to_broadcast([128, 512]),
                      reads=["modrow%d" % l], writes=["x_f%d" % hh])
            for i in range(4):
                xb = xt[i % 2]; xk = "x_xt%d" % (i % 2)
                S.dma("sp", xb[:], x_src[t0 + i * 128:t0 + (i + 1) * 128, :], reads=[xs_key], writes=[xk])
                for hh in range(2):
                    pi_ = pzc[0] % 2; pzc[0] += 1
                    for k in range(8):
                        P(lambda e, k=k, i=i, hh=hh, pi_=pi_: e.matmul(pz[pi_][:], lhsT=ymixb[:, k, i * 128:(i + 1) * 128], rhs=Wo[hh][0][:, k, :], start=(k == 0), stop=(k == 7)),
                          ["x_ymixb", Wo[hh][1]], ["x_pz%d" % pi_])
                    hs = slice(hh * 512, hh * 512 + 512)
                    V(lambda e, pi_=pi_, hs=hs, hh=hh: e.tensor_tensor(out=hf[:, hs], in0=pz[pi_][:], in1=f32t[hh][:], op=ALU.mult), ["x_pz%d" % pi_, "x_f%d" % hh], ["x_hf"])
                    V(lambda e, xb=xb, hs=hs: e.tensor_tensor(out=xb[:, hs], in0=xb[:, hs], in1=hf[:, hs], op=ALU.add), ["x_hf", xk], [xk])
                S.dma("sp", x_dst[t0 + i * 128:t0 + (i + 1) * 128, :], xb[:], reads=[xk], writes=[xd_key])
            w_rel(2)
    S.barrier()


_NAMES = ["ada_w", "ada_b", "norm1_g", "norm2_g", "w_in", "hg_lb_table", "hg_norm_w", "rw_mu", "rw_w0", "rw_w2",
          "rw_a0", "rw_a2", "rw_g2", "rw_k_k", "rw_k_a", "rw_ln_w", "rw_ln_b", "br_hg", "br_ret", "br_rw", "w_out",
          "router_g", "router_e", "moe_w1", "moe_w3", "moe_w2"]


def make_in_maps(inputs):
    f = lambda a: np.ascontiguousarray(np.asarray(a, dtype=np.float32))
    shared = {n: f(inputs[n]) for n in _NAMES}
    shared["rw_r_k"] = f(inputs["rw_r_k"]).reshape(NL, 512)
    shared["final_g"] = f(inputs["final_g"]).reshape(1, D)
    x = f(inputs["x"]); c = f(inputs["c"])
    pos = np.ascontiguousarray(np.asarray(inputs["positions"], dtype=np.int32))
    maps = []
    for b in range(8):
        m = dict(shared)
        m["x"] = x[b]; m["c"] = c[b:b + 1]; m["positions"] = pos[b:b + 1]
        maps.append(m)
    return maps


def kernel(**inputs):
    nc = build()
    maps = make_in_maps(inputs)
    res = run_bass_kernel_spmd(nc, maps, core_ids=list(range(8)))
    return np.stack([np.asarray(r["out"], dtype=np.float32) for r in res.results], axis=0)
```

```python
import contextlib
import math
import numpy as np
import concourse.bass as bass
import concourse.mybir as mybir
from concourse.bass_utils import run_bass_kernel_spmd

F32 = mybir.dt.float32
BF16 = mybir.dt.bfloat16
I32 = mybir.dt.int32
AF = mybir.ActivationFunctionType
ALU = mybir.AluOpType
AX = mybir.AxisListType

T = 4096
D = 1024
NL = 2
NE = 32
DE = 512
IN_COLS = 8960
EPS = 1e-6
D1_SEQ = False
PI = math.pi


class Sched:
    def __init__(self, nc, stack):
        self.nc = nc
        self.stack = stack
        self.eng = {"pe": nc.tensor, "act": nc.scalar, "dve": nc.vector, "pool": nc.gpsimd, "sp": nc.sync}
        self.esem = {}
        self.ecount = {}
        for e in self.eng:
            self.esem[e] = stack.enter_context(nc.semaphore("s_" + e))
            self.ecount[e] = 0
        self.sems = dict((id(s), s) for s in self.esem.values())
        self.seen = {e: {} for e in self.eng}
        self.lastw = {}
        self.reads = {}
        self.dsem = {}
        self.dcount = {}
        self.n_inst = 0
        self.n_wait = 0
        self.psum_keys = set()

    @property
    def cur_counts(self):
        return {id(self.esem[e]): (lambda e=e: self.ecount[e]) for e in ("act", "dve", "pe")}

    def _dma_sem(self, key):
        if key not in self.dsem:
            s = self.stack.enter_context(self.nc.semaphore("d%d" % len(self.dsem)))
            self.dsem[key] = s
            self.dcount[key] = 0
            self.sems[id(s)] = s
        return self.dsem[key]

    def _deps(self, e, reads, writes):
        evs = []
        for k in reads:
            w = self.lastw.get(k)
            if w is not None:
                evs.append(w)
        for k in writes:
            w = self.lastw.get(k)
            if w is not None and not (e != "dma" and w[2] == e):
                evs.append(w)
            for r in self.reads.get(k, ()):
                if e != "dma" and r[2] == e:
                    continue
                evs.append(r)
        return evs

    def _wait(self, e, evs):
        seen = self.seen[e]
        best = {}
        for (sid, val, _src) in evs:
            if seen.get(sid, 0) >= val:
                continue
            if best.get(sid, 0) < val:
                best[sid] = val
        for sid, val in best.items():
            if getattr(self, "coarse", False) and sid in self.cur_counts:
                val = max(val, self.cur_counts[sid]())
            self.eng[e].wait_ge(self.sems[sid], val)
            seen[sid] = val
            self.n_wait += 1

    def _record(self, ev, reads, writes):
        for k in reads:
            lst = self.reads.setdefault(k, [])
            lst[:] = [r for r in lst if r[0] != ev[0]]
            lst.append(ev)
        for k in writes:
            self.lastw[k] = ev
            self.reads[k] = []

    def op(self, e, fn, reads=(), writes=()):
        px = [k for k in reads if k in self.psum_keys and k not in writes]
        if px:
            writes = list(writes) + px
        evs = self._deps(e, reads, writes)
        if e == "pe":
            evs = [x for x in evs if x[2] != "pe"]
        self._wait(e, evs)
        inst = fn(self.eng[e])
        self.ecount[e] += 1
        inst.then_inc(self.esem[e], 1)
        ev = (id(self.esem[e]), self.ecount[e], e)
        self._record(ev, reads, writes)
        self.n_inst += 1
        return inst

    def dma(self, q, out, in_, reads=(), writes=(), **kw):
        evs = self._deps("dma", reads, writes)
        self._wait(q, evs)
        key = writes[0]
        s = self._dma_sem(key)
        inst = self.eng[q].dma_start(out=out, in_=in_, **kw)
        self.dcount[key] += 16
        inst.then_inc(s, 16)
        ev = (id(s), self.dcount[key], "dma")
        self._record(ev, reads, writes)
        self.n_inst += 1
        return inst

    def barrier(self):
        evs = [(id(self.esem[e]), self.ecount[e], e) for e in self.eng if self.ecount[e] > 0]
        evs += [(id(self.dsem[k]), self.dcount[k], "dma") for k in self.dsem if self.dcount[k] > 0]
        for e in self.eng:
            self._wait(e, [x for x in evs if x[2] != e or e == "dma"])
        self.nbar = getattr(self, "nbar", 0) + 1
        for e in self.eng:
            s_ = self.stack.enter_context(self.nc.semaphore("s_%s_%d" % (e, self.nbar)))
            self.esem[e] = s_
            self.sems[id(s_)] = s_
            self.ecount[e] = 0
        self.lastw = {}
        self.reads = {}


class Ctx:
    pass


_UID = [0]


def _un(name):
    _UID[0] += 1
    return "%s_u%d" % (name, _UID[0])


def _consts(S, nc, st, g):
    sb = lambda name, shape, dt=F32: st.enter_context(nc.sbuf_tensor(_un(name), list(shape), dt))
    g.ident = sb("ident", [128, 128])
    g.identb = sb("identb", [128, 128], BF16)
    g.ones = sb("ones", [128, 128])
    S.op("pool", lambda e: e.memset(g.ones[:], 1.0), writes=["ones"])
    S.op("pool", lambda e: e.memset(g.ident[:], 1.0), writes=["ident"])
    S.op("pool", lambda e: e.affine_select(out=g.ident[:], in_=g.ident[:], pattern=[[1, 128]],
                                           compare_op=ALU.is_equal, fill=0.0, base=0, channel_multiplier=-1),
         reads=["ident"], writes=["ident"])
    S.op("dve", lambda e: e.tensor_copy(out=g.identb[:], in_=g.ident[:]), reads=["ident"], writes=["identb"])
    g.eps = sb("epsc", [128, 1])
    S.op("pool", lambda e: e.memset(g.eps[:], EPS), writes=["epsc"])


def _rms_rstd(S, g, xt, xkey, junk, jkey, ss, rstd, tag):
    S.op("act", lambda e: e.activation(out=junk, in_=xt, func=AF.Square, accum_out=ss),
         reads=[xkey], writes=[jkey, tag + "ss"])
    S.op("dve", lambda e: e.tensor_scalar(out=ss, in0=ss, scalar1=1.0 / D, scalar2=EPS, op0=ALU.mult, op1=ALU.add),
         reads=[tag + "ss"], writes=[tag + "ss"])
    S.op("act", lambda e: e.activation(out=ss, in_=ss, func=AF.Sqrt), reads=[tag + "ss"], writes=[tag + "ss"])
    S.op("dve", lambda e: e.reciprocal(out=rstd, in_=ss), reads=[tag + "ss"], writes=[tag + "rstd"])


def _phase0(S, nc, g, io):
    with contextlib.ExitStack() as st:
        sb = lambda name, shape, dt=F32: st.enter_context(nc.sbuf_tensor(_un(name), list(shape), dt))
        cc = sb("p0_c", [128, 8])
        aw = [sb("p0_aw%d" % i, [128, 8, 512]) for i in range(2)]
        ab = sb("p0_ab", [1, 6 * D])
        row = sb("p0_row", [1, 6 * D])
        ps = [st.enter_context(nc.psum_tensor(_un("p0_ps%d" % i), [128, 512], F32)) for i in range(2)]
        S.psum_keys.update(["p0_ps0", "p0_ps1"])
        S.dma("sp", cc[:], io.c.rearrange("o (k p) -> p (o k)", p=128), writes=["p0_c"], allow_slow_non_contiguous=True)
        S.op("act", lambda e: e.activation(out=cc[:], in_=cc[:], func=AF.Silu), reads=["p0_c"], writes=["p0_c"])
        for l in range(NL):
            S.dma("sp", ab[:], io.ada_b[l:l + 1, :], writes=["p0_ab"])
            for nb in range(12):
                b = nb % 2
                S.dma("sp", aw[b][:], io.ada_w[l, :, nb * 512:(nb + 1) * 512].rearrange("(k p) n -> p k n", p=128),
                      writes=["p0_aw%d" % b])
                for k in range(8):
                    S.op("pe", lambda e, k=k, b=b: e.matmul(ps[b][0:1, :], lhsT=cc[:, k:k + 1], rhs=aw[b][:, k, :],
                                                           start=(k == 0), stop=(k == 7)),
                         reads=["p0_c", "p0_aw%d" % b], writes=["p0_ps%d" % b])
                S.op("dve", lambda e, b=b, nb=nb: e.tensor_tensor(out=row[0:1, nb * 512:(nb + 1) * 512], in0=ps[b][0:1, :],
                                                                 in1=ab[0:1, nb * 512:(nb + 1) * 512], op=ALU.add),
                     reads=["p0_ps%d" % b, "p0_ab"], writes=["p0_row"])
            S.dma("sp", io.modrow[l:l + 1, :], row[:], reads=["p0_row"], writes=["modrow%d" % l])
    S.barrier()


def _load_mod(S, nc, g, io, l, which, gain_ap, A, Akey, B, Bkey, G, Gkey, tmp, tkey):
    o = 0 if which == 1 else 3
    mr = io.modrow
    S.dma("sp", B, mr[l:l + 1, (o + 0) * D:(o + 1) * D].to_broadcast([128, D]), reads=["modrow%d" % l], writes=[Bkey])
    S.dma("sp", A, mr[l:l + 1, (o + 1) * D:(o + 2) * D].to_broadcast([128, D]), reads=["modrow%d" % l], writes=[Akey])
    if G is not None:
        S.dma("sp", G, mr[l:l + 1, (o + 2) * D:(o + 3) * D].to_broadcast([128, D]), reads=["modrow%d" % l], writes=[Gkey])
    S.dma("sp", tmp, gain_ap.to_broadcast([128, D]), writes=[tkey])
    S.op("dve", lambda e: e.scalar_tensor_tensor(out=A, in0=A, scalar=1.0, in1=tmp, op0=ALU.add, op1=ALU.mult),
         reads=[Akey, tkey], writes=[Akey])


def _moe_phase(S, nc, g, io, l, x_src, xs_key, x_dst, xd_key, final, pre=None):
    NPASS = 2
    TP = T // NPASS
    NT = TP // 128
    with contextlib.ExitStack() as st:
        sb = lambda name, shape, dt=F32: st.enter_context(nc.sbuf_tensor(_un(name), list(shape), dt))
        pst = lambda name, shape, dt=F32: (S.psum_keys.add(name), st.enter_context(nc.psum_tensor(_un(name), list(shape), dt)))[1]
        A2 = sb("m_A2", [128, D]); B2 = sb("m_B2", [128, D]); G2 = sb("m_G2", [128, D])
        FG = sb("m_FG", [128, D])
        h2T = sb("m_h2T", [128, 8, TP], BF16)
        yacc = sb("m_yacc", [128, NT, D])
        combH = sb("m_combH", [32, TP], BF16); combL = sb("m_combL", [32, TP], BF16)
        Wr = sb("m_Wr", [128, 8, 36])
        xt = [sb("m_xt%d" % i, [128, D]) for i in range(2)]
        hfs = [sb("m_hf%d" % i, [128, D]) for i in range(2)]
        hTfs = [sb("m_hTf%d" % i, [128, 8, 128]) for i in range(2)]
        smalls = [sb("m_small%d" % i, [128, 160]) for i in range(2)]
        sss = [sb("m%d_ss" % i, [128, 1]) for i in range(2)]; rstds = [sb("m%d_rstd" % i, [128, 1]) for i in range(2)]
        hf = hfs[0]; ss = sss[0]; rstd = rstds[0]
        w1 = [sb("m_w1_%d" % i, [128, 8, DE], BF16) for i in range(2)]
        w3 = [sb("m_w3_%d" % i, [128, 8, DE], BF16) for i in range(2)]
        w2 = [sb("m_w2_%d" % i, [128, 4, D], BF16) for i in range(2)]
        sil = [sb("m_sil%d" % i, [128, 512], BF16) for i in range(2)]
        tmp = [sb("m_tmp%d" % i, [128, 512], BF16) for i in range(2)]
        actT = [sb("m_act%d" % i, [128, 4, 512], BF16) for i in range(2)]
        p_h1 = [pst("m_ph1_%d" % i, [128, 512]) for i in range(2)]
        p_h3 = [pst("m_ph3_%d" % i, [128, 512]) for i in range(2)]
        p_cb = pst("m_pcb", [128, 512])
        p_y = [pst("m_py%d" % i, [128, 512]) for i in range(2)]
        p_tb = pst("m_ptb", [128, 8, 128], BF16)
        p_cbs = [(p_cb[:], "m_pcb"), (p_tb[:].rearrange("p a b -> p (a b)").bitcast(F32), "m_ptb")]

        _load_mod(S, nc, g, io, l, 2, io.norm2_g[l:l + 1, :], A2[:], "m_A2", B2[:], "m_B2", G2[:], "m_G2",
                  hf[:], "m_hf0")
        if final:
            S.dma("sp", FG[:], io.final_g.to_broadcast([128, D]), writes=["m_FG"])
        S.dma("sp", Wr[:, :, 0:4], io.router_g[l].rearrange("(k p) n -> p k n", p=128), writes=["m_Wr"])
        S.dma("sp", Wr[:, :, 4:36], io.router_e[l].rearrange("(k p) n -> p k n", p=128), writes=["m_Wr"])

        def load_expert(e):
            b = e % 2
            S.dma("pool", w1[b][:], io.moe_w1[l, e].rearrange("(k p) n -> p k n", p=128), writes=["m_w1_%d" % b])
            S.dma("pool", w3[b][:], io.moe_w3[l, e].rearrange("(k p) n -> p k n", p=128), writes=["m_w3_%d" % b])
            S.dma("pool", w2[b][:], io.moe_w2[l, e].rearrange("(k p) n -> p k n", p=128), writes=["m_w2_%d" % b])

        pre = list(pre) if pre is not None else []
        d3_pending = []

        def d3_tile(tb0, i):
            par = i % 2
            xb = xt[par]; xk = "m_xt%d" % par
            hf_ = hfs[par]; hfk = "m_hf%d" % par
            S.dma("sp", xb[:], x_src[tb0 + i * 128:tb0 + (i + 1) * 128, :], reads=[xs_key], writes=[xk])
            S.op("dve", lambda e: e.tensor_tensor(out=hf_[:], in0=yacc[:, i, :], in1=G2[:], op=ALU.mult),
                 reads=["m_yacc%d" % i, "m_G2"], writes=[hfk])
            S.op("dve", lambda e: e.tensor_tensor(out=xb[:], in0=xb[:], in1=hf_[:], op=ALU.add),
                 reads=[hfk, xk], writes=[xk])
            if final:
                _rms_rstd(S, g, xb[:], xk, hf_[:], hfk, sss[par][:], rstds[par][:], "m%d_" % par)
                S.op("dve", lambda e: e.scalar_tensor_tensor(out=xb[:], in0=xb[:], scalar=rstds[par][:], in1=FG[:],
                                                             op0=ALU.mult, op1=ALU.mult),
                     reads=[xk, "m%d_rstd" % par, "m_FG"], writes=[xk])
            S.dma("sp", x_dst[tb0 + i * 128:tb0 + (i + 1) * 128, :], xb[:], reads=[xk], writes=[xd_key])
        for ps_ in range(NPASS):
            t0 = ps_ * TP
            def d1_tile(i, par):
                xb = xt[par]; xk = "m_xt%d" % par
                hf_ = hfs[par]; hfk = "m_hf%d" % par
                hTf_ = hTfs[par]; hTk = "m_hTf%d" % par
                small = smalls[par]; K = "m_small%d" % par
                ss_ = sss[par]; rstd_ = rstds[par]; tg = "m%d_" % par
                py = p_y[par]; pyk = "m_py%d" % par
                plg, plk = (p_cb, "m_pcb") if par == 0 else (p_h1[0], "m_ph1_0")
                S.dma("sp", xb[:], x_src[t0 + i * 128:t0 + (i + 1) * 128, :], reads=[xs_key], writes=[xk])
                _rms_rstd(S, g, xb[:], xk, hf_[:], hfk, ss_[:], rstd_[:], tg)
                yield
                S.op("dve", lambda e: e.scalar_tensor_tensor(out=hf_[:], in0=xb[:], scalar=rstd_[:], in1=A2[:],
                                                             op0=ALU.mult, op1=ALU.mult),
                     reads=[xk, tg + "rstd", "m_A2"], writes=[hfk])
                S.op("dve", lambda e: e.tensor_tensor(out=hf_[:], in0=hf_[:], in1=B2[:], op=ALU.add),
                     reads=[hfk, "m_B2"], writes=[hfk])
                yield
                for hh in range(2):
                    for k in range(4):
                        kk = hh * 4 + k
                        S.op("pe", lambda e, k=k, kk=kk: e.transpose(py[:, k * 128:(k + 1) * 128],
                                                                     hf_[:, kk * 128:(kk + 1) * 128], g.ident[:]),
                             reads=[hfk, "ident"], writes=[pyk])
                    S.op("dve", lambda e, hh=hh: e.tensor_copy(out=hTf_[:, hh * 4:(hh + 1) * 4, :], in_=py[:]),
                         reads=[pyk], writes=[hTk])
                    yield
                S.op("act", lambda e: e.copy(out=h2T[:, :, i * 128:(i + 1) * 128], in_=hTf_[:]),
                     reads=[hTk], writes=["m_h2T"])
                for k in range(8):
                    S.op("pe", lambda e, k=k: e.matmul(plg[:, 0:36], lhsT=hTf_[:, k, :], rhs=Wr[:, k, :],
                                                       start=(k == 0), stop=(k == 7)),
                         reads=[hTk, "m_Wr"], writes=[plk])
                yield
                Lg = small[:, 0:36]
                gmax = small[:, 36:37]; oh = small[:, 40:44]; eg = small[:, 44:48]; sumg = small[:, 48:49]
                gw = small[:, 49:50]; esel = small[:, 52:60]; top8 = small[:, 60:68]; negm1 = small[:, 68:69]
                p2 = small[:, 69:70]; den = small[:, 70:71]; w1g = small[:, 71:72]; w2g = small[:, 72:73]
                c1 = small[:, 76:84]; c2 = small[:, 84:92]; comb = small[:, 96:128]; ngmax = small[:, 37:38]
                vop = lambda fn: S.op("dve", fn, reads=[K], writes=[K])
                S.op("dve", lambda e: e.tensor_copy(out=Lg, in_=plg[:, 0:36]), reads=[plk], writes=[K])
                vop(lambda e: e.tensor_reduce(out=gmax, in_=small[:, 0:4], axis=AX.X, op=ALU.max))
                vop(lambda e: e.tensor_scalar(out=oh, in0=small[:, 0:4], scalar1=gmax, scalar2=None, op0=ALU.is_equal))
                vop(lambda e: e.tensor_scalar(out=ngmax, in0=gmax, scalar1=-1.0, scalar2=None, op0=ALU.mult))
                yield
                S.op("act", lambda e: e.activation(out=eg, in_=small[:, 0:4], func=AF.Exp, bias=ngmax, accum_out=sumg),
                     reads=[K], writes=[K])
                vop(lambda e: e.reciprocal(out=gw, in_=sumg))
                vop(lambda e: e.tensor_scalar(out=esel, in0=small[:, 4:12], scalar1=small[:, 40:41], scalar2=None,
                                              op0=ALU.mult))
                yield
                for gi in range(1, 4):
                    vop(lambda e, gi=gi: e.scalar_tensor_tensor(out=esel, in0=small[:, 4 + 8 * gi:12 + 8 * gi],
                                                               scalar=small[:, 40 + gi:41 + gi], in1=esel,
                                                               op0=ALU.mult, op1=ALU.add))
                    yield
                vop(lambda e: e.max(out=top8, in_=esel))
                vop(lambda e: e.tensor_scalar(out=negm1, in0=small[:, 60:61], scalar1=-1.0, scalar2=None, op0=ALU.mult))
                yield
                S.op("act", lambda e: e.activation(out=p2, in_=small[:, 61:62], func=AF.Exp, bias=negm1),
                     reads=[K], writes=[K])
                vop(lambda e: e.tensor_scalar(out=den, in0=p2, scalar1=1.0, scalar2=None, op0=ALU.add))
                yield
                vop(lambda e: e.reciprocal(out=den, in_=den))
                yield
                vop(lambda e: e.tensor_tensor(out=w1g, in0=den, in1=gw, op=ALU.mult))
                yield
                vop(lambda e: e.tensor_tensor(out=w2g, in0=w1g, in1=p2, op=ALU.mult))
                vop(lambda e: e.tensor_scalar(out=c1, in0=esel, scalar1=small[:, 60:61], scalar2=w1g,
                                              op0=ALU.is_equal, op1=ALU.mult))
                yield
                vop(lambda e: e.tensor_scalar(out=c2, in0=esel, scalar1=small[:, 61:62], scalar2=w2g,
                                              op0=ALU.is_equal, op1=ALU.mult))
                yield
                vop(lambda e: e.tensor_tensor(out=c1, in0=c1, in1=c2, op=ALU.add))
                yield
                for gi in range(4):
                    vop(lambda e, gi=gi: e.tensor_scalar(out=small[:, 96 + 8 * gi:104 + 8 * gi], in0=c1,
                                                        scalar1=small[:, 40 + gi:41 + gi], scalar2=None, op0=ALU.mult))
                yield
                S.op("pe", lambda e: e.transpose(plg[0:32, 128:256], comb, g.ident[:]),
                     reads=[K, "ident"], writes=[plk])
                S.op("dve", lambda e: e.tensor_copy(out=combH[:, i * 128:(i + 1) * 128], in_=plg[0:32, 128:256]),
                     reads=[plk], writes=["m_combH"])
                S.op("dve", lambda e: e.tensor_tensor(out=combL[:, i * 128:(i + 1) * 128], in0=plg[0:32, 128:256],
                                                      in1=combH[:, i * 128:(i + 1) * 128], op=ALU.subtract),
                     reads=[plk, "m_combH"], writes=["m_combL"])

            for i in range(0, NT, 2):
                gens = [d1_tile(i, 0), d1_tile(i + 1, 1)]
                if D1_SEQ:
                    for gn in gens:
                        for _ in gn:
                            pass
                    gens = []
                while gens:
                    for gn in list(gens):
                        try:
                            next(gn)
                        except StopIteration:
                            gens.remove(gn)
            units = [(ex, blk) for ex in range(NE) for blk in range(TP // 512)]

            def front(ex, blk, it, fcs=range(4)):
                b = ex % 2; c0 = blk * 512; ab = it % 2
                pcb, pck = p_cbs[it % 2]
                wk = ["m_w1_%d" % b, "m_w3_%d" % b]
                if 0 in fcs:
                    S.op("pe", lambda e: e.matmul(pcb, lhsT=g.identb[0:32, ex:ex + 1].to_broadcast([32, 128]),
                                                  rhs=combH[:, c0:c0 + 512], start=True, stop=False),
                         reads=["identb", "m_combH"], writes=[pck])
                    S.op("pe", lambda e: e.matmul(pcb, lhsT=g.identb[0:32, ex:ex + 1].to_broadcast([32, 128]),
                                                  rhs=combL[:, c0:c0 + 512], start=False, stop=True),
                         reads=["identb", "m_combL"], writes=[pck])
                for fc in fcs:
                    pb = fc % 2
                    for k in range(8):
                        S.op("pe", lambda e, k=k, fc=fc, pb=pb: e.matmul(
                            p_h1[pb][:], lhsT=w1[b][:, k, fc * 128:(fc + 1) * 128], rhs=h2T[:, k, c0:c0 + 512],
                            start=(k == 0), stop=(k == 7)), reads=[wk[0], "m_h2T"], writes=["m_ph1_%d" % pb])
                    for k in range(8):
                        S.op("pe", lambda e, k=k, fc=fc, pb=pb: e.matmul(
                            p_h3[pb][:], lhsT=w3[b][:, k, fc * 128:(fc + 1) * 128], rhs=h2T[:, k, c0:c0 + 512],
                            start=(k == 0), stop=(k == 7)), reads=[wk[1], "m_h2T"], writes=["m_ph3_%d" % pb])
                    S.op("act", lambda e, pb=pb: e.activation(out=sil[pb][:], in_=p_h1[pb][:], func=AF.Silu),
                         reads=["m_ph1_%d" % pb], writes=["m_sil%d" % pb])
                    S.op("dve", lambda e, pb=pb: e.tensor_tensor(out=tmp[pb][:], in0=sil[pb][:], in1=p_h3[pb][:], op=ALU.mult),
                         reads=["m_sil%d" % pb, "m_ph3_%d" % pb], writes=["m_tmp%d" % pb])
                    S.op("dve", lambda e, pb=pb, fc=fc: e.tensor_tensor(out=actT[ab][:, fc, :], in0=tmp[pb][:], in1=pcb, op=ALU.mult),
                         reads=["m_tmp%d" % pb, pck], writes=["m_act%d" % ab])

            def back(ex, blk, it, tls=range(4)):
                b = ex % 2; ab = it % 2
                for tl in tls:
                    ti = blk * 4 + tl
                    if ex == 0 and d3_pending:
                        for (tb0, i_) in [x_ for x_ in d3_pending if x_[1] == ti]:
                            d3_tile(tb0, i_)
                            d3_pending.remove((tb0, i_))
                    for hh in range(2):
                        for fc in range(4):
                            S.op("pe", lambda e, fc=fc, tl=tl, hh=hh: e.matmul(
                                p_y[hh][:], lhsT=actT[ab][:, fc, tl * 128:(tl + 1) * 128],
                                rhs=w2[b][:, fc, hh * 512:(hh + 1) * 512], start=(fc == 0), stop=(fc == 3)),
                                 reads=["m_act%d" % ab, "m_w2_%d" % b], writes=["m_py%d" % hh])
                        ya = yacc[:, ti, hh * 512:(hh + 1) * 512]
                        yk = "m_yacc%d" % ti
                        if ex == 0:
                            S.op("dve", lambda e, ya=ya, hh=hh: e.tensor_copy(out=ya, in_=p_y[hh][:]),
                                 reads=["m_py%d" % hh], writes=[yk])
                        else:
                            S.op("dve", lambda e, ya=ya, hh=hh: e.tensor_tensor(out=ya, in0=ya, in1=p_y[hh][:], op=ALU.add),
                                 reads=["m_py%d" % hh, yk], writes=[yk])

            load_expert(0)
            load_expert(1)
            for it, (ex, blk) in enumerate(units):
                for st_ in range(4):
                    front(ex, blk, it, fcs=[st_])
                    if it > 0:
                        back(units[it - 1][0], units[it - 1][1], it - 1, tls=[st_])
                if it > 0:
                    pex, pblk = units[it - 1]
                    if pblk == TP // 512 - 1 and pex + 2 < NE:
                        load_expert(pex + 2)
                        if pre:
                            pre.pop(0)()
            back(units[-1][0], units[-1][1], len(units) - 1)
            while pre:
                pre.pop(0)()
            S.coarse = False
            if ps_ + 1 < NPASS:
                d3_pending = [(t0, i) for i in range(NT)]
            else:
                for i in range(NT):
                    d3_tile(t0, i)
    S.barrier()


def build(mixer=True, nlayers=NL, moe=True, nblocks=8, dbg_blk=None):
    nc = bass.Bass("TRN2", target_bir_lowering=False)
    io = Ctx()
    din = lambda name, shape, dt=F32: nc.dram_tensor(name, list(shape), dt, kind="ExternalInput").ap()
    io.x = din("x", [T, D]); io.c = din("c", [1, D]); io.pos = din("positions", [1, T], I32)
    io.ada_w = din("ada_w", [NL, D, 6 * D]); io.ada_b = din("ada_b", [NL, 6 * D])
    io.norm1_g = din("norm1_g", [NL, D]); io.norm2_g = din("norm2_g", [NL, D])
    io.w_in = din("w_in", [NL, D, IN_COLS])
    io.hg_lb_table = din("hg_lb_table", [NL, 512]); io.hg_norm_w = din("hg_norm_w", [NL, 128])
    io.rw_mu = din("rw_mu", [NL, 1792]); io.rw_w0 = din("rw_w0", [NL, 512]); io.rw_w2 = din("rw_w2", [NL, 64, 512])
    io.rw_a0 = din("rw_a0", [NL, 512]); io.rw_a2 = din("rw_a2", [NL, 64, 512]); io.rw_g2 = din("rw_g2", [NL, 128, 512])
    io.rw_k_k = din("rw_k_k", [NL, 512]); io.rw_k_a = din("rw_k_a", [NL, 512]); io.rw_r_k = din("rw_r_k", [NL, 512])
    io.rw_ln_w = din("rw_ln_w", [NL, 512]); io.rw_ln_b = din("rw_ln_b", [NL, 512])
    io.br_hg = din("br_hg", [NL, 512, D]); io.br_ret = din("br_ret", [NL, 512, D]); io.br_rw = din("br_rw", [NL, 512, D])
    io.w_out = din("w_out", [NL, D, D])
    io.router_g = din("router_g", [NL, D, 4]); io.router_e = din("router_e", [NL, D, 32])
    io.moe_w1 = din("moe_w1", [NL, NE, D, DE]); io.moe_w3 = din("moe_w3", [NL, NE, D, DE])
    io.moe_w2 = din("moe_w2", [NL, NE, DE, D])
    io.final_g = din("final_g", [1, D])
    io.out = nc.dram_tensor("out", [T, D], F32, kind="ExternalOutput").ap()
    io.modrow = nc.dram_tensor("modrow", [NL, 6 * D], F32, kind="Internal").ap()
    io.xa = nc.dram_tensor("xa", [T, D], F32, kind="Internal").ap()
    io.xb = nc.dram_tensor("xb", [T, D], F32, kind="Internal").ap()
    io.wbf = nc.dram_tensor("wbf", [NL, 23, 128, 4096], BF16, kind="Internal").ap()
    g = Ctx()
    with contextlib.ExitStack() as st:
        S = Sched(nc, st)
        _consts(S, nc, st, g)
        dbg = None
        if dbg_blk is not None:
            dout = nc.dram_tensor("dbg_y", [3, 128, 4, 512], F32, kind="ExternalOutput").ap()
            dbg = (dout, dbg_blk)
        if mixer:
            for f_ in _precast_mixer_weights(S, io, 0):
                f_()
        _phase0(S, nc, g, io)
        cur, ck = io.x, "x_in"
        for l in range(nlayers):
            last = (l == nlayers - 1)
            if mixer:
                mdst, mk = (io.out, "out") if (last and not moe) else (io.xa, "xa")
                _mixer_phase(S, nc, g, io, l, cur, ck, mdst, mk, nblocks=nblocks, dbg=dbg if l == 0 else None)
                cur, ck = mdst, mk
            if moe:
                dst, dk = (io.out, "out") if last else (io.xb, "xb")
                pre = _precast_mixer_weights(S, io, l + 1) if (mixer and not last) else None
                _moe_phase(S, nc, g, io, l, cur, ck, dst, dk, final=last, pre=pre)
                cur, ck = dst, dk
        S.barrier()
        print("instructions", S.n_inst, "waits", S.n_wait, "dma sems", len(S.dsem), "barriers", getattr(S, "nbar", 0))
    return nc


def _mixer_consts(S, nc, st, g):
    sb = lambda name, shape, dt=F32: st.enter_context(nc.sbuf_tensor(_un(name), list(shape), dt))
    g.blk = sb("c_blk", [128, 128])
    S.op("pool", lambda e: e.memset(g.blk[:], 0.0), writes=["c_blk"])
    S.op("pool", lambda e: e.memset(g.blk[0:64, 0:64], 1.0), reads=["c_blk"], writes=["c_blk"])
    S.op("pool", lambda e: e.memset(g.blk[64:128, 64:128], 1.0), reads=["c_blk"], writes=["c_blk"])
    g.mge = sb("c_mge", [128, 64])
    g.m4 = sb("c_m4", [128, 256])
    S.op("pool", lambda e: e.memset(g.mge[:], 1.0), writes=["c_mge"])
    S.op("pool", lambda e: e.memset(g.m4[:], 1.0), writes=["c_m4"])
    for hh in range(2):
        rows = slice(hh * 64, hh * 64 + 64)
        S.op("pool", lambda e, rows=rows: e.affine_select(out=g.mge[rows, :], in_=g.mge[rows, :], pattern=[[1, 64]],
                                                          compare_op=ALU.is_ge, fill=0.0, base=0, channel_multiplier=-1),
             reads=["c_mge"], writes=["c_mge"])
        for q in range(4):
            op = ALU.is_gt if q % 2 == 0 else ALU.is_ge
            S.op("pool", lambda e, rows=rows, q=q, op=op: e.affine_select(
                out=g.m4[rows, q * 64:(q + 1) * 64], in_=g.m4[rows, q * 64:(q + 1) * 64], pattern=[[1, 64]],
                compare_op=op, fill=0.0, base=0, channel_multiplier=-1), reads=["c_m4"], writes=["c_m4"])
    g.blkb = sb("c_blkb", [128, 128], BF16); g.onesb = sb("c_onesb", [128, 128], BF16)
    S.op("dve", lambda e: e.tensor_copy(out=g.blkb[:], in_=g.blk[:]), reads=["c_blk"], writes=["c_blkb"])
    S.op("dve", lambda e: e.tensor_copy(out=g.onesb[:], in_=g.ones[:]), reads=["ones"], writes=["c_onesb"])
    g.mgei = sb("c_mgei", [128, 64], I32)
    S.op("dve", lambda e: e.tensor_copy(out=g.mgei[:], in_=g.mge[:]), reads=["c_mge"], writes=["c_mgei"])
    g.rm = sb("c_rm", [128, 512])
    S.op("pool", lambda e: e.memset(g.rm[:], 1.0), writes=["c_rm"])
    S.op("pool", lambda e: e.memset(g.rm[:].rearrange("p (c j) -> p c j", j=64)[:, :, 0:1], 0.0),
         reads=["c_rm"], writes=["c_rm"])
    g.J = sb("c_J", [128, 128]); g.Ji = sb("c_Ji", [128, 128], I32)
    g.DF = sb("c_DF", [128, 128])
    S.op("pool", lambda e: e.iota(g.Ji[:], pattern=[[1, 128]], base=0, channel_multiplier=0), writes=["c_Ji"])
    S.op("dve", lambda e: e.tensor_copy(out=g.J[:], in_=g.Ji[:]), reads=["c_Ji"], writes=["c_J"])
    S.op("pool", lambda e: e.iota(g.Ji[:], pattern=[[1, 128]], base=0, channel_multiplier=-1), reads=["c_Ji"], writes=["c_Ji"])
    S.op("dve", lambda e: e.tensor_copy(out=g.DF[:], in_=g.Ji[:]), reads=["c_Ji"], writes=["c_DF"])
    g.XI = sb("c_XI", [128, 4, 128]); g.ZE = sb("c_ZE", [128, 4, 128]); g.DM = sb("c_DM", [128, 4, 128])
    g.lng = [math.log(1.0 - 2.0 ** (-5.0 - h)) for h in range(4)]
    g.cb = sb("c_cb", [128, 16])
    for h in range(4):
        lg = g.lng[h]
        S.op("pool", lambda e, h=h, lg=lg: e.memset(g.cb[:, h:h + 1], lg), reads=["c_cb"], writes=["c_cb"])
        S.op("pool", lambda e, h=h, lg=lg: e.memset(g.cb[:, 4 + h:5 + h], 127.0 * lg), reads=["c_cb"], writes=["c_cb"])
    for h in range(4):
        lg = g.lng[h]
        S.op("act", lambda e, h=h, lg=lg: e.activation(out=g.XI[:, h, :], in_=g.J[:], func=AF.Exp, scale=lg, bias=g.cb[:, h:h + 1]),
             reads=["c_J", "c_cb"], writes=["c_XI"])
        S.op("act", lambda e, h=h, lg=lg: e.activation(out=g.ZE[:, h, :], in_=g.J[:], func=AF.Exp, scale=-lg, bias=g.cb[:, 4 + h:5 + h]),
             reads=["c_J", "c_cb"], writes=["c_ZE"])
        S.op("act", lambda e, h=h, lg=lg: e.activation(out=g.DM[:, h, :], in_=g.DF[:], func=AF.Exp, scale=lg),
             reads=["c_DF"], writes=["c_DM"])
        S.op("pool", lambda e, h=h: e.affine_select(out=g.DM[:, h, :], in_=g.DM[:, h, :], pattern=[[1, 128]],
                                                    compare_op=ALU.is_ge, fill=0.0, base=0, channel_multiplier=-1),
             reads=["c_DM"], writes=["c_DM"])
    g.invf = sb("c_invf", [128, 1]); g.pm = sb("c_pm", [128, 1], I32); g.sgn = sb("c_sgn", [128, 1])
    for hh in range(2):
        rows = slice(hh * 64, hh * 64 + 64)
        S.op("pool", lambda e, rows=rows: e.iota(g.pm[rows, :], pattern=[[0, 1]], base=0, channel_multiplier=1),
             reads=["c_pm"], writes=["c_pm"])
    S.op("dve", lambda e: e.tensor_copy(out=g.invf[:], in_=g.pm[:]), reads=["c_pm"], writes=["c_invf"])
    S.op("act", lambda e: e.activation(out=g.invf[:], in_=g.invf[:], func=AF.Exp, scale=-math.log(10000.0) / 64.0),
         reads=["c_invf"], writes=["c_invf"])
    S.op("pool", lambda e: e.memset(g.sgn[0:64, :], -1.0), reads=["c_sgn"], writes=["c_sgn"])
    S.op("pool", lambda e: e.memset(g.sgn[64:128, :], 1.0), reads=["c_sgn"], writes=["c_sgn"])


def _mixer_wlist(io, l):
    wsrc = lambda c0, n=512: (io.w_in[l, :, c0:c0 + n], 8, n)
    wlist = [wsrc(0), wsrc(512), wsrc(1024), wsrc(1536), wsrc(2048), wsrc(2560), wsrc(3072), wsrc(3584),
             wsrc(4096 + 1536, 256), wsrc(4096), wsrc(4608), wsrc(5120)]
    for b_, brn_ in enumerate((io.br_hg, io.br_ret, io.br_rw)):
        wlist += [(brn_[l], 4, 1024), wsrc(5888 + b_ * 1024), wsrc(5888 + b_ * 1024 + 512)]
    wlist += [(io.w_out[l, :, 0:512], 8, 512), (io.w_out[l, :, 512:1024], 8, 512)]
    return wlist


def _precast_mixer_weights(S, io, l):
    def mk(j, src, kp, ncols):
        def f():
            dst = io.wbf[l, j, :, 0:kp * ncols].rearrange("p (k n) -> p k n", k=kp)
            S.dma("pool", dst, src.rearrange("(k p) n -> p k n", p=128), writes=["wbf%d" % l])
        return f
    return [mk(j, src, kp, ncols) for j, (src, kp, ncols) in enumerate(_mixer_wlist(io, l))]


def _mixer_phase(S, nc, g, io, l, x_src, xs_key, x_dst, xd_key, nblocks=8, dbg=None):
    BT = 512
    E05 = math.exp(-0.5)
    with contextlib.ExitStack() as st:
        sb = lambda name, shape, dt=F32: st.enter_context(nc.sbuf_tensor(_un(name), list(shape), dt))
        pst_ = lambda name, shape, dt=F32: (S.psum_keys.add(name), st.enter_context(nc.psum_tensor(_un(name), list(shape), dt)))[1]
        V = lambda fn, r, w: S.op("dve", fn, reads=r, writes=w)
        A = lambda fn, r, w: S.op("act", fn, reads=r, writes=w)
        P = lambda fn, r, w: S.op("pe", fn, reads=r, writes=w)
        G = lambda fn, r, w: S.op("pool", fn, reads=r, writes=w)
        _mixer_consts(S, nc, st, g)
        A1 = sb("x_A1", [128, D]); B1 = sb("x_B1", [128, D])
        xt = [sb("x_xt%d" % i, [128, D]) for i in range(2)]
        hf = sb("x_hf", [128, D]); hb = sb("x_hb", [128, D], BF16)
        ss = sb("x_ss", [128, 1]); rstd = sb("x_rstd", [128, 1])
        hT = sb("x_hT", [128, 8, BT], BF16)
        wb = [sb("x_wb%d" % i, [128, 4096], BF16) for i in range(3)]
        yT = [sb("x_yT%d" % i, [128, 4, BT], BF16) for i in range(3)]
        ymix = sb("x_ymix", [128, 8, BT])
        ymixb = sb("x_ymixb", [128, 8, BT], BF16)
        cols = sb("x_cols", [128, 64])
        lbt = sb("x_lbt", [128, 8])
        f32t = [sb("x_f%d" % i, [128, BT]) for i in range(6)]
        f513 = sb("x_f513", [128, BT + 1])
        posi = f32t[5][:].bitcast(I32)
        carry = sb("x_carry", [128, 16])
        bq = [sb("x_bq%d" % i, [128, 4, BT], BF16) for i in range(6)]
        vtm = sb("x_vtm", [128, 4, 512], BF16)
        Sst = sb("x_S", [128, 4, 128]); Sbf = sb("x_Sbf", [128, 4, 128], BF16)
        Rst = sb("x_R", [128, 4, 128]); Rbf = sb("x_Rbf", [128, 4, 128], BF16)
        Wst = sb("x_Wst", [128, 4, 64])
        dS = sb("x_dS", [128, 4, 8]); dSr = sb("x_dSr", [128, 4, 8])
        sc4 = sb("x_sc4", [128, 4, 128], BF16)
        schg = sb("x_schg", [128, 4, 64], BF16)
        ktm = sb("x_ktm", [128, 4, 128], BF16)
        cosT = sb("x_cos", [128, BT]); sinT = sb("x_sin", [128, BT])
        lora = [sb("x_lora0", [128, BT], BF16), sb("x_lora1", [128, BT], BF16)]
        w2a2 = sb("x_w2a2", [128, 512], BF16); g2t = sb("x_g2", [128, 512], BF16)
        rw = [sb("x_rw%d" % i, [128, BT]) for i in range(3)]
        ARs = [sb("x_AR%d" % p, [128, 8, 128], BF16) for p in range(4)]
        bts = [sb("x_bt%d" % p, [128, BT], BF16) for p in range(4)]
        kts = [sb("x_kt%d" % p, [128, BT], BF16) for p in range(4)]
        vbs = [sb("x_vb%d" % p, [128, BT], BF16) for p in range(4)]
        bons = [sb("x_bon%d" % p, [128, BT], BF16) for p in range(4)]
        ABKs = [sb("x_ABK%d" % p, [128, 256], BF16) for p in range(4)]
        MTs = [sb("x_MT%d" % p, [128, 128], BF16) for p in range(4)]
        MNs = [[sb("x_MN%d_%d" % (p, i), [128, 128], BF16) for i in range(2)] for p in range(4)]
        NPs = [[MTs[p], sb("x_NP%d_1" % p, [128, 128], BF16)] for p in range(4)]
        Xs = [sb("x_X%d" % p, [128, 128], BF16) for p in range(4)]
        Wbf = sb("x_Wbf", [128, 4, 64], BF16)
        TMss = [sb("x_TMs%d" % p, [128, 192], BF16) for p in range(4)]
        Wsbs = [sb("x_Wsb%d" % p, [128, 64], BF16) for p in range(4)]
        Usbs = [sb("x_Usb%d" % p, [128, 64], BF16) for p in range(4)]
        tSs = [sb("x_tS%d" % p, [128, 64]) for p in range(4)]
        pz = [pst_("x_pz%d" % i, [128, 512]) for i in range(2)]
        ptb = pst_("x_ptb", [128, 8, 128], BF16)
        pw = [pst_("x_pw%d" % i, [128, 512]) for i in range(4)]
        psc, po, pss = pw[0], pw[1], pw[2]

        _load_mod(S, nc, g, io, l, 1, io.norm1_g[l:l + 1, :], A1[:], "x_A1", B1[:], "x_B1", None, None, hf[:], "x_hf")
        CK = "x_cols"
        colv = lambda v, n: v.rearrange("o (j p) -> p (o j)", p=128)
        nslow = dict(allow_slow_non_contiguous=True)
        S.dma("sp", cols[:, 0:14], colv(io.rw_mu[l:l + 1, :], 14), writes=[CK], **nslow)
        for i, nm in enumerate(["rw_w0", "rw_a0", "rw_k_k", "rw_k_a", "rw_r_k", "rw_ln_w", "rw_ln_b"]):
            S.dma("sp", cols[:, 16 + 4 * i:20 + 4 * i], colv(getattr(io, nm)[l:l + 1, :], 4), reads=[CK], writes=[CK], **nslow)
        c_w0, c_a0, c_kk, c_ka, c_rk, c_lw, c_lb = [lambda p, i=i: cols[:, 16 + 4 * i + p:17 + 4 * i + p] for i in range(7)]
        c_omka = lambda p: cols[:, 44 + p:45 + p]
        V(lambda e: e.tensor_scalar(out=cols[:, 44:48], in0=cols[:, 28:32], scalar1=-1.0, scalar2=1.0, op0=ALU.mult, op1=ALU.add), [CK], [CK])
        S.dma("sp", cols[:, 48:49], io.hg_norm_w[l:l + 1, :].rearrange("o p -> p o"), reads=[CK], writes=[CK], **nslow)
        S.dma("sp", lbt[:, 0:4], colv(io.hg_lb_table[0:1, :], 4), writes=["x_lbt"], **nslow)
        S.dma("sp", lbt[:, 4:8], colv(io.hg_lb_table[1:2, :], 4), reads=["x_lbt"], writes=["x_lbt"], **nslow)
        c_lbv = lambda h: cols[:, 52 + h:53 + h]
        c_oml = lambda h: cols[:, 56 + h:57 + h]
        if l == 0:
            V(lambda e: e.memset(cols[:, 52:56], 0.0), [CK], [CK])
        else:
            V(lambda e: e.tensor_tensor(out=cols[:, 52:56], in0=lbt[:, 4:8], in1=lbt[:, 0:4], op=ALU.subtract), ["x_lbt", CK], [CK])
            A(lambda e: e.activation(out=cols[:, 52:56], in_=cols[:, 52:56], func=AF.Sigmoid), [CK], [CK])
        V(lambda e: e.tensor_scalar(out=cols[:, 56:60], in0=cols[:, 52:56], scalar1=-1.0, scalar2=1.0, op0=ALU.mult, op1=ALU.add), [CK], [CK])
        S.dma("pool", w2a2[0:64, :], io.rw_w2[l], writes=["x_w2a2"])
        S.dma("pool", w2a2[64:128, :], io.rw_a2[l], reads=["x_w2a2"], writes=["x_w2a2"])
        S.dma("pool", g2t[:], io.rw_g2[l], writes=["x_g2"])
        for p in range(4):
            G(lambda e, p=p: e.memset(Wbf[:, p, :], 0.0), [], ["x_Wbf%d" % p])
            G(lambda e, p=p: e.memset(Wst[:, p, :], 0.0), [], ["x_Wst%d" % p])
            G(lambda e, p=p: e.memset(MTs[p][:], 0.0), [], ["x_MT%d" % p])
        G(lambda e: e.memset(carry[:], 0.0), [], ["x_carry"])
        for h_ in range(4):
            G(lambda e, h_=h_: e.memset(Rst[:, h_, :], 0.0), [], ["x_R%d" % h_])
            G(lambda e, h_=h_: e.memset(Rbf[:, h_, :], 0.0), [], ["x_Rbf%d" % h_])
        for h_ in range(4):
            G(lambda e, h_=h_: e.memset(schg[:, h_, :], 0.0), [], ["x_schg%d" % h_])
            G(lambda e, h_=h_: e.memset(Sst[:, h_, :], 0.0), [], ["x_S%d" % h_])
            G(lambda e, h_=h_: e.memset(Sbf[:, h_, :], 0.0), [], ["x_Sbf%d" % h_])

        wlist = _mixer_wlist(io, l)
        NW = len(wlist)
        wst = {"issued": 0, "got": 0, "rel": -1, "total": NW * nblocks}

        def w_issue():
            j = wst["issued"]; wst["issued"] += 1
            src, kparts, ncols = wlist[j % NW]
            i = j % 3
            S.dma("sp", wb[i][:, 0:kparts * ncols], io.wbf[l, j % NW, :, 0:kparts * ncols],
                  reads=["wbf%d" % l], writes=["x_wb%d" % i])

        def w_pump():
            while wst["issued"] < wst["total"] and wst["issued"] <= wst["got"] + 1 and wst["issued"] - 3 <= wst["rel"]:
                w_issue()

        def w_get():
            j = wst["got"]; wst["got"] += 1
            while wst["issued"] <= j:
                assert wst["issued"] - 3 <= wst["rel"], "weight buffer still live"
                w_issue()
            src, kparts, ncols = wlist[j % NW]
            i = j % 3
            view = wb[i][:, 0:kparts * ncols].rearrange("p (k n) -> p k n", k=kparts)
            return view, "x_wb%d" % i

        def w_rel(n=1):
            wst["rel"] += n
            w_pump()

        pzc = [0]

        def proj_fm(W, wk, c0, nc_=128, row0=0, same=False):
            if same:
                i = (pzc[0] - 1) % 2
            else:
                i = pzc[0] % 2
                pzc[0] += 1
            for k in range(8):
                P(lambda e, k=k, i=i: e.matmul(pz[i][row0:row0 + nc_, :], lhsT=W[:, k, c0:c0 + nc_], rhs=hT[:, k, :],
                                               start=(k == 0), stop=(k == 7)), [wk, "x_hT"], ["x_pz%d" % i])
            return pz[i], "x_pz%d" % i

        def wgrp(c0, ncols=512):
            return w_get()

        for blk in range(nblocks):
            t0 = blk * BT
            for i in range(4):
                xb = xt[i % 2]; xk = "x_xt%d" % (i % 2)
                S.dma("sp", xb[:], x_src[t0 + i * 128:t0 + (i + 1) * 128, :], reads=[xs_key], writes=[xk])
                _rms_rstd(S, g, xb[:], xk, hf[:], "x_hf", ss[:], rstd[:], "x_")
                V(lambda e, xb=xb: e.scalar_tensor_tensor(out=hf[:], in0=xb[:], scalar=rstd[:], in1=A1[:], op0=ALU.mult, op1=ALU.mult),
                  [xk, "x_rstd", "x_A1"], ["x_hf"])
                V(lambda e: e.tensor_tensor(out=hb[:], in0=hf[:], in1=B1[:], op=ALU.add), ["x_hf", "x_B1"], ["x_hb"])
                for k in range(8):
                    P(lambda e, k=k: e.transpose(ptb[:, k, :], hb[:, k * 128:(k + 1) * 128], g.identb[:]), ["x_hb", "identb"], ["x_ptb"])
                A(lambda e, i=i: e.copy(out=hT[:, :, i * 128:(i + 1) * 128], in_=ptb[:]), ["x_ptb"], ["x_hT"])

            qsil, qh, qs_, kh, kt_, sgt = bq
            qk = ["x_bq%d" % i for i in range(6)]
            W, wk = wgrp(0)
            for h in range(4):
                pzt, pk = proj_fm(W, wk, h * 128)
                A(lambda e, h=h, pzt=pzt: e.activation(out=qsil[:, h, :], in_=pzt[:], func=AF.Silu), [pk], [qk[0]])
            w_rel()
            W, wk = wgrp(512)
            lf, cum, tmpa, tmpb, kf = f32t[0], f32t[1], f32t[2], f32t[3], f32t[4]
            c3 = lambda tl: tl[:].rearrange("p (c j) -> p c j", j=64)
            for h in range(4):
                pzt, pk = proj_fm(W, wk, h * 128)
                A(lambda e, pzt=pzt: e.activation(out=tmpa[:], in_=pzt[:], func=AF.Sigmoid), [pk], ["x_f2"])
                V(lambda e, h=h: e.tensor_scalar(out=tmpa[:], in0=tmpa[:], scalar1=c_oml(h), scalar2=c_lbv(h), op0=ALU.mult, op1=ALU.add),
                  ["x_f2", CK], ["x_f2"])
                A(lambda e: e.activation(out=lf[:], in_=tmpa[:], func=AF.Ln), ["x_f2"], ["x_f0"])
                V(lambda e: e.tensor_scalar(out=kf[:], in0=tmpa[:], scalar1=-1.0, scalar2=1.0, op0=ALU.mult, op1=ALU.add), ["x_f2"], ["x_f4"])
                V(lambda e: e.tensor_tensor_scan(out=cum[:], data0=g.rm[:], data1=lf[:], initial=0.0, op0=ALU.mult, op1=ALU.add),
                  ["c_rm", "x_f0"], ["x_f1"])
                V(lambda e: e.tensor_tensor(out=c3(tmpa), in0=c3(cum), in1=c3(cum)[:, :, 31:32].to_broadcast([128, 8, 64]), op=ALU.subtract),
                  ["x_f1"], ["x_f2"])
                A(lambda e: e.activation(out=tmpb[:], in_=tmpa[:], func=AF.Exp), ["x_f2"], ["x_f3"])
                V(lambda e, h=h: e.scalar_tensor_tensor(out=qh[:, h, :], in0=qsil[:, h, :], scalar=128.0 ** -0.5, in1=tmpb[:], op0=ALU.mult, op1=ALU.mult),
                  [qk[0], "x_f3"], [qk[1]])
                A(lambda e: e.activation(out=tmpb[:], in_=tmpa[:], func=AF.Exp, scale=-1.0), ["x_f2"], ["x_f3"])
                V(lambda e, h=h: e.tensor_tensor(out=kh[:, h, :], in0=kf[:], in1=tmpb[:], op=ALU.mult), ["x_f4", "x_f3"], [qk[3]])
                A(lambda e: e.activation(out=tmpb[:], in_=cum[:], func=AF.Exp), ["x_f1"], ["x_f3"])
                V(lambda e, h=h: e.scalar_tensor_tensor(out=qs_[:, h, :], in0=qsil[:, h, :], scalar=128.0 ** -0.5, in1=tmpb[:], op0=ALU.mult, op1=ALU.mult),
                  [qk[0], "x_f3"], [qk[2]])
                V(lambda e, h=h: e.tensor_copy(out=dS[:, h, :], in_=c3(tmpb)[:, :, 63]), ["x_f3"], ["x_dS"])
                V(lambda e: e.tensor_tensor(out=c3(tmpa), in0=c3(cum)[:, :, 63:64].to_broadcast([128, 8, 64]), in1=c3(cum), op=ALU.subtract),
                  ["x_f1"], ["x_f2"])
                A(lambda e: e.activation(out=tmpb[:], in_=tmpa[:], func=AF.Exp), ["x_f2"], ["x_f3"])
                V(lambda e, h=h: e.tensor_tensor(out=kt_[:, h, :], in0=kf[:], in1=tmpb[:], op=ALU.mult), ["x_f4", "x_f3"], [qk[4]])
            w_rel()
            W, wk = wgrp(1024)
            for i in range(4):
                pi_ = pzc[0] % 2; pzc[0] += 1
                for k in range(8):
                    P(lambda e, k=k, i=i, pi_=pi_: e.matmul(pz[pi_][:], lhsT=hT[:, k, i * 128:(i + 1) * 128], rhs=W[:, k, :], start=(k == 0), stop=(k == 7)),
                      [wk, "x_hT"], ["x_pz%d" % pi_])
                A(lambda e, i=i, pi_=pi_: e.copy(out=vtm[:, i, :], in_=pz[pi_][:]), ["x_pz%d" % pi_], ["x_vtm"])
            w_rel()
            W, wk = wgrp(1536)
            for h in range(4):
                pzt, pk = proj_fm(W, wk, h * 128)
                A(lambda e, h=h, pzt=pzt: e.activation(out=sgt[:, h, :], in_=pzt[:], func=AF.Silu), [pk], [qk[5]])
            w_rel()
            def rope_gen():
                S.dma("sp", posi, io.pos[0:1, t0:t0 + BT].to_broadcast([128, BT]), writes=["x_f5"])
                ang, rr, nf, mm = f32t[0], f32t[1], f32t[2], f32t[3]
                V(lambda e: e.tensor_copy(out=ang[:], in_=posi), ["x_f5"], ["x_f0"])
                yield
                V(lambda e: e.tensor_scalar(out=ang[:], in0=ang[:], scalar1=g.invf[:], scalar2=None, op0=ALU.mult), ["x_f0", "c_invf"], ["x_f0"])
                yield
                for which in range(2):
                    dst, dk_ = (sinT, "x_sin") if which == 0 else (cosT, "x_cos")
                    off = 0.0 if which == 0 else PI / 2
                    V(lambda e, off=off: e.tensor_scalar(out=rr[:], in0=ang[:], scalar1=off, scalar2=None, op0=ALU.add), ["x_f0"], ["x_f1"])
                    yield
                    V(lambda e: e.tensor_scalar(out=posi, in0=rr[:], scalar1=1.0 / (2 * PI), scalar2=None, op0=ALU.mult), ["x_f1"], ["x_f5"])
                    yield
                    V(lambda e: e.tensor_copy(out=nf[:], in_=posi), ["x_f5"], ["x_f2"])
                    yield
                    V(lambda e: e.scalar_tensor_tensor(out=rr[:], in0=nf[:], scalar=-2 * PI, in1=rr[:], op0=ALU.mult, op1=ALU.add), ["x_f2", "x_f1"], ["x_f1"])
                    yield
                    V(lambda e: e.tensor_scalar(out=mm[:], in0=rr[:], scalar1=PI, scalar2=None, op0=ALU.is_gt), ["x_f1"], ["x_f3"])
                    yield
                    V(lambda e: e.scalar_tensor_tensor(out=rr[:], in0=mm[:], scalar=-2 * PI, in1=rr[:], op0=ALU.mult, op1=ALU.add), ["x_f3", "x_f1"], ["x_f1"])
                    yield
                    V(lambda e: e.tensor_scalar(out=mm[:], in0=rr[:], scalar1=-PI, scalar2=None, op0=ALU.is_lt), ["x_f1"], ["x_f3"])
                    yield
                    V(lambda e: e.scalar_tensor_tensor(out=rr[:], in0=mm[:], scalar=2 * PI, in1=rr[:], op0=ALU.mult, op1=ALU.add), ["x_f3", "x_f1"], ["x_f1"])
                    yield
                    V(lambda e: e.tensor_scalar(out=rr[:], in0=rr[:], scalar1=3.1415925, scalar2=-3.1415925, op0=ALU.min, op1=ALU.max), ["x_f1"], ["x_f1"])
                    yield
                    if which == 0:
                        A(lambda e, dst=dst: e.activation(out=dst[:], in_=rr[:], func=AF.Sin, scale=g.sgn[:]), ["x_f1", "c_sgn"], [dk_])
                        yield
                    else:
                        A(lambda e, dst=dst: e.activation(out=dst[:], in_=rr[:], func=AF.Sin), ["x_f1"], [dk_])
                        yield
                yield

            rg = rope_gen()
            for c in range(8):
                par = c % 2; rows = slice(par * 64, par * 64 + 64); cs = slice(c * 64, c * 64 + 64); tl = c // 2
                for h in range(4):
                    P(lambda e, h=h: e.matmul(pz[0][rows, h * 64:(h + 1) * 64], lhsT=kh[:, h, cs], rhs=qh[:, h, cs], start=True, stop=True),
                      [qk[3], qk[1]], ["x_pz0"])
                for h in range(4):
                    V(lambda e, h=h: e.copy_predicated(out=schg[rows, h, :], mask=g.mgei[rows, :], data=pz[0][rows, h * 64:(h + 1) * 64]),
                      ["x_pz0", "c_mgei"], ["x_schg%d" % h])
                for h in range(4):
                    P(lambda e, h=h: e.matmul(pw[h][:, cs], lhsT=vtm[rows, tl, h * 128:(h + 1) * 128], rhs=schg[rows, h, :], start=True, stop=False),
                      ["x_vtm", "x_schg%d" % h], ["x_pw%d" % h])
                    P(lambda e, h=h: e.matmul(pw[h][:, cs], lhsT=Sbf[:, h, :], rhs=qs_[:, h, cs], start=False, stop=True),
                      ["x_Sbf%d" % h, qk[2]], ["x_pw%d" % h])
                for h in range(4):
                    P(lambda e, h=h: e.transpose(ptb[rows, h, :], kt_[:, h, cs], g.identb[:]), [qk[4], "identb"], ["x_ptb"])
                for h in range(4):
                    A(lambda e, h=h: e.copy(out=ktm[rows, h, :], in_=ptb[rows, h, :]), ["x_ptb"], ["x_ktm%d" % h])
                for h in range(4):
                    P(lambda e, h=h: e.matmul(pz[1][:, h * 128:(h + 1) * 128], lhsT=ktm[rows, h, :], rhs=vtm[rows, tl, h * 128:(h + 1) * 128], start=True, stop=True),
                      ["x_ktm%d" % h, "x_vtm"], ["x_pz1"])
                for h in range(4):
                    V(lambda e, h=h: e.scalar_tensor_tensor(out=Sst[:, h, :], in0=Sst[:, h, :], scalar=dS[:, h, c:c + 1], in1=pz[1][:, h * 128:(h + 1) * 128], op0=ALU.mult, op1=ALU.add),
                      ["x_S%d" % h, "x_dS", "x_pz1"], ["x_S%d" % h])
                for h in range(4):
                    A(lambda e, h=h: e.copy(out=Sbf[:, h, :], in_=Sst[:, h, :]), ["x_S%d" % h], ["x_Sbf%d" % h])
                for _ in range(4):
                    next(rg, None)
            for _ in rg:
                pass
            for h in range(4):
                A(lambda e, h=h: e.activation(out=bq[0][:, h, :], in_=pw[h][:], func=AF.Square), ["x_pw%d" % h], ["x_bq0"])
            for h in range(4):
                P(lambda e, h=h: e.matmul(pz[h % 2][:], lhsT=g.onesb[:], rhs=bq[0][:, h, :], start=True, stop=True), ["c_onesb", "x_bq0"], ["x_pz%d" % (h % 2)])
                V(lambda e, h=h: e.tensor_scalar(out=f32t[h][:], in0=pz[h % 2][:], scalar1=1.0 / 128, scalar2=EPS, op0=ALU.mult, op1=ALU.add), ["x_pz%d" % (h % 2)], ["x_f%d" % h])
            for h in range(4):
                A(lambda e, h=h: e.activation(out=f32t[h][:], in_=f32t[h][:], func=AF.Sqrt), ["x_f%d" % h], ["x_f%d" % h])
            for h in range(4):
                V(lambda e, h=h: e.reciprocal(out=f32t[h][:], in_=f32t[h][:]), ["x_f%d" % h], ["x_f%d" % h])
            for h in range(4):
                V(lambda e, h=h: e.tensor_tensor(out=f32t[h][:], in0=pw[h][:], in1=f32t[h][:], op=ALU.mult), ["x_pw%d" % h, "x_f%d" % h], ["x_f%d" % h])
            for h in range(4):
                V(lambda e, h=h: e.scalar_tensor_tensor(out=yT[0][:, h, :], in0=f32t[h][:], scalar=cols[:, 48:49], in1=sgt[:, h, :], op0=ALU.mult, op1=ALU.mult),
                  ["x_f%d" % h, CK, qk[5]], ["x_yT0"])

            qr, qx, kr, kz, sgr = bq[0], bq[1], bq[2], bq[3], bq[4]
            c4 = lambda ap: ap.rearrange("p (c j) -> p c j", j=128)
            for isk in range(2):
                if isk == 1:
                    w_rel()
                W, wk = wgrp(2048 + isk * 512)
                for h in range(4):
                    pzt, pk = proj_fm(W, wk, h * 128)
                    V(lambda e, pzt=pzt: e.tensor_tensor(out=f32t[4][:], in0=pzt[:], in1=cosT[:], op=ALU.mult), [pk, "x_cos"], ["x_f4"])
                    proj_fm(W, wk, h * 128 + 64, 64, 0)
                    pzr, pkr = proj_fm(W, wk, h * 128, 64, 64, same=True)
                    V(lambda e, pzr=pzr: e.tensor_tensor(out=f32t[5][:], in0=pzr[:], in1=sinT[:], op=ALU.mult), [pkr, "x_sin"], ["x_f5"])
                    V(lambda e: e.tensor_tensor(out=f32t[4][:], in0=f32t[4][:], in1=f32t[5][:], op=ALU.add), ["x_f4", "x_f5"], ["x_f4"])
                    if isk == 0:
                        A(lambda e, h=h: e.mul(out=qr[:, h, :], in_=f32t[4][:], mul=128.0 ** -0.5), ["x_f4"], [qk[0]])
                        V(lambda e, h=h: e.scalar_tensor_tensor(out=c4(qx[:, h, :]), in0=c4(f32t[4][:]), scalar=128.0 ** -0.5,
                                                                in1=g.XI[:, h:h + 1, :].to_broadcast([128, 4, 128]), op0=ALU.mult, op1=ALU.mult),
                          ["x_f4", "c_XI"], [qk[1]])
                    else:
                        A(lambda e, h=h: e.copy(out=kr[:, h, :], in_=f32t[4][:]), ["x_f4"], [qk[2]])
                        V(lambda e, h=h: e.tensor_tensor(out=c4(kz[:, h, :]), in0=c4(f32t[4][:]), in1=g.ZE[:, h:h + 1, :].to_broadcast([128, 4, 128]), op=ALU.mult),
                          ["x_f4", "c_ZE"], [qk[3]])
            w_rel()
            W, wk = wgrp(3072)
            for i in range(4):
                pi_ = pzc[0] % 2; pzc[0] += 1
                for k in range(8):
                    P(lambda e, k=k, i=i, pi_=pi_: e.matmul(pz[pi_][:], lhsT=hT[:, k, i * 128:(i + 1) * 128], rhs=W[:, k, :], start=(k == 0), stop=(k == 7)),
                      [wk, "x_hT"], ["x_pz%d" % pi_])
                A(lambda e, i=i, pi_=pi_: e.copy(out=vtm[:, i, :], in_=pz[pi_][:]), ["x_pz%d" % pi_], ["x_vtm"])
            w_rel()
            W, wk = wgrp(3584)
            for h in range(4):
                pzt, pk = proj_fm(W, wk, h * 128)
                A(lambda e, h=h, pzt=pzt: e.activation(out=sgr[:, h, :], in_=pzt[:], func=AF.Silu), [pk], [qk[4]])
            w_rel()
            gam = [math.exp(128.0 * g.lng[h]) for h in range(4)]
            scr = sc4
            for c in range(4):
                cs = slice(c * 128, c * 128 + 128)
                for h in range(4):
                    P(lambda e, h=h: e.matmul(pz[0][:, h * 128:(h + 1) * 128], lhsT=kr[:, h, cs], rhs=qr[:, h, cs], start=True, stop=True), [qk[2], qk[0]], ["x_pz0"])
                for h in range(4):
                    V(lambda e, h=h: e.tensor_tensor(out=scr[:, h, :], in0=pz[0][:, h * 128:(h + 1) * 128], in1=g.DM[:, h, :], op=ALU.mult), ["x_pz0", "c_DM"], ["x_sc4_%d" % h])
                for h in range(4):
                    P(lambda e, h=h: e.matmul(pw[h][:, cs], lhsT=vtm[:, c, h * 128:(h + 1) * 128], rhs=scr[:, h, :], start=True, stop=False),
                      ["x_vtm", "x_sc4_%d" % h], ["x_pw%d" % h])
                    P(lambda e, h=h: e.matmul(pw[h][:, cs], lhsT=Rbf[:, h, :], rhs=qx[:, h, cs], start=False, stop=True), ["x_Rbf%d" % h, qk[1]], ["x_pw%d" % h])
                for h in range(4):
                    P(lambda e, h=h: e.transpose(ptb[:, h, :], kz[:, h, cs], g.identb[:]), [qk[3], "identb"], ["x_ptb"])
                for h in range(4):
                    A(lambda e, h=h: e.copy(out=ktm[:, h, :], in_=ptb[:, h, :]), ["x_ptb"], ["x_ktm%d" % h])
                for h in range(4):
                    P(lambda e, h=h: e.matmul(pz[1][:, h * 128:(h + 1) * 128], lhsT=ktm[:, h, :], rhs=vtm[:, c, h * 128:(h + 1) * 128], start=True, stop=True),
                      ["x_ktm%d" % h, "x_vtm"], ["x_pz1"])
                for h in range(4):
                    V(lambda e, h=h: e.scalar_tensor_tensor(out=Rst[:, h, :], in0=Rst[:, h, :], scalar=gam[h], in1=pz[1][:, h * 128:(h + 1) * 128], op0=ALU.mult, op1=ALU.add),
                      ["x_R%d" % h, "x_pz1"], ["x_R%d" % h])
                for h in range(4):
                    A(lambda e, h=h: e.copy(out=Rbf[:, h, :], in_=Rst[:, h, :]), ["x_R%d" % h], ["x_Rbf%d" % h])
            for h in range(4):
                A(lambda e, h=h: e.activation(out=bq[0][:, h, :], in_=pw[h][:], func=AF.Square), ["x_pw%d" % h], ["x_bq0"])
            for h in range(4):
                P(lambda e, h=h: e.matmul(pz[h % 2][:], lhsT=g.onesb[:], rhs=bq[0][:, h, :], start=True, stop=True), ["c_onesb", "x_bq0"], ["x_pz%d" % (h % 2)])
                V(lambda e, h=h: e.tensor_scalar(out=f32t[h][:], in0=pz[h % 2][:], scalar1=1.0 / 128, scalar2=EPS, op0=ALU.mult, op1=ALU.add), ["x_pz%d" % (h % 2)], ["x_f%d" % h])
            for h in range(4):
                A(lambda e, h=h: e.activation(out=f32t[h][:], in_=f32t[h][:], func=AF.Sqrt), ["x_f%d" % h], ["x_f%d" % h])
            for h in range(4):
                V(lambda e, h=h: e.reciprocal(out=f32t[h][:], in_=f32t[h][:]), ["x_f%d" % h], ["x_f%d" % h])
            for h in range(4):
                V(lambda e, h=h: e.tensor_tensor(out=f32t[h][:], in0=pw[h][:], in1=f32t[h][:], op=ALU.mult), ["x_pw%d" % h, "x_f%d" % h], ["x_f%d" % h])
            for h in range(4):
                V(lambda e, h=h: e.tensor_tensor(out=yT[1][:, h, :], in0=f32t[h][:], in1=sgr[:, h, :], op=ALU.mult), ["x_f%d" % h, qk[4]], ["x_yT1"])

            def shifted(pzt, pk, j, dst, dkey):
                A(lambda e: e.copy(out=f513[:, 1:BT + 1], in_=pzt[:]), [pk], ["x_f513"])
                A(lambda e: e.copy(out=f513[:, 0:1], in_=carry[:, j:j + 1]), ["x_carry", "x_f513"], ["x_f513"])
                V(lambda e: e.tensor_tensor(out=dst[:], in0=f513[:, 0:BT], in1=f513[:, 1:BT + 1], op=ALU.subtract), ["x_f513"], [dkey])
                V(lambda e: e.scalar_tensor_tensor(out=dst[:], in0=dst[:], scalar=cols[:, j:j + 1], in1=f513[:, 1:BT + 1], op0=ALU.mult, op1=ALU.add),
                  [dkey, CK, "x_f513"], [dkey])
                A(lambda e: e.copy(out=carry[:, j:j + 1], in_=f513[:, BT:BT + 1]), ["x_f513"], ["x_carry"])

            W, wk = wgrp(4096 + 1536, 256)
            pzt, pk = proj_fm(W, wk, 0)
            shifted(pzt, pk, 12, lora[0], "x_lora0")
            A(lambda e: e.activation(out=lora[0][0:64, :], in_=lora[0][0:64, :], func=AF.Tanh), ["x_lora0"], ["x_lora0"])
            pzt, pk = proj_fm(W, wk, 128)
            shifted(pzt, pk, 13, lora[1], "x_lora1")
            A(lambda e: e.activation(out=lora[1][:], in_=lora[1][:], func=AF.Sigmoid), ["x_lora1"], ["x_lora1"])
            w_rel()
            Wr_, wkr = wgrp(4096)
            Wk_, wkk = wgrp(4096 + 512)
            Wv_, wkv = wgrp(4096 + 1024)
            rs, ks, vs = rw
            rk = ["x_rw0", "x_rw1", "x_rw2"]
            ar3 = lambda tl: tl[:].rearrange("p (c j) -> p c j", j=64)
            t0_, t1_, t2_, t3_ = f32t[0], f32t[1], f32t[2], f32t[3]
            pq = pw[3]; PQ = ["x_pw3"]
            for p in range(4):
                pc = slice(p * 128, p * 128 + 128)
                AR = ARs[p]; bt = bts[p]; kt = kts[p]; vb = vbs[p]; bon = bons[p]
                KAR = "x_AR%d" % p; KBT = "x_bt%d" % p; KKT = "x_kt%d" % p; KVB = "x_vb%d" % p; KBON = "x_bon%d" % p
                pzt, pk = proj_fm(Wr_, wkr, p * 128); shifted(pzt, pk, p, rs, rk[0])
                pzt, pk = proj_fm(Wk_, wkk, p * 128); shifted(pzt, pk, 4 + p, ks, rk[1])
                pzt, pk = proj_fm(Wv_, wkv, p * 128); shifted(pzt, pk, 8 + p, vs, rk[2])
                A(lambda e, vb=vb: e.copy(out=vb[:], in_=vs[:]), [rk[2]], [KVB])
                P(lambda e, pc=pc: e.matmul(pq[:], lhsT=w2a2[0:64, pc], rhs=lora[0][0:64, :], start=True, stop=True), ["x_w2a2", "x_lora0"], PQ)
                A(lambda e, p=p: e.activation(out=t0_[:], in_=pq[:], func=AF.Sigmoid, bias=c_w0(p)), PQ + [CK], ["x_f0"])
                P(lambda e, pc=pc: e.matmul(pq[:], lhsT=w2a2[64:128, pc], rhs=lora[0][64:128, :], start=True, stop=True), ["x_w2a2", "x_lora0"], PQ)
                A(lambda e, p=p: e.activation(out=t1_[:], in_=pq[:], func=AF.Sigmoid, bias=c_a0(p)), PQ + [CK], ["x_f1"])
                V(lambda e, p=p: e.tensor_scalar(out=t2_[:], in0=ks[:], scalar1=c_kk(p), scalar2=None, op0=ALU.mult), [rk[1], CK], ["x_f2"])
                A(lambda e, kt=kt: e.activation(out=kt[:], in_=t2_[:], func=AF.Square), ["x_f2"], [KKT])
                P(lambda e, kt=kt: e.matmul(pq[:], lhsT=g.blkb[:], rhs=kt[:], start=True, stop=True), ["c_blkb", KKT], PQ)
                V(lambda e: e.tensor_scalar(out=t3_[:], in0=pq[:], scalar1=1e-24, scalar2=None, op0=ALU.max), PQ, ["x_f3"])
                A(lambda e: e.activation(out=t3_[:], in_=t3_[:], func=AF.Sqrt), ["x_f3"], ["x_f3"])
                V(lambda e: e.reciprocal(out=t3_[:], in_=t3_[:]), ["x_f3"], ["x_f3"])
                V(lambda e: e.tensor_tensor(out=t2_[:], in0=t2_[:], in1=t3_[:], op=ALU.mult), ["x_f2", "x_f3"], ["x_f2"])
                V(lambda e, p=p: e.tensor_scalar(out=t3_[:], in0=t1_[:], scalar1=c_ka(p), scalar2=c_omka(p), op0=ALU.mult, op1=ALU.add), ["x_f1", CK], ["x_f3"])
                V(lambda e: e.tensor_tensor(out=ks[:], in0=ks[:], in1=t3_[:], op=ALU.mult), [rk[1], "x_f3"], [rk[1]])
                V(lambda e, p=p, bt=bt: e.scalar_tensor_tensor(out=bt[:], in0=rs[:], scalar=c_rk(p), in1=ks[:], op0=ALU.mult, op1=ALU.mult), [rk[0], CK, rk[1]], [KBT])
                P(lambda e, bt=bt: e.matmul(pq[:], lhsT=g.blkb[:], rhs=bt[:], start=True, stop=True), ["c_blkb", KBT], PQ)
                V(lambda e, bon=bon: e.tensor_tensor(out=bon[:], in0=pq[:], in1=vs[:], op=ALU.mult), PQ + [rk[2]], [KBON])
                cs_, u_ = f32t[4], f32t[5]
                V(lambda e: e.tensor_tensor_scan(out=cs_[:], data0=g.rm[:], data1=t0_[:], initial=0.0, op0=ALU.mult, op1=ALU.add), ["c_rm", "x_f0"], ["x_f4"])
                A(lambda e: e.activation(out=u_[:], in_=cs_[:], func=AF.Exp, scale=-E05), ["x_f4"], ["x_f5"])
                V(lambda e, AR=AR: e.tensor_tensor(out=AR[:, :, 64:128], in0=ar3(rs), in1=ar3(u_), op=ALU.mult), [rk[0], "x_f5"], [KAR])
                V(lambda e, p=p: e.tensor_copy(out=dSr[:, p, :], in_=c3(u_)[:, :, 63]), ["x_f5"], ["x_dSr"])
                A(lambda e: e.activation(out=u_[:], in_=cs_[:], func=AF.Exp, scale=E05), ["x_f4"], ["x_f5"])
                V(lambda e, kt=kt: e.tensor_tensor(out=kt[:], in0=ks[:], in1=u_[:], op=ALU.mult), [rk[1], "x_f5"], [KKT])
                V(lambda e: e.tensor_tensor(out=t3_[:], in0=t2_[:], in1=t1_[:], op=ALU.mult), ["x_f2", "x_f1"], ["x_f3"])
                V(lambda e, bt=bt: e.tensor_tensor(out=bt[:], in0=t3_[:], in1=u_[:], op=ALU.mult), ["x_f3", "x_f5"], [KBT])
                V(lambda e: e.tensor_tensor(out=cs_[:], in0=cs_[:], in1=t0_[:], op=ALU.subtract), ["x_f4", "x_f0"], ["x_f4"])
                A(lambda e: e.activation(out=u_[:], in_=cs_[:], func=AF.Exp, scale=-E05), ["x_f4"], ["x_f5"])
                V(lambda e, AR=AR: e.scalar_tensor_tensor(out=AR[:, :, 0:64], in0=ar3(t2_), scalar=-1.0, in1=ar3(u_), op0=ALU.mult, op1=ALU.mult), ["x_f2", "x_f5"], [KAR])
            w_rel(3)
            HH = [slice(0, 64), slice(64, 128)]
            PR = range(4)
            ka = lambda p: "x_pw%d" % p
            kb = lambda p: "x_pw%d" % p
            for c in range(8):
                cs = slice(c * 64, c * 64 + 64)
                for p in PR:
                    for rows in HH:
                        for q, (lt, lk) in enumerate(((bts[p], "x_bt%d" % p), (kts[p], "x_kt%d" % p))):
                            P(lambda e, rows=rows, q=q, lt=lt, p=p: e.matmul(pw[p][rows, q * 128:(q + 1) * 128], lhsT=lt[rows, cs], rhs=ARs[p][rows, c, :], start=True, stop=True),
                              [lk, "x_AR%d" % p], [ka(p)])
                for p in PR:
                    V(lambda e, p=p: e.tensor_tensor(out=ABKs[p][:], in0=pw[p][:, 0:256], in1=g.m4[:], op=ALU.mult), [ka(p), "c_m4"], ["x_ABK%d" % p])
                for p in PR:
                    for rows in HH:
                        for q, (src, sk) in enumerate(((bts[p], "x_bt%d" % p), (kts[p], "x_kt%d" % p), (vbs[p], "x_vb%d" % p))):
                            P(lambda e, rows=rows, q=q, src=src, p=p: e.matmul(pw[p][rows, 256 + q * 64:256 + (q + 1) * 64], lhsT=src[rows, cs], rhs=g.identb[rows, rows], start=True, stop=True),
                              [sk, "identb"], [kb(p)])
                for p in PR:
                    A(lambda e, p=p: e.copy(out=TMss[p][:], in_=pw[p][:, 256:448]), [kb(p)], ["x_TMs%d" % p])
                for p in PR:
                    A(lambda e, p=p: e.copy(out=MTs[p][0:64, 0:64], in_=ABKs[p][0:64, 0:64]), ["x_ABK%d" % p], ["x_MT%d" % p])
                    A(lambda e, p=p: e.copy(out=MTs[p][64:128, 64:128], in_=ABKs[p][64:128, 0:64]), ["x_ABK%d" % p], ["x_MT%d" % p])
                for p in PR:
                    P(lambda e, p=p: e.transpose(ptb[:, p, :], MTs[p][:], g.identb[:]), ["x_MT%d" % p, "identb"], ["x_ptb"])
                for p in PR:
                    A(lambda e, p=p: e.copy(out=MNs[p][0][:], in_=ptb[:, p, :]), ["x_ptb"], ["x_MN%d_0" % p])
                    V(lambda e, p=p: e.tensor_tensor(out=Xs[p][:], in0=MTs[p][:], in1=g.identb[:], op=ALU.add), ["x_MT%d" % p, "identb"], ["x_X%d" % p])
                curP = [(MTs[p], "x_MT%d" % p) for p in PR]
                curT = [(MNs[p][0], "x_MN%d_0" % p) for p in PR]
                for lev in range(1, 6):
                    nT = [(MNs[p][lev % 2], "x_MN%d_%d" % (p, lev % 2)) for p in PR]
                    nP = [(NPs[p][lev % 2], ("x_NP%d_1" % p) if lev % 2 == 1 else ("x_MT%d" % p)) for p in PR]
                    for p in PR:
                        P(lambda e, p=p, a_=curP[p][0], b_=curT[p][0]: e.matmul(pw[p][:, 0:128], lhsT=a_[:], rhs=b_[:], start=True, stop=True), [curP[p][1], curT[p][1]], [ka(p)])
                        if lev < 5:
                            P(lambda e, p=p, a_=curP[p][0], b_=curT[p][0]: e.matmul(pw[p][:, 128:256], lhsT=b_[:], rhs=a_[:], start=True, stop=True), [curP[p][1], curT[p][1]], [ka(p)])
                    for p in PR:
                        A(lambda e, p=p, t_=nT[p][0]: e.copy(out=t_[:], in_=pw[p][:, 0:128]), [ka(p)], [nT[p][1]])
                        if lev < 5:
                            V(lambda e, p=p, t_=nP[p][0]: e.tensor_copy(out=t_[:], in_=pw[p][:, 128:256]), [ka(p)], [nP[p][1]])
                    for p in PR:
                        P(lambda e, p=p, t_=nT[p][0]: e.matmul(pw[p][:, 256:384], lhsT=t_[:], rhs=Xs[p][:], start=True, stop=True), [nT[p][1], "x_X%d" % p], [kb(p)])
                    for p in PR:
                        V(lambda e, p=p: e.tensor_tensor(out=Xs[p][:], in0=Xs[p][:], in1=pw[p][:, 256:384], op=ALU.add), ["x_X%d" % p, kb(p)], ["x_X%d" % p])
                    if lev < 5:
                        curP = nP
                    curT = nT
                for p in PR:
                    for rows in HH:
                        P(lambda e, rows=rows, p=p: e.matmul(pw[p][rows, 384:448], lhsT=ARs[p][rows, c, 0:64], rhs=Wbf[rows, p, :], start=True, stop=False), ["x_AR%d" % p, "x_Wbf%d" % p], [kb(p)])
                        P(lambda e, rows=rows, p=p: e.matmul(pw[p][rows, 384:448], lhsT=ABKs[p][rows, 128:192], rhs=TMss[p][rows, 128:192], start=False, stop=True), ["x_ABK%d" % p, "x_TMs%d" % p], [kb(p)])
                for p in PR:
                    V(lambda e, p=p: e.tensor_copy(out=Wsbs[p][:], in_=pw[p][:, 384:448]), [kb(p)], ["x_Wsb%d" % p])
                for p in PR:
                    P(lambda e, p=p: e.matmul(pw[p][:, 448:512], lhsT=Xs[p][:], rhs=Wsbs[p][:], start=True, stop=True), ["x_X%d" % p, "x_Wsb%d" % p], [kb(p)])
                for p in PR:
                    A(lambda e, p=p: e.copy(out=Usbs[p][:], in_=pw[p][:, 448:512]), [kb(p)], ["x_Usb%d" % p])
                for p in PR:
                    for rows in HH:
                        P(lambda e, rows=rows, p=p: e.matmul(pw[p][rows, 256:320], lhsT=Wbf[rows, p, :], rhs=ARs[p][rows, c, 64:128], start=True, stop=False), ["x_Wbf%d" % p, "x_AR%d" % p], [kb(p)])
                        P(lambda e, rows=rows, p=p: e.matmul(pw[p][rows, 256:320], lhsT=Usbs[p][rows, :], rhs=ABKs[p][rows, 64:128], start=False, stop=False), ["x_Usb%d" % p, "x_ABK%d" % p], [kb(p)])
                        P(lambda e, rows=rows, p=p: e.matmul(pw[p][rows, 256:320], lhsT=TMss[p][rows, 128:192], rhs=ABKs[p][rows, 192:256], start=False, stop=True), ["x_TMs%d" % p, "x_ABK%d" % p], [kb(p)])
                for p in PR:
                    A(lambda e, p=p: e.copy(out=ymix[:, p, cs], in_=pw[p][:, 256:320]), [kb(p)], ["x_ymix%d" % p])
                for p in PR:
                    for rows in HH:
                        P(lambda e, rows=rows, p=p: e.matmul(pw[p][rows, 320:384], lhsT=TMss[p][rows, 0:64], rhs=Usbs[p][rows, :], start=True, stop=False), ["x_TMs%d" % p, "x_Usb%d" % p], [kb(p)])
                        P(lambda e, rows=rows, p=p: e.matmul(pw[p][rows, 320:384], lhsT=TMss[p][rows, 64:128], rhs=TMss[p][rows, 128:192], start=False, stop=True), ["x_TMs%d" % p], [kb(p)])
                for p in PR:
                    V(lambda e, p=p: e.tensor_tensor(out=tSs[p][:], in0=Wst[:, p, :], in1=pw[p][:, 320:384], op=ALU.add), ["x_Wst%d" % p, kb(p)], ["x_tS%d" % p])
                    V(lambda e, p=p: e.tensor_scalar(out=Wst[:, p, :], in0=tSs[p][:], scalar1=dSr[:, p, c:c + 1], scalar2=None, op0=ALU.mult), ["x_tS%d" % p, "x_dSr"], ["x_Wst%d" % p])
                    A(lambda e, p=p: e.copy(out=Wbf[:, p, :], in_=Wst[:, p, :]), ["x_Wst%d" % p], ["x_Wbf%d" % p])
            for p in range(4):
                pc = slice(p * 128, p * 128 + 128)
                ta = f32t[2 * (p % 2)]; tak = "x_f%d" % (2 * (p % 2)); tb_ = f32t[2 * (p % 2) + 1]; tbk = "x_f%d" % (2 * (p % 2) + 1)
                pe_ = pw[p]; PEK = ["x_pw%d" % p]
                OK_ = "x_ymix%d" % p
                P(lambda e, p=p, pe_=pe_: e.matmul(pe_[:], lhsT=g.blk[:], rhs=ymix[:, p, :], start=True, stop=True), ["c_blk", OK_], PEK)
                V(lambda e, p=p, pe_=pe_, ta=ta: e.scalar_tensor_tensor(out=ta[:], in0=pe_[:], scalar=-1.0 / 64, in1=ymix[:, p, :], op0=ALU.mult, op1=ALU.add), PEK + [OK_], [tak])
                A(lambda e, ta=ta, p=p: e.activation(out=bts[p][:], in_=ta[:], func=AF.Square), [tak], ["x_bt%d" % p])
                P(lambda e, pe_=pe_, p=p: e.matmul(pe_[:], lhsT=g.blkb[:], rhs=bts[p][:], start=True, stop=True), ["c_blkb", "x_bt%d" % p], PEK)
                V(lambda e, pe_=pe_, tb_=tb_: e.tensor_scalar(out=tb_[:], in0=pe_[:], scalar1=1.0 / 64, scalar2=64e-5, op0=ALU.mult, op1=ALU.add), PEK, [tbk])
                A(lambda e, tb_=tb_: e.activation(out=tb_[:], in_=tb_[:], func=AF.Sqrt), [tbk], [tbk])
                V(lambda e, tb_=tb_: e.reciprocal(out=tb_[:], in_=tb_[:]), [tbk], [tbk])
                V(lambda e, ta=ta, tb_=tb_: e.tensor_tensor(out=ta[:], in0=ta[:], in1=tb_[:], op=ALU.mult), [tak, tbk], [tak])
                V(lambda e, p=p, ta=ta: e.tensor_scalar(out=ta[:], in0=ta[:], scalar1=c_lw(p), scalar2=c_lb(p), op0=ALU.mult, op1=ALU.add), [tak, CK], [tak])
                V(lambda e, p=p, ta=ta: e.tensor_tensor(out=ta[:], in0=ta[:], in1=bons[p][:], op=ALU.add), [tak, "x_bon%d" % p], [tak])
                P(lambda e, pc=pc, pe_=pe_: e.matmul(pe_[:], lhsT=g2t[:, pc], rhs=lora[1][:], start=True, stop=True), ["x_g2", "x_lora1"], PEK)
                V(lambda e, p=p, ta=ta, pe_=pe_: e.tensor_tensor(out=yT[2][:, p, :], in0=ta[:], in1=pe_[:], op=ALU.mult), [tak] + PEK, ["x_yT2"])

            if dbg is not None and blk == dbg[1]:
                for b in range(3):
                    S.dma("pool", dbg[0][b], yT[b][:], reads=["x_yT%d" % b], writes=["dbg"])

            for b, brn in enumerate((io.br_hg, io.br_ret, io.br_rw)):
                BR, bk = w_get()
                for gh in range(2):
                    W, wk = w_get()
                    for dl in range(4):
                        dc = gh * 4 + dl
                        pzt, pk = proj_fm(W, wk, dl * 128)
                        db = dl % 2
                        sgm = f32t[2 * db]; sgk = "x_f%d" % (2 * db); prd = f32t[2 * db + 1]; prk = "x_f%d" % (2 * db + 1)
                        pbr = pw[db]; pbk = "x_pw%d" % db
                        A(lambda e, pzt=pzt, sgm=sgm: e.activation(out=sgm[:], in_=pzt[:], func=AF.Sigmoid), [pk], [sgk])
                        for k in range(4):
                            P(lambda e, k=k, dc=dc, b=b, BR=BR, pbr=pbr: e.matmul(pbr[:], lhsT=BR[:, k, dc * 128:(dc + 1) * 128], rhs=yT[b][:, k, :], start=(k == 0), stop=(k == 3)),
                              [bk, "x_yT%d" % b], [pbk])
                        ymk = "x_ymix%d" % dc
                        if b == 0:
                            V(lambda e, dc=dc, sgm=sgm, pbr=pbr: e.tensor_tensor(out=ymix[:, dc, :], in0=sgm[:], in1=pbr[:], op=ALU.mult), [sgk, pbk], [ymk])
                        else:
                            V(lambda e, sgm=sgm, pbr=pbr, prd=prd: e.tensor_tensor(out=prd[:], in0=sgm[:], in1=pbr[:], op=ALU.mult), [sgk, pbk], [prk])
                            if b == 1:
                                V(lambda e, dc=dc, prd=prd: e.tensor_tensor(out=ymix[:, dc, :], in0=ymix[:, dc, :], in1=prd[:], op=ALU.add), [ymk, prk], [ymk])
                            else:
                                V(lambda e, dc=dc, prd=prd: e.tensor_tensor(out=ymixb[:, dc, :], in0=ymix[:, dc, :], in1=prd[:], op=ALU.add), [ymk, prk], ["x_ymixb"])
                    if gh == 1:
                        w_rel(3)
            Wo = [w_get() for hh in range(2)]
            for hh in range(2):
                S.dma("sp", f32t[hh][:], io.modrow[l:l + 1, 2 * D + hh * 512:2 * D + (hh + 1) * 512].to_broadcast([128, 512]),
                      reads=["modrow%d" % l], writes=["x_f%d" % hh])
            for i in range(4):
                xb = xt[i % 2]; xk = "x_xt%d" % (i % 2)
                S.dma("sp", xb[:], x_src[t0 + i * 128:t0 + (i + 1) * 128, :], reads=[xs_key], writes=[xk])
                for hh in range(2):
                    pi_ = pzc[0] % 2; pzc[0] += 1
                    for k in range(8):
                        P(lambda e, k=k, i=i, hh=hh, pi_=pi_: e.matmul(pz[pi_][:], lhsT=ymixb[:, k, i * 128:(i + 1) * 128], rhs=Wo[hh][0][:, k, :], start=(k == 0), stop=(k == 7)),
                          ["x_ymixb", Wo[hh][1]], ["x_pz%d" % pi_])
                    hs = slice(hh * 512, hh * 512 + 512)
                    V(lambda e, pi_=pi_, hs=hs, hh=hh: e.tensor_tensor(out=hf[:, hs], in0=pz[pi_][:], in1=f32t[hh][:], op=ALU.mult), ["x_pz%d" % pi_, "x_f%d" % hh], ["x_hf"])
                    V(lambda e, xb=xb, hs=hs: e.tensor_tensor(out=xb[:, hs], in0=xb[:, hs], in1=hf[:, hs], op=ALU.add), ["x_hf", xk], [xk])
                S.dma("sp", x_dst[t0 + i * 128:t0 + (i + 1) * 128, :], xb[:], reads=[xk], writes=[xd_key])
            w_rel(2)
    S.barrier()


_NAMES = ["ada_w", "ada_b", "norm1_g", "norm2_g", "w_in", "hg_lb_table", "hg_norm_w", "rw_mu", "rw_w0", "rw_w2",
          "rw_a0", "rw_a2", "rw_g2", "rw_k_k", "rw_k_a", "rw_ln_w", "rw_ln_b", "br_hg", "br_ret", "br_rw", "w_out",
          "router_g", "router_e", "moe_w1", "moe_w3", "moe_w2"]


def make_in_maps(inputs):
    f = lambda a: np.ascontiguousarray(np.asarray(a, dtype=np.float32))
    shared = {n: f(inputs[n]) for n in _NAMES}
    shared["rw_r_k"] = f(inputs["rw_r_k"]).reshape(NL, 512)
    shared["final_g"] = f(inputs["final_g"]).reshape(1, D)
    x = f(inputs["x"]); c = f(inputs["c"])
    pos = np.ascontiguousarray(np.asarray(inputs["positions"], dtype=np.int32))
    maps = []
    for b in range(8):
        m = dict(shared)
        m["x"] = x[b]; m["c"] = c[b:b + 1]; m["positions"] = pos[b:b + 1]
        maps.append(m)
    return maps


def kernel(**inputs):
    nc = build()
    maps = make_in_maps(inputs)
    res = run_bass_kernel_spmd(nc, maps, core_ids=list(range(8)))
    return np.stack([np.asarray(r["out"], dtype=np.float32) for r in res.results], axis=0)
```
